# Optimizing a Trainium2 kernel written in Bass

```python
import math
import jax, jax.numpy as jnp
from jax import lax
import numpy as np

D_MODEL = 1024
BATCH = 16
SEQ = 4096
DEPTH = 2

GRID_W = 64
CTX_LEN = 256
HEAD_DIM = 64
Q_BLOCK = 128
ROPE_THETA = 10000.0
NORM_EPS = 1e-6
GQA_HEADS = 8
GQA_KV_HEADS = 2
RET_HEADS = 4
RET_DK = HEAD_DIM
RET_DV = 2 * HEAD_DIM
RET_CHUNK = 128
DIFF_HEADS = D_MODEL // (2 * HEAD_DIM)
DIFF_DV = 2 * HEAD_DIM
A_Q = GQA_HEADS * HEAD_DIM
A_KV = GQA_KV_HEADS * HEAD_DIM
B_QK = RET_HEADS * RET_DK
B_V = RET_HEADS * RET_DV
EVEN_IN = A_Q + 2 * A_KV + 2 * B_QK + 2 * B_V
EVEN_MIX = A_Q + B_V
C_QK = 2 * DIFF_HEADS * HEAD_DIM
C_V = DIFF_HEADS * DIFF_DV
ODD_IN = 2 * C_QK + C_V
ODD_MIX = C_V
PEER_HEADS = 8
PEER_NKEYS = 128
PEER_EXPERTS = PEER_NKEYS * PEER_NKEYS
PEER_DKEY = 256
PEER_DHALF = PEER_DKEY // 2
PEER_TOPK = 16
PEER_BLOCK = 128
N_EVEN = (DEPTH + 1) // 2
N_ODD = DEPTH // 2

kernel_name = "hybrid_gqa_retention_diffattn_peer_dit"


def _split(p, sizes):
    out, off = [], 0
    for s in sizes:
        out.append(p[..., off:off + s])
        off += s
    return out


def _rmsnorm(x, g=None):
    xf = x.astype(jnp.float32)
    y = xf * lax.rsqrt(jnp.mean(xf * xf, axis=-1, keepdims=True) + NORM_EPS)
    if g is not None:
        y = y * g.astype(jnp.float32)
    return y.astype(x.dtype)


def _modulate(h, shift, scale):
    return h * (1.0 + scale) + shift


def _axial_angles(n_tokens):
    rows = n_tokens // GRID_W
    row_id = jnp.broadcast_to(jnp.arange(rows)[:, None], (rows, GRID_W)).reshape(-1).astype(jnp.float32)
    col_id = jnp.broadcast_to(jnp.arange(GRID_W)[None, :], (rows, GRID_W)).reshape(-1).astype(jnp.float32)
    axis_dim = HEAD_DIM // 2
    inv = ROPE_THETA ** (-jnp.arange(0, axis_dim, 2, dtype=jnp.float32) / axis_dim)
    ang_r = row_id[:, None] * inv[None, :]
    ang_c = col_id[:, None] * inv[None, :]
    return (jnp.cos(ang_r), jnp.sin(ang_r), jnp.cos(ang_c), jnp.sin(ang_c))


def _rot_half(u, cos, sin):
    u1, u2 = jnp.split(u, 2, axis=-1)
    return jnp.concatenate([u1 * cos - u2 * sin, u1 * sin + u2 * cos], axis=-1)


def _axial_rope(x, rope):
    cr, sr, cc, sc = (t[None, :, None, :] for t in rope)
    xf = x.astype(jnp.float32)
    half = HEAD_DIM // 2
    out = jnp.concatenate([_rot_half(xf[..., :half], cr, sr),
                           _rot_half(xf[..., half:], cc, sc)], axis=-1)
    return out.astype(x.dtype)


def _gqa_attend(q, k, v):
    b, t, kvh, g, d = q.shape
    nb = t // Q_BLOCK
    qb = jnp.swapaxes(q.reshape(b, nb, Q_BLOCK, kvh, g, d), 0, 1)
    scale = d ** -0.5

    def block(qi):
        s = jnp.einsum('bqhgd,bkhd->bhgqk', qi, k).astype(jnp.float32) * scale
        p = jax.nn.softmax(s, axis=-1).astype(v.dtype)
        return jnp.einsum('bhgqk,bkhd->bqhgd', p, v)

    o = lax.map(block, qb)
    return jnp.swapaxes(o, 0, 1).reshape(b, t, kvh * g * d)


def _diff_attend(q1, q2, k1, k2, v, lam):
    b, t, h, d = q1.shape
    nb = t // Q_BLOCK
    scale = d ** -0.5

    def split(a):
        return jnp.swapaxes(a.reshape(b, nb, Q_BLOCK, h, d), 0, 1)

    def block(qs):
        qa, qb = qs
        p1 = jax.nn.softmax(jnp.einsum('bqhd,bkhd->bhqk', qa, k1).astype(jnp.float32) * scale, axis=-1)
        p2 = jax.nn.softmax(jnp.einsum('bqhd,bkhd->bhqk', qb, k2).astype(jnp.float32) * scale, axis=-1)
        p = (p1 - lam * p2).astype(v.dtype)
        return jnp.einsum('bhqk,bkhe->bqhe', p, v)

    o = lax.map(block, (split(q1), split(q2)))
    return jnp.swapaxes(o, 0, 1).reshape(b, t, h, v.shape[-1])


def _retention(q, k, v, log_g, s0):
    b, t, h, dk = q.shape
    dv = v.shape[-1]
    n = t // RET_CHUNK
    qc = q.reshape(b, n, RET_CHUNK, h, dk)
    kc = k.reshape(b, n, RET_CHUNK, h, dk)
    vc = v.reshape(b, n, RET_CHUNK, h, dv)
    i = jnp.arange(RET_CHUNK, dtype=jnp.float32)
    rel = i[:, None] - i[None, :]
    dmask = jnp.where(rel >= 0, jnp.exp(log_g[:, None, None] * jnp.maximum(rel, 0.0)), 0.0)
    sc = jnp.einsum('bnihd,bnjhd->bnhij', qc, kc) * dmask
    intra = jnp.einsum('bnhij,bnjhe->bnihe', sc, vc)
    k_dec = jnp.exp(log_g[None, :] * (RET_CHUNK - 1.0 - i)[:, None])
    chunk_kv = jnp.einsum('bnjhd,jh,bnjhe->nbhde', kc, k_dec, vc)
    chunk_decay = jnp.exp(log_g * RET_CHUNK)[None, :, None, None]

    def step(state, kv):
        return chunk_decay * state + kv, state

    s_final, s_prev = lax.scan(step, s0, chunk_kv)
    q_dec = jnp.exp(log_g[None, :] * (i + 1.0)[:, None])
    cross = jnp.einsum('bnihd,ih,nbhde->bnihe', qc, q_dec, s_prev)
    return (intra + cross).reshape(b, t, h, dv), s_final


def _even_mixer(h, hc, w_in, w_out, q_norm_g, k_norm_g, ret_log_rate, rope, need_ctx):
    b = h.shape[0]
    log_gf = -jnp.exp(ret_log_rate[0].astype(jnp.float32))
    log_gb = -jnp.exp(ret_log_rate[1].astype(jnp.float32))

    def project(z, use_rope):
        bz, t, _ = z.shape
        qa, ka, va, qb, kb, vb, gb = _split(z @ w_in, (A_Q, A_KV, A_KV, B_QK, B_QK, B_V, B_V))
        qa = _rmsnorm(qa.reshape(bz, t, GQA_HEADS, HEAD_DIM), q_norm_g)
        ka = _rmsnorm(ka.reshape(bz, t, GQA_KV_HEADS, HEAD_DIM), k_norm_g)
        va = va.reshape(bz, t, GQA_KV_HEADS, HEAD_DIM)
        qb = qb.reshape(bz, t, RET_HEADS, RET_DK)
        kb = kb.reshape(bz, t, RET_HEADS, RET_DK) * (RET_DK ** -0.5)
        vb = vb.reshape(bz, t, RET_HEADS, RET_DV)
        if use_rope:
            qa, ka, qb, kb = (_axial_rope(u, rope) for u in (qa, ka, qb, kb))
        qa = qa.reshape(bz, t, GQA_KV_HEADS, GQA_HEADS // GQA_KV_HEADS, HEAD_DIM)
        return qa, ka, va, qb, kb, vb, gb

    def flip(u):
        return jnp.flip(u, axis=1)

    def ret_out(o, g):
        bz, t = o.shape[:2]
        return _rmsnorm(o).reshape(bz, t, B_V).astype(g.dtype) * jax.nn.silu(g)

    qa, ka, va, qb, kb, vb, gb = project(h, True)
    qac, kac, vac, qbc, kbc, vbc, gbc = project(hc, False)
    ya = _gqa_attend(qa, jnp.concatenate([kac, ka], axis=1), jnp.concatenate([vac, va], axis=1))
    zeros = jnp.zeros((b, RET_HEADS, RET_DK, RET_DV), jnp.float32)
    oc_f, sc_f = _retention(qbc, kbc, vbc, log_gf, zeros)
    oc_b, sc_b = _retention(flip(qbc), flip(kbc), flip(vbc), log_gb, zeros)
    o_f, _ = _retention(qb, kb, vb, log_gf, sc_f)
    o_b, _ = _retention(flip(qb), flip(kb), flip(vb), log_gb, sc_b)
    y = jnp.concatenate([ya, ret_out(o_f + flip(o_b), gb)], axis=-1) @ w_out
    yc = None
    if need_ctx:
        yac = _gqa_attend(qac, kac, vac)
        yc = jnp.concatenate([yac, ret_out(oc_f + flip(oc_b), gbc)], axis=-1) @ w_out
    return y, yc


def _odd_mixer(h, hc, w_in, w_out, lam_p, subln_g, lam_init, rope, need_ctx):
    lf = lam_p.astype(jnp.float32)
    lam = jnp.exp(jnp.sum(lf[0] * lf[1])) - jnp.exp(jnp.sum(lf[2] * lf[3])) + lam_init

    def project(z, use_rope):
        bz, t, _ = z.shape
        q, k, v = _split(z @ w_in, (C_QK, C_QK, C_V))
        q = q.reshape(bz, t, 2 * DIFF_HEADS, HEAD_DIM)
        k = k.reshape(bz, t, 2 * DIFF_HEADS, HEAD_DIM)
        if use_rope:
            q, k = _axial_rope(q, rope), _axial_rope(k, rope)
        q = q.reshape(bz, t, DIFF_HEADS, 2, HEAD_DIM)
        k = k.reshape(bz, t, DIFF_HEADS, 2, HEAD_DIM)
        return q[..., 0, :], q[..., 1, :], k[..., 0, :], k[..., 1, :], v.reshape(bz, t, DIFF_HEADS, DIFF_DV)

    def finish(o):
        bz, t = o.shape[:2]
        return (_rmsnorm(o, subln_g) * (1.0 - lam_init)).reshape(bz, t, ODD_MIX) @ w_out

    q1, q2, k1, k2, v = project(h, True)
    q1c, q2c, k1c, k2c, vc = project(hc, False)
    y = finish(_diff_attend(q1, q2,
                            jnp.concatenate([k1c, k1], axis=1),
                            jnp.concatenate([k2c, k2], axis=1),
                            jnp.concatenate([vc, v], axis=1), lam))
    yc = finish(_diff_attend(q1c, q2c, k1c, k2c, vc, lam)) if need_ctx else None
    return y, yc


def _peer(h, w_q, keys, u_tab, v_tab):
    n, d = h.shape
    hb = h.reshape(n // PEER_BLOCK, PEER_BLOCK, d)

    def block(xb):
        q = (xb @ w_q).reshape(PEER_BLOCK, PEER_HEADS, 2, PEER_DHALF)
        s = jnp.einsum('thpd,hpnd->thpn', q, keys).astype(jnp.float32)
        s_top, i_top = lax.top_k(s, PEER_TOPK)
        cand = (s_top[:, :, 0, :, None] + s_top[:, :, 1, None, :]).reshape(PEER_BLOCK, PEER_HEADS, PEER_TOPK * PEER_TOPK)
        cid = (i_top[:, :, 0, :, None] * PEER_NKEYS + i_top[:, :, 1, None, :]).reshape(PEER_BLOCK, PEER_HEADS, PEER_TOPK * PEER_TOPK)
        best, pos = lax.top_k(cand, PEER_TOPK)
        eid = jnp.take_along_axis(cid, pos, axis=-1)
        gate = jax.nn.softmax(best, axis=-1)
        act = jax.nn.gelu(jnp.einsum('td,thkd->thk', xb, u_tab[eid]).astype(jnp.float32), approximate=False)
        return jnp.einsum('thk,thkd->td', (gate * act).astype(xb.dtype), v_tab[eid])

    return lax.map(block, hb).reshape(n, d)


def setup_inputs(seed: int = 0) -> dict:
    key = jax.random.key(seed)
    ks = jax.random.split(key, 24)
    f32 = jnp.float32

    def nrm(k, shape, scale):
        return jax.random.normal(k, shape, f32) * scale

    ret_base = jnp.log(-jnp.log1p(-jnp.exp2(-5.0 - jnp.arange(RET_HEADS, dtype=f32))))
    return {
        "x": nrm(ks[0], (BATCH, SEQ, D_MODEL), 1.0),
        "c": nrm(ks[1], (BATCH, D_MODEL), 1.0),
        "ctx": nrm(ks[2], (BATCH, CTX_LEN, D_MODEL), 1.0),
        "c_ctx": nrm(ks[3], (D_MODEL,), 1.0),
        "ada_w": nrm(ks[4], (DEPTH, D_MODEL, 6 * D_MODEL), 0.5 * D_MODEL ** -0.5),
        "ada_b": nrm(ks[5], (DEPTH, 6 * D_MODEL), 0.01),
        "norm1_g": 1.0 + nrm(ks[6], (DEPTH, D_MODEL), 0.02),
        "norm2_g": 1.0 + nrm(ks[7], (DEPTH, D_MODEL), 0.02),
        "ev_w_in": nrm(ks[8], (N_EVEN, D_MODEL, EVEN_IN), D_MODEL ** -0.5),
        "ev_w_out": nrm(ks[9], (N_EVEN, EVEN_MIX, D_MODEL), EVEN_MIX ** -0.5),
        "gqa_q_norm_g": 1.0 + nrm(ks[10], (N_EVEN, HEAD_DIM), 0.02),
        "gqa_k_norm_g": 1.0 + nrm(ks[11], (N_EVEN, HEAD_DIM), 0.02),
        "ret_log_rate": ret_base[None, None, :] + nrm(ks[12], (N_EVEN, 2, RET_HEADS), 0.05),
        "od_w_in": nrm(ks[13], (N_ODD, D_MODEL, ODD_IN), D_MODEL ** -0.5),
        "od_w_out": nrm(ks[14], (N_ODD, ODD_MIX, D_MODEL), ODD_MIX ** -0.5),
        "diff_lambda": nrm(ks[15], (N_ODD, 4, HEAD_DIM), 0.1),
        "diff_subln_g": 1.0 + nrm(ks[16], (N_ODD, DIFF_DV), 0.02),
        "peer_w_q": nrm(ks[17], (DEPTH, D_MODEL, PEER_HEADS * PEER_DKEY), D_MODEL ** -0.5),
        "peer_keys": nrm(ks[18], (DEPTH, PEER_HEADS, 2, PEER_NKEYS, PEER_DHALF), PEER_DHALF ** -0.5),
        "peer_u": nrm(ks[19], (DEPTH, PEER_EXPERTS, D_MODEL), D_MODEL ** -0.5),
        "peer_v": nrm(ks[20], (DEPTH, PEER_EXPERTS, D_MODEL), 0.5),
        "final_g": 1.0 + nrm(ks[21], (D_MODEL,), 0.02),
    }


def reference(x, c, ctx, c_ctx, ada_w, ada_b, norm1_g, norm2_g, ev_w_in, ev_w_out,
              gqa_q_norm_g, gqa_k_norm_g, ret_log_rate, od_w_in, od_w_out, diff_lambda,
              diff_subln_g, peer_w_q, peer_keys, peer_u, peer_v, final_g):
    b, s, d = x.shape
    lc = ctx.shape[1]
    rope = _axial_angles(s)
    xc = ctx
    for l in range(DEPTH):
        need_ctx = l < DEPTH - 1
        mod = jax.nn.silu(c) @ ada_w[l] + ada_b[l]
        mod_c = jax.nn.silu(c_ctx) @ ada_w[l] + ada_b[l]
        sh1, sc1, g1, sh2, sc2, g2 = [m[:, None, :] for m in jnp.split(mod, 6, axis=-1)]
        sh1c, sc1c, g1c, sh2c, sc2c, g2c = [m[None, None, :] for m in jnp.split(mod_c, 6, axis=-1)]
        h = _modulate(_rmsnorm(x, norm1_g[l]), sh1, sc1)
        hc = _modulate(_rmsnorm(xc, norm1_g[l]), sh1c, sc1c)
        if l % 2 == 0:
            e = l // 2
            y, yc = _even_mixer(h, hc, ev_w_in[e], ev_w_out[e], gqa_q_norm_g[e], gqa_k_norm_g[e],
                                ret_log_rate[e], rope, need_ctx)
        else:
            o = l // 2
            lam_init = 0.8 - 0.6 * math.exp(-0.3 * l)
            y, yc = _odd_mixer(h, hc, od_w_in[o], od_w_out[o], diff_lambda[o], diff_subln_g[o],
                               lam_init, rope, need_ctx)
        x = x + (g1 * y).astype(x.dtype)
        h2 = _modulate(_rmsnorm(x, norm2_g[l]), sh2, sc2).reshape(b * s, d)
        if need_ctx:
            xc = xc + (g1c * yc).astype(xc.dtype)
            hc2 = _modulate(_rmsnorm(xc, norm2_g[l]), sh2c, sc2c).reshape(b * lc, d)
            f = _peer(jnp.concatenate([h2, hc2], axis=0), peer_w_q[l], peer_keys[l], peer_u[l], peer_v[l])
            xc = xc + (g2c * f[b * s:].reshape(b, lc, d)).astype(xc.dtype)
        else:
            f = _peer(h2, peer_w_q[l], peer_keys[l], peer_u[l], peer_v[l])
        x = x + (g2 * f[:b * s].reshape(b, s, d)).astype(x.dtype)
    return _rmsnorm(x, final_g)
```

```python
import math
from contextlib import ExitStack

import numpy as np
import concourse.bass as bass
import concourse.mybir as mybir
from concourse.bass_utils import run_bass_kernel_spmd

F32 = mybir.dt.float32
BF16 = mybir.dt.bfloat16
U32 = mybir.dt.uint32
AF = mybir.ActivationFunctionType
ALU = mybir.AluOpType
AX = mybir.AxisListType

D = 1024
KT = 8
EPS = 1e-6
NEG = -1.0e30


class Buf:
    __slots__ = ("t", "w", "r")

    def __init__(self, t=None):
        self.t = t
        self.w = None
        self.r = {}

    def __getitem__(self, k):
        return self.t[k]


class KB:
    RING = 6

    def __init__(self, nc, es):
        self.nc = nc
        self.es = es
        self.h = {"pe": nc.tensor, "act": nc.scalar, "dve": nc.vector, "pool": nc.gpsimd, "sp": nc.sync}
        self.sem = {}
        self.cnt = {}
        self.seen = {}
        self.ring = {}
        for e in self.h:
            self.sem[e] = es.enter_context(nc.semaphore("s_" + e))
            self.cnt[e] = 0
            self.seen[e] = {}
        self.nsem = 0
        self.uid = 0
        self.cur = es

    def _ring(self, q):
        if q not in self.ring:
            sems = [self.es.enter_context(self.nc.semaphore("d_%s_%d" % (q, i))) for i in range(self.RING)]
            self.ring[q] = {"sems": sems, "vals": [0] * self.RING, "i": 0}
        return self.ring[q]

    def sb(self, shape, dt, name=None):
        self.uid += 1
        nm = "%s_%d" % (name or "sb", self.uid)
        return Buf(self.cur.enter_context(self.nc.sbuf_tensor(nm, list(shape), dt)))

    def barrier(self):
        for e in self.h:
            for o in self.h:
                if o != e and self.cnt[o] > 0:
                    self._wait(e, (self.sem[o], self.cnt[o], o, o))
            for q, ring in self.ring.items():
                for j in range(self.RING):
                    if ring["vals"][j] > 0:
                        self._wait(e, (ring["sems"][j], ring["vals"][j], None, "d_%s_%d" % (q, j)))

    def scope(self):
        kb = self

        class _S:
            def __enter__(s_):
                s_.prev = kb.cur
                s_.st = ExitStack()
                kb.cur = s_.st
                return s_

            def __exit__(s_, *a):
                kb.barrier()
                s_.st.close()
                kb.cur = s_.prev
                return False
        return _S()

    def ps(self, shape, dt, name=None):
        self.uid += 1
        return Buf(self.es.enter_context(self.nc.psum_tensor(name or ("ps%d" % self.uid), list(shape), dt)))

    def _wait(self, eng, tok):
        if tok is None:
            return
        sem, val, owner, key = tok
        if self.seen[eng].get(key, 0) >= val:
            return
        self.h[eng].wait_ge(sem, val)
        self.seen[eng][key] = val

    def _deps(self, eng, reads, writes):
        for b in reads:
            self._wait(eng, b.w)
        for b in writes:
            self._wait(eng, b.w)
            for t in b.r.values():
                self._wait(eng, t)

    def op(self, eng, fn, reads=(), writes=()):
        self._deps(eng, reads, writes)
        ins = fn(self.h[eng])
        self.cnt[eng] += 1
        ins.then_inc(self.sem[eng], 1)
        tok = (self.sem[eng], self.cnt[eng], eng, eng)
        for b in reads:
            b.r[eng] = tok
        for b in writes:
            b.w = tok
            b.r = {}
        return tok

    def dma(self, q, out, in_, reads=(), writes=(), indirect=None, **kw):
        self._deps(q, reads, writes)
        ring = self._ring(q)
        j = ring["i"] % self.RING
        ring["i"] += 1
        sem = ring["sems"][j]
        prev = ring["vals"][j]
        key = "d_%s_%d" % (q, j)
        if prev > 0:
            self._wait(q, (sem, prev, None, key))
        if indirect is None:
            ins = self.h[q].dma_start(out=out, in_=in_, **kw)
        else:
            ins = self.h[q].indirect_dma_start(out=out, out_offset=None, in_=in_,
                                               in_offset=bass.IndirectOffsetOnAxis(ap=indirect, axis=0),
                                               bounds_check=self.bnd_reg(), oob_is_err=False)
        ins.then_inc(sem, 16)
        ring["vals"][j] = prev + 16
        tok = (sem, prev + 16, None, key)
        for b in reads:
            b.r[key] = tok
        for b in writes:
            b.w = tok
            b.r = {}
        return tok

    def bnd_reg(self):
        if getattr(self, "_bnd", None) is None:
            self._bnd = self.nc.gpsimd.alloc_register("bnd")
            self.nc.gpsimd.reg_mov(self._bnd, 16383)
        return self._bnd

    def finish(self, bufs):
        for b in bufs:
            self._wait("sp", b.w)


def bc(ap, shape):
    return ap.to_broadcast(list(shape))


def build(NB, S, LC, stop_after=None, dbg=False):
    NT = S // 128
    NCX = LC // 128
    NU = NCX + NT
    R = NB + 1
    nc = bass.Bass("TRN2", target_bir_lowering=False)

    def din(name, shape, dt=F32):
        return nc.dram_tensor(name, list(shape), dt, kind="ExternalInput").ap()

    x_in = din("x", [NB, S, D])
    ctx_in = din("ctx", [NB, LC, D])
    cT_in = din("cT", [128, KT, R])
    ada_w = din("ada_w", [2, D, 6 * D])
    ada_b = din("ada_b", [2, 6 * D])
    ada_bT = din("ada_bT", [2, 128, 48])
    n1gT = din("n1gT", [2, 128, KT])
    n2g = din("n2g", [2, D])
    final_g = din("final_g", [D])
    ev_w_in = din("ev_w_in", [D, 2304])
    ev_w_out = din("ev_w_out", [D, D])
    od_w_in = din("od_w_in", [D, 3072])
    od_w_out = din("od_w_out", [D, D])
    qg_in = din("qg", [64])
    kg_in = din("kg", [64])
    rate_in = din("rate", [8])
    lam_in = din("lam", [256])
    subln_in = din("subln", [128])
    peer_wq = din("peer_wq", [2, D, 2048])
    keysT = din("keysT", [2, 128, 16, 128])
    peer_u = [din("peer_u%d" % i, [16384, D]) for i in range(2)]
    peer_v = [din("peer_v%d" % i, [16384, D]) for i in range(2)]
    ident_in = din("ident", [128, 128])
    rope_in = din("rope", [128, NT, 64])
    iota_in = din("iota16", [128, 16])
    relm_in = din("relm", [128, 128])
    pidx_in = din("pidx", [128, 2])
    fidx_in = din("fidx", [128, 128])
    out_d = nc.dram_tensor("out", [NB, S, D], F32, kind="ExternalOutput").ap()

    def dscr(name, shape, dt=F32):
        return nc.dram_tensor(name, list(shape), dt, kind="Internal").ap()

    XSA = dscr("XSA", [NB, S, D])
    XCA = dscr("XCA", [NB, LC, D])
    XSB = dscr("XSB", [NB, S, D])
    MODR = dscr("MODR", [2, R, 6 * D])
    KVD = dscr("KVD", [NU, 128, 4, 128])
    STD = dscr("STD", [NU, 128, 4, 128], BF16)
    UVB = [dscr("UVB%d" % i, [16384, 2 * D], BF16) for i in range(2)]
    uTAB = Buf()

    es = ExitStack()
    k = KB(nc, es)
    op, dma = k.op, k.dma
    uXSA, uXCA, uXSB, uMODR, uKVD, uSTD, uOUT = (Buf() for _ in range(7))
    uIN = Buf()

    ident_f = k.sb([128, 128], F32, "ident_f")
    ident_b = k.sb([128, 128], BF16, "ident_b")
    ones_b = k.sb([128, 128], BF16, "ones_b")
    iota16 = k.sb([128, 16], F32, "iota16")
    rope = k.sb([128, NT, 64], F32, "rope")
    dma("sp", ident_f[:], ident_in, [uIN], [ident_f])
    dma("sp", iota16[:], iota_in, [uIN], [iota16])
    dma("sp", rope[:], rope_in, [uIN], [rope])
    op("dve", lambda e: e.tensor_copy(out=ident_b[:], in_=ident_f[:]), [ident_f], [ident_b])
    op("dve", lambda e: e.memset(ones_b[:], 1.0), [], [ones_b])

    PB = [k.ps([128, 512], F32, "pb%d" % i) for i in range(6)]
    PT_ = k.ps([128, 1024], BF16, "ptb")
    PT2 = k.ps([128, 1024], BF16, "ptb2")

    sc_silu = k.sb([128, KT, R], F32, "sc_silu")
    dma("sp", sc_silu[:], cT_in, [uIN], [sc_silu])
    op("act", lambda e: e.activation(out=sc_silu[:], in_=sc_silu[:], func=AF.Silu), [sc_silu], [sc_silu])
    A1 = k.sb([128, R, KT], F32, "A1")
    B1 = k.sb([128, R, KT], F32, "B1")
    modT = k.sb([128, 16, R], F32, "modT")
    n1g_sb = k.sb([128, KT], F32, "n1g_sb")
    abT_sb = k.sb([128, 48], F32, "abT_sb")
    rows = {}

    xt_ring = [k.sb([128, D], F32, "xt%d" % i) for i in range(2)]
    xn_b = k.sb([128, D], BF16, "xn_b")
    junk = k.sb([128, D], F32, "junk")
    st4 = k.sb([128, 8], F32, "st4")
    hT_ring = [k.sb([128, KT, 128], BF16, "hT%d" % i) for i in range(2)]
    CW = 256
    wstage = [k.sb([128, KT, CW], F32, "wst%d" % i) for i in range(2)]
    cnt = {"xt": 0, "hT": 0, "ws": 0}

    def load_w(dst, col0, src_ap, c0, n):
        off = 0
        while off < n:
            m = min(CW, n - off)
            ws = wstage[cnt["ws"] % 2]
            cnt["ws"] += 1
            dma("sp", ws[:, :, 0:m], src_ap[:, c0 + off:c0 + off + m].rearrange("(k p) n -> p k n", p=128),
                [uIN], [ws])
            op("pool", lambda e, ws=ws, m=m, o=off: e.tensor_copy(out=dst[:, :, col0 + o:col0 + o + m],
                                                               in_=ws[:, :, 0:m]), [ws], [dst])
            off += m

    def rstd_of(xt, col):
        op("act", lambda e: e.activation(out=junk[:], in_=xt[:], func=AF.Square, accum_out=st4[:, col:col + 1]),
           [xt], [junk, st4])
        op("dve", lambda e: e.tensor_scalar(out=st4[:, col:col + 1], in0=st4[:, col:col + 1], scalar1=1.0 / D,
                                            scalar2=EPS, op0=ALU.mult, op1=ALU.add), [st4], [st4])
        op("act", lambda e: e.activation(out=st4[:, col:col + 1], in_=st4[:, col:col + 1], func=AF.Sqrt),
           [st4], [st4])
        op("dve", lambda e: e.reciprocal(out=st4[:, col:col + 1], in_=st4[:, col:col + 1]), [st4], [st4])

    def tile_prep(src_ap, src_unit, ri):
        xt = xt_ring[cnt["xt"] % 2]
        cnt["xt"] += 1
        hT = hT_ring[cnt["hT"] % 2]
        cnt["hT"] += 1
        dma("sp", xt[:], src_ap, [src_unit], [xt])
        rstd_of(xt, 0)
        op("dve", lambda e: e.tensor_scalar(out=xn_b[:], in0=xt[:], scalar1=st4[:, 0:1], scalar2=None,
                                            op0=ALU.mult), [xt, st4], [xn_b])
        for kk in range(KT):
            op("pe", lambda e, kk=kk: e.transpose(out=PT_[:, kk * 128:(kk + 1) * 128],
                                                  in_=xn_b[:, kk * 128:(kk + 1) * 128], identity=ident_b[:]),
               [xn_b, ident_b], [PT_])
        ptv = PT_[:, :].rearrange("p (k t) -> p k t", k=KT)
        op("dve", lambda e: e.tensor_tensor(out=hT[:], in0=ptv, in1=bc(A1[:, ri, :].unsqueeze(2), [128, KT, 128]),
                                            op=ALU.mult), [PT_, A1], [hT])
        op("dve", lambda e: e.tensor_tensor(out=hT[:], in0=hT[:], in1=bc(B1[:, ri, :].unsqueeze(2), [128, KT, 128]),
                                            op=ALU.add), [B1], [hT])
        return xt, hT

    def proj(hT, W, c0, n, pbank, poff=0):
        for kk in range(KT):
            op("pe", lambda e, kk=kk: e.matmul(pbank[:, poff:poff + n], lhsT=hT[:, kk, :], rhs=W[:, kk, c0:c0 + n],
                                               start=(kk == 0), stop=(kk == KT - 1)), [hT, W], [pbank])

    def rope_apply(dst, src, nh, ti, tmp):
        (db, d0), (sbf, s0) = dst, src
        sv = sbf[:, s0:s0 + nh * 64].rearrange("p (h a b c) -> p h a b c", h=nh, a=2, b=2)
        dv = db[:, d0:d0 + nh * 64].rearrange("p (h a b c) -> p h a b c", h=nh, a=2, b=2)
        t1 = tmp[0][:, 0:nh * 32].rearrange("p (h a c) -> p h a c", h=nh, a=2)
        t2 = tmp[1][:, 0:nh * 32].rearrange("p (h a c) -> p h a c", h=nh, a=2)
        rv = rope[:, ti, :].rearrange("p (a b c) -> p a b c", a=2, b=2)
        cosb = bc(rv[:, :, 0, :].unsqueeze(1), [128, nh, 2, 16])
        sinb = bc(rv[:, :, 1, :].unsqueeze(1), [128, nh, 2, 16])
        u1 = sv[:, :, :, 0, :]
        u2 = sv[:, :, :, 1, :]
        op("dve", lambda e: e.tensor_tensor(out=t1, in0=u1, in1=cosb, op=ALU.mult), [sbf, rope], [tmp[0]])
        op("dve", lambda e: e.tensor_tensor(out=t2, in0=u2, in1=sinb, op=ALU.mult), [sbf, rope], [tmp[1]])
        op("dve", lambda e: e.tensor_tensor(out=dv[:, :, :, 0, :], in0=t1, in1=t2, op=ALU.subtract),
           [tmp[0], tmp[1]], [db])
        op("dve", lambda e: e.tensor_tensor(out=t1, in0=u1, in1=sinb, op=ALU.mult), [sbf, rope], [tmp[0]])
        op("dve", lambda e: e.tensor_tensor(out=t2, in0=u2, in1=cosb, op=ALU.mult), [sbf, rope], [tmp[1]])
        op("dve", lambda e: e.tensor_tensor(out=dv[:, :, :, 1, :], in0=t1, in1=t2, op=ALU.add),
           [tmp[0], tmp[1]], [db])

    def mod_phase(l):
      with k.scope():
        modrow_sb = k.sb([R, 6 * D], F32, "modrow_sb")
        adab_rows = k.sb([R, 6 * D], F32, "adab_rows")
        dma("sp", adab_rows[:], ada_b[l, :].partition_broadcast(R), [uIN], [adab_rows])
        dma("sp", abT_sb[:], ada_bT[l], [uIN], [abT_sb])
        dma("sp", n1g_sb[:], n1gT[l], [uIN], [n1g_sb])
        pm, pt = PB[0], PB[1]
        for cc in range(6 * D // CW):
            ws = wstage[cnt["ws"] % 2]
            cnt["ws"] += 1
            dma("sp", ws[:], ada_w[l, :, cc * CW:(cc + 1) * CW].rearrange("(k p) n -> p k n", p=128), [uIN], [ws])
            for kk in range(KT):
                op("pe", lambda e, kk=kk: e.matmul(pm[0:R, 0:CW], lhsT=sc_silu[:, kk, :], rhs=ws[:, kk, :],
                                                   start=(kk == 0), stop=(kk == KT - 1)), [sc_silu, ws], [pm])
            op("dve", lambda e, cc=cc: e.tensor_tensor(out=modrow_sb[:, cc * CW:(cc + 1) * CW], in0=pm[0:R, 0:CW],
                                                       in1=adab_rows[:, cc * CW:(cc + 1) * CW], op=ALU.add),
               [pm, adab_rows], [modrow_sb])
            if cc < 2048 // CW:
                for jj in range(CW // 128):
                    j = cc * (CW // 128) + jj
                    for kk in range(KT):
                        op("pe", lambda e, kk=kk, jj=jj, j=j: e.matmul(
                            pt[:, j * R:(j + 1) * R], lhsT=ws[:, kk, jj * 128:(jj + 1) * 128], rhs=sc_silu[:, kk, :],
                            start=(kk == 0), stop=(kk == KT - 1)), [sc_silu, ws], [pt])
        op("dve", lambda e: e.tensor_tensor(out=modT[:], in0=pt[:, 0:16 * R].rearrange("p (j r) -> p j r", r=R),
                                            in1=bc(abT_sb[:, 0:16].unsqueeze(2), [128, 16, R]), op=ALU.add),
           [pt, abT_sb], [modT])
        for r in range(R):
            op("dve", lambda e, r=r: e.scalar_tensor_tensor(out=A1[:, r, :], in0=modT[:, 8:16, r], scalar=1.0,
                                                            in1=n1g_sb[:], op0=ALU.add, op1=ALU.mult),
               [modT, n1g_sb], [A1])
            op("dve", lambda e, r=r: e.tensor_copy(out=B1[:, r, :], in_=modT[:, 0:8, r]), [modT], [B1])
        dma("sp", MODR[l], modrow_sb[:], [modrow_sb], [uMODR])

    def alloc_rows_attn():
        rows["G1"] = [k.sb([128, D], F32, "rowG1_%d" % i) for i in range(2)]

    def alloc_rows_peer():
        rows["G2"] = [k.sb([128, D], F32, "rowG2_%d" % i) for i in range(2)]
        rows["SH2"] = [k.sb([128, D], F32, "rowSH2_%d" % i) for i in range(2)]
        rows["g2"] = [k.sb([128, D], F32, "rowg2_%d" % i) for i in range(2)]

    def load_rows_attn(l, r, slot):
        dma("sp", rows["G1"][slot][:], MODR[l, r, 2 * D:3 * D].partition_broadcast(128), [uMODR], [rows["G1"][slot]])

    def load_rows_peer(l, r, slot):
        def row(c0):
            return MODR[l, r, c0:c0 + D].partition_broadcast(128)
        rowG2, rowSH2, rowg2 = rows["G2"], rows["SH2"], rows["g2"]
        n2g_row = junk
        dma("sp", n2g_row[:], n2g[l, :].partition_broadcast(128), [uIN], [n2g_row])
        dma("sp", rowSH2[slot][:], row(3 * D), [uMODR], [rowSH2[slot]])
        dma("sp", rowG2[slot][:], row(4 * D), [uMODR], [rowG2[slot]])
        dma("sp", rowg2[slot][:], row(5 * D), [uMODR], [rowg2[slot]])
        op("dve", lambda e: e.scalar_tensor_tensor(out=rowG2[slot][:], in0=rowG2[slot][:], scalar=1.0,
                                                   in1=n2g_row[:], op0=ALU.add, op1=ALU.mult),
           [n2g_row], [rowG2[slot]])

    ynew = k.sb([128, D], F32, "ynew")
    KTall = Vall = Wp = Wo = QTst = mixT = basex = PTr = qtm = qtb = rt = sq8 = None
    rec = rec2 = ot1 = ot2 = osq = qg_row = kg_row = None
    cnt["ptr"] = 0

    def alloc_attn(kt_shape, v_shape, wcols, wo_shape, q_shape, mix_shape):
        nonlocal KTall, Vall, Wp, Wo, QTst, mixT, basex, PTr, qtm, qtb, rt, sq8, rec, rec2, ot1, ot2, osq
        nonlocal qg_row, kg_row
        if kt_shape is not None:
            KTall = k.sb(kt_shape, BF16, "KTall")
            Vall = k.sb(v_shape, BF16, "Vall")
        Wp = k.sb([128, KT, wcols], BF16, "Wp")
        Wo = k.sb(wo_shape, BF16, "Wo")
        QTst = k.sb(q_shape, BF16, "QTst")
        mixT = k.sb(mix_shape, BF16, "mixT")
        basex = k.sb([128, D], F32, "basex")
        PTr = [k.sb([128, 512], BF16, "ptr%d" % i) for i in range(3)]
        qtm = k.sb([128, 1024], F32, "qtm")
        qtb = k.sb([128, 1024], BF16, "qtb")
        rt = [k.sb([128, 512], F32, "rt%d" % i) for i in range(2)]
        sq8 = k.sb([128, 16], F32, "sq8")
        rec = k.sb([128, 512], F32, "rec")
        rec2 = k.sb([128, 512], F32, "rec2")
        ot1 = k.sb([128, 512], F32, "ot1")
        ot2 = k.sb([128, 512], F32, "ot2")
        osq = k.sb([128, 512], BF16, "osq")
        qg_row = k.sb([128, 64], F32, "qg_row")
        kg_row = k.sb([128, 64], F32, "kg_row")
        dma("sp", qg_row[:], qg_in.partition_broadcast(128), [uIN], [qg_row])
        dma("sp", kg_row[:], kg_in.partition_broadcast(128), [uIN], [kg_row])
        alloc_rows_attn()

    def head_rms(src_buf, c0, nh, g_row, dst_buf, d0):
        sv = src_buf[:, c0:c0 + nh * 64].rearrange("p (h c) -> p h c", h=nh)
        tv = rt[0][:, 0:nh * 64].rearrange("p (h c) -> p h c", h=nh)
        op("dve", lambda e: e.tensor_tensor(out=tv, in0=sv, in1=sv, op=ALU.mult), [src_buf], [rt[0]])
        op("dve", lambda e: e.tensor_reduce(out=sq8[:, 0:nh], in_=tv, axis=AX.X, op=ALU.add), [rt[0]], [sq8])
        op("dve", lambda e: e.tensor_scalar(out=sq8[:, 0:nh], in0=sq8[:, 0:nh], scalar1=1.0 / 64, scalar2=EPS,
                                            op0=ALU.mult, op1=ALU.add), [], [sq8])
        op("act", lambda e: e.activation(out=sq8[:, 0:nh], in_=sq8[:, 0:nh], func=AF.Sqrt), [sq8], [sq8])
        op("dve", lambda e: e.reciprocal(out=sq8[:, 0:nh], in_=sq8[:, 0:nh]), [sq8], [sq8])
        dv = dst_buf[:, d0:d0 + nh * 64].rearrange("p (h c) -> p h c", h=nh)
        op("dve", lambda e: e.tensor_tensor(out=dv, in0=sv, in1=bc(sq8[:, 0:nh].unsqueeze(2), [128, nh, 64]),
                                            op=ALU.mult), [src_buf, sq8], [dst_buf])
        op("dve", lambda e: e.tensor_tensor(out=dv, in0=dv, in1=bc(g_row[:, :].unsqueeze(1), [128, nh, 64]),
                                            op=ALU.mult), [g_row], [dst_buf])

    def tiles_of(b):
        return [(True, i) for i in range(NCX)] + [(False, i) for i in range(NT)]

    def src_tile(l, b, is_ctx, i):
        if l == 0:
            return (ctx_in[b, i * 128:(i + 1) * 128, :], uIN) if is_ctx else (x_in[b, i * 128:(i + 1) * 128, :], uIN)
        return (XCA[b, i * 128:(i + 1) * 128, :], uXCA) if is_ctx else (XSA[b, i * 128:(i + 1) * 128, :], uXSA)

    def dst_tile(l, b, is_ctx, i):
        if l == 0:
            return (XCA[b, i * 128:(i + 1) * 128, :], uXCA) if is_ctx else (XSA[b, i * 128:(i + 1) * 128, :], uXSA)
        return (None, None) if is_ctx else (XSB[b, i * 128:(i + 1) * 128, :], uXSB)

    def out_proj_store(l, b, is_ctx, tiles, first, nchunk):
        for si, ti in enumerate(tiles):
            for half in range(2):
                for c in range(nchunk):
                    op("pe", lambda e, c=c, half=half, si=si: e.matmul(
                        PB[4 + half][:, :], lhsT=mixT[:, c, si * 128:(si + 1) * 128],
                        rhs=Wo[:, c, half * 512:(half + 1) * 512], start=(c == 0), stop=(c == nchunk - 1)),
                       [mixT, Wo], [PB[4 + half]])
            dap, dunit = dst_tile(l, b, is_ctx, ti)
            base = basex
            if first:
                sap, sunit = src_tile(l, b, is_ctx, ti)
                dma("sp", base[:], sap, [sunit], [base])
            else:
                dma("sp", base[:], dap, [dunit], [base])
            g1 = rows["G1"][1 if is_ctx else 0]
            for half in range(2):
                op("dve", lambda e, half=half: e.tensor_tensor(out=ynew[:, half * 512:(half + 1) * 512],
                                                               in0=PB[4 + half][:, :],
                                                               in1=g1[:, half * 512:(half + 1) * 512], op=ALU.mult),
                   [PB[4 + half], g1], [ynew])
            op("dve", lambda e: e.tensor_tensor(out=ynew[:], in0=ynew[:], in1=base[:], op=ALU.add), [base], [ynew])
            dma("sp", dap, ynew[:], [ynew], [dunit])

    def supertiles(b, with_ctx):
        sts = []
        if with_ctx:
            sts.append((True, list(range(NCX))))
        for s0 in range(0, NT, 4):
            sts.append((False, list(range(s0, min(s0 + 4, NT)))))
        return sts

    def load_wo(src_rows, pk, nchunk):
        for c0 in range(0, D, CW):
            stg = wstage[cnt["ws"] % 2]
            cnt["ws"] += 1
            dma("sp", stg[0:pk, 0:nchunk, :], src_rows[:, c0:c0 + CW].rearrange("(c p) n -> p c n", p=pk),
                [uIN], [stg])
            op("pool", lambda e: e.tensor_copy(out=Wo[0:pk, 0:nchunk, c0:c0 + CW], in_=stg[0:pk, 0:nchunk, :]),
               [stg], [Wo])

    def gqa_pass(b, first):
      with k.scope():
        l = 0
        alloc_attn([64, 2, NU * 128], [128, NU, 256], 768, [64, 8, D], [64, 8, 512], [64, 8, 512])
        load_rows_attn(0, b, 0)
        load_rows_attn(0, NB, 1)
        load_w(Wp, 0, ev_w_in, 512, 256)
        load_w(Wp, 256, ev_w_in, 0, 512)
        load_wo(ev_w_out[0:512, :], 64, 8)
        vv = Vall[:, :, 0:256].rearrange("p u (h c) -> p u h c", h=2)
        op("pool", lambda e: e.memset(vv[:, :, :, 64:128], 1.0), [], [Vall])
        for u, (is_ctx, ti) in enumerate(tiles_of(b)):
            sap, sunit = src_tile(l, b, is_ctx, ti)
            xt, hT = tile_prep(sap, sunit, NB if is_ctx else b)
            proj(hT, Wp, 0, 256, PB[0])
            op("act", lambda e: e.activation(out=qtm[:, 0:256], in_=PB[0][:, 0:256], func=AF.Copy), [PB[0]], [qtm])
            head_rms(qtm, 0, 2, kg_row, qtm, 0)
            if not is_ctx:
                rope_apply((qtm, 256), (qtm, 0), 2, ti, rt)
                ksrc = 256
            else:
                ksrc = 0
            op("act", lambda e, ksrc=ksrc: e.activation(out=qtb[:, 0:128], in_=qtm[:, ksrc:ksrc + 128], func=AF.Copy),
               [qtm], [qtb])
            for h in range(2):
                op("pe", lambda e, h=h: e.transpose(out=PT2[0:64, h * 128:(h + 1) * 128],
                                                    in_=qtb[:, h * 64:(h + 1) * 64], identity=ident_b[:]),
                   [qtb, ident_b], [PT2])
            op("act", lambda e, u=u: e.activation(
                out=KTall[0:64, 0:2, u * 128:(u + 1) * 128],
                in_=PT2[0:64, 0:256].rearrange("p (h t) -> p h t", h=2), func=AF.Copy), [PT2], [KTall])
            op("act", lambda e, u=u: e.activation(
                out=Vall[:, u, 0:256].rearrange("p (h c) -> p h c", h=2)[:, :, 0:64],
                in_=qtm[:, 128:256].rearrange("p (h c) -> p h c", h=2), func=AF.Copy), [qtm], [Vall])
        for (is_ctx, tiles) in supertiles(b, True):
            N = 128 * len(tiles)
            for si, ti in enumerate(tiles):
                sap, sunit = src_tile(l, b, is_ctx, ti)
                xt, hT = tile_prep(sap, sunit, NB if is_ctx else b)
                proj(hT, Wp, 256, 512, PB[0])
                op("act", lambda e: e.activation(out=qtm[:, 0:512], in_=PB[0][:, :], func=AF.Copy), [PB[0]], [qtm])
                head_rms(qtm, 0, 8, qg_row, qtm, 0)
                if not is_ctx:
                    rope_apply((qtm, 512), (qtm, 0), 8, ti, rt)
                    qsrc = 512
                else:
                    qsrc = 0
                op("act", lambda e, qsrc=qsrc: e.activation(out=qtb[:, 0:512], in_=qtm[:, qsrc:qsrc + 512],
                                                           func=AF.Copy), [qtm], [qtb])
                for h in range(8):
                    op("pe", lambda e, h=h: e.transpose(out=PT2[0:64, h * 128:(h + 1) * 128],
                                                        in_=qtb[:, h * 64:(h + 1) * 64], identity=ident_b[:]),
                       [qtb, ident_b], [PT2])
                op("act", lambda e, si=si: e.activation(
                    out=QTst[0:64, :, si * 128:(si + 1) * 128],
                    in_=PT2[0:64, :].rearrange("p (h t) -> p h t", h=8), func=AF.Copy), [PT2], [QTst])
            keys = list(range(NCX)) if is_ctx else list(range(NU))
            for head in range(8):
                kvh = head // 4
                acc = PB[2 + head % 2]
                for ui, u in enumerate(keys):
                    sp_ = PB[ui % 2]
                    op("pe", lambda e, u=u, sp_=sp_: e.matmul(
                        sp_[:, 0:N], lhsT=KTall[0:64, kvh, u * 128:(u + 1) * 128], rhs=QTst[0:64, head, 0:N],
                        start=True, stop=True), [KTall, QTst], [sp_])
                    pt = PTr[cnt["ptr"] % 3]
                    cnt["ptr"] += 1
                    op("act", lambda e, sp_=sp_, pt=pt: e.activation(out=pt[:, 0:N], in_=sp_[:, 0:N], func=AF.Exp,
                                                                     scale=0.125), [sp_], [pt])
                    op("pe", lambda e, u=u, pt=pt, ui=ui: e.matmul(
                        acc[:, 0:N], lhsT=Vall[:, u, kvh * 128:(kvh + 1) * 128], rhs=pt[:, 0:N],
                        start=(ui == 0), stop=(ui == len(keys) - 1)), [Vall, pt], [acc])
                op("dve", lambda e: e.reciprocal(out=rec[64:128, 0:N], in_=acc[64:128, 0:N]), [acc], [rec])
                op("dve", lambda e: e.tensor_tensor(out=mixT[0:64, head, 0:N], in0=acc[0:64, 0:N],
                                                    in1=rec[64:128, 0:N], op=ALU.mult), [acc, rec], [mixT])
            out_proj_store(l, b, is_ctx, tiles, first, 8)

    ret_c = {}

    def ret_setup():
        lg = k.sb([128, 8], F32, "lg")
        dma("sp", lg[:], rate_in.partition_broadcast(128), [uIN], [lg])
        op("act", lambda e: e.activation(out=lg[:], in_=lg[:], func=AF.Exp), [lg], [lg])
        op("dve", lambda e: e.tensor_scalar(out=lg[:], in0=lg[:], scalar1=-1.0, scalar2=None, op0=ALU.mult), [], [lg])
        relm = k.sb([128, 128], F32, "relm")
        pidx = k.sb([128, 2], F32, "pidx")
        fidx = k.sb([128, 128], F32, "fidx")
        dma("sp", relm[:], relm_in, [uIN], [relm])
        dma("sp", pidx[:], pidx_in, [uIN], [pidx])
        dma("sp", fidx[:], fidx_in, [uIN], [fidx])
        relp = k.sb([128, 128], F32, "relp")
        reln = k.sb([128, 128], F32, "reln")
        mp = k.sb([128, 128], F32, "mp")
        mn = k.sb([128, 128], F32, "mn")
        op("dve", lambda e: e.tensor_scalar(out=relp[:], in0=relm[:], scalar1=0.0, scalar2=None, op0=ALU.max),
           [relm], [relp])
        op("dve", lambda e: e.tensor_scalar(out=reln[:], in0=relm[:], scalar1=-1.0, scalar2=0.0, op0=ALU.mult,
                                            op1=ALU.max), [relm], [reln])
        op("dve", lambda e: e.tensor_scalar(out=mp[:], in0=relm[:], scalar1=0.0, scalar2=None, op0=ALU.is_ge),
           [relm], [mp])
        op("dve", lambda e: e.tensor_scalar(out=mn[:], in0=relm[:], scalar1=0.0, scalar2=None, op0=ALU.is_le),
           [relm], [mn])
        DT = k.sb([128, 4, 128], F32, "DT")
        QD = k.sb([128, 4, 128], F32, "QD")
        KD = k.sb([128, 4, 2], F32, "KD")
        DEC = k.sb([128, 4], F32, "DEC")
        tmpm = k.sb([128, 128], F32, "tmpm")
        for h in range(4):
            lf, lb = lg[:, h:h + 1], lg[:, 4 + h:5 + h]
            op("act", lambda e, lf=lf: e.activation(out=tmpm[:], in_=relp[:], func=AF.Exp, scale=lf), [relp, lg], [tmpm])
            op("dve", lambda e, h=h: e.tensor_tensor(out=DT[:, h, :], in0=tmpm[:], in1=mp[:], op=ALU.mult),
               [tmpm, mp], [DT])
            op("act", lambda e, lb=lb: e.activation(out=tmpm[:], in_=reln[:], func=AF.Exp, scale=lb), [reln, lg], [tmpm])
            op("dve", lambda e: e.tensor_tensor(out=tmpm[:], in0=tmpm[:], in1=mn[:], op=ALU.mult), [mn], [tmpm])
            op("dve", lambda e, h=h: e.tensor_tensor(out=DT[:, h, :], in0=DT[:, h, :], in1=tmpm[:], op=ALU.add),
               [tmpm], [DT])
            op("act", lambda e, h=h, lf=lf: e.activation(out=QD[0:64, h, :], in_=fidx[0:64, :], func=AF.Exp,
                                                         scale=lg[0:64, h:h + 1]), [fidx, lg], [QD])
            op("act", lambda e, h=h: e.activation(out=QD[64:128, h, :], in_=fidx[64:128, :], func=AF.Exp,
                                                  scale=lg[64:128, 4 + h:5 + h]), [fidx, lg], [QD])
            op("act", lambda e, h=h, lf=lf: e.activation(out=KD[:, h, 0:1], in_=pidx[:, 0:1], func=AF.Exp, scale=lf),
               [pidx, lg], [KD])
            op("act", lambda e, h=h, lb=lb: e.activation(out=KD[:, h, 1:2], in_=pidx[:, 1:2], func=AF.Exp, scale=lb),
               [pidx, lg], [KD])
            op("act", lambda e, h=h: e.activation(out=DEC[0:64, h:h + 1], in_=lg[0:64, h:h + 1], func=AF.Exp,
                                                  scale=128.0), [lg], [DEC])
            op("act", lambda e, h=h: e.activation(out=DEC[64:128, h:h + 1], in_=lg[64:128, 4 + h:5 + h], func=AF.Exp,
                                                  scale=128.0), [lg], [DEC])
        ret_c.update(DT=DT, QD=QD, KD=KD, DEC=DEC)
        ret_c["kbd"] = k.sb([128, 4, 128], BF16, "kbd")
        ret_c["vb"] = k.sb([128, 512], BF16, "vbb")
        ret_c["kvs"] = k.sb([128, 4, 128], F32, "kvs")
        ret_c["S"] = k.sb([128, 4, 128], F32, "Sst")
        ret_c["Sb"] = k.sb([128, 4, 128], BF16, "Sbb")
        ret_c["Sl"] = k.sb([128, 4, 128], BF16, "Sl")
        ret_c["qdup"] = k.sb([128, 4, 128], BF16, "qdup")
        ret_c["qT"] = k.sb([64, 4, 128], BF16, "qTr")
        ret_c["qdT"] = k.sb([128, 4, 128], BF16, "qdT")
        ret_c["kT"] = k.sb([64, 4, 128], BF16, "kTr")
        ret_c["scm"] = k.sb([128, 4, 128], BF16, "scm")
        ret_c["gsl"] = k.sb([128, 512], F32, "gsl")
        ret_c["om"] = k.sb([128, 512], F32, "om")
        ret_c["omb"] = k.sb([128, 512], BF16, "omb")

    def ret_qkv(b, is_ctx, ti, need_q):
        l = 0
        rc = ret_c
        sap, sunit = src_tile(l, b, is_ctx, ti)
        xt, hT = tile_prep(sap, sunit, NB if is_ctx else b)
        proj(hT, Wp, 0, 512, PB[0])
        proj(hT, Wp, 512, 512, PB[1])
        op("act", lambda e: e.activation(out=qtm[:, 0:256], in_=PB[0][:, 0:256], func=AF.Copy), [PB[0]], [qtm])
        op("act", lambda e: e.activation(out=qtm[:, 256:512], in_=PB[0][:, 256:512], func=AF.Copy, scale=0.125),
           [PB[0]], [qtm])
        op("act", lambda e: e.activation(out=rc["vb"][:], in_=PB[1][:, :], func=AF.Copy), [PB[1]], [rc["vb"]])
        if not is_ctx:
            rope_apply((qtm, 512), (qtm, 0), 8, ti, rt)
            s0 = 512
        else:
            s0 = 0
        kv4 = qtm[:, s0 + 256:s0 + 512].rearrange("p (h c) -> p h c", h=4)
        op("dve", lambda e: e.tensor_copy(out=qtb[:, 0:256], in_=qtm[:, s0 + 256:s0 + 512]), [qtm], [qtb])
        for d_ in range(2):
            op("dve", lambda e, d_=d_: e.tensor_tensor(
                out=rc["kbd"][:, :, d_ * 64:(d_ + 1) * 64], in0=kv4,
                in1=bc(rc["KD"][:, :, d_:d_ + 1], [128, 4, 64]), op=ALU.mult), [qtm, rc["KD"]], [rc["kbd"]])
        for h in range(4):
            op("pe", lambda e, h=h: e.transpose(out=PT2[0:64, h * 128:(h + 1) * 128], in_=qtb[:, h * 64:(h + 1) * 64],
                                                identity=ident_b[:]), [qtb, ident_b], [PT2])
        op("act", lambda e: e.activation(out=rc["kT"][:], in_=PT2[0:64, 0:512].rearrange("p (h t) -> p h t", h=4),
                                         func=AF.Copy), [PT2], [rc["kT"]])
        if need_q:
            qv4 = qtm[:, s0:s0 + 256].rearrange("p (h c) -> p h c", h=4)
            for d_ in range(2):
                op("dve", lambda e, d_=d_: e.tensor_copy(out=rc["qdup"][:, :, d_ * 64:(d_ + 1) * 64], in_=qv4),
                   [qtm], [rc["qdup"]])
            for h in range(4):
                op("pe", lambda e, h=h: e.transpose(out=PT_[:, h * 128:(h + 1) * 128], in_=rc["qdup"][:, h, :],
                                                    identity=ident_b[:]), [rc["qdup"], ident_b], [PT_])
            ptv = PT_[:, 0:512].rearrange("p (h t) -> p h t", h=4)
            op("dve", lambda e: e.tensor_copy(out=rc["qT"][:], in_=PT_[0:64, 0:512].rearrange("p (h t) -> p h t", h=4)),
               [PT_], [rc["qT"]])
            op("dve", lambda e: e.tensor_tensor(out=rc["qdT"][:], in0=ptv, in1=rc["QD"][:], op=ALU.mult),
               [PT_, rc["QD"]], [rc["qdT"]])
            if stop_after == "ret_qa":
                return xt
            proj(hT, Wp, 1024, 512, PB[2])
            if stop_after == "ret_qb":
                return xt
            op("act", lambda e: e.activation(out=rc["gsl"][:], in_=PB[2][:, :], func=AF.Silu), [PB[2]], [rc["gsl"]])
        return xt

    def ret_pass(b, first):
      with k.scope():
        l = 0
        alloc_attn(None, None, 1536, [128, 4, D], [128, 1, 128], [128, 4, 512])
        ret_setup()
        rc = ret_c
        load_rows_attn(0, b, 0)
        load_rows_attn(0, NB, 1)
        load_w(Wp, 0, ev_w_in, 768, 256)
        load_w(Wp, 256, ev_w_in, 1024, 256)
        load_w(Wp, 512, ev_w_in, 1280, 512)
        load_w(Wp, 1024, ev_w_in, 1792, 512)
        load_wo(ev_w_out[512:1024, :], 128, 4)
        tl = tiles_of(b)
        if stop_after == "ret_setup":
            return
        for u, (is_ctx, ti) in enumerate(tl):
            ret_qkv(b, is_ctx, ti, False)
            for h in range(4):
                op("pe", lambda e, h=h: e.matmul(PB[3][:, h * 128:(h + 1) * 128], lhsT=rc["kbd"][:, h, :],
                                                 rhs=rc["vb"][:, h * 128:(h + 1) * 128], start=True, stop=True),
                   [rc["kbd"], rc["vb"]], [PB[3]])
            op("act", lambda e: e.activation(out=rc["kvs"][:], in_=PB[3][:, :].rearrange("p (h c) -> p h c", h=4),
                                             func=AF.Copy), [PB[3]], [rc["kvs"]])
            dma("sp", KVD[u], rc["kvs"][:], [rc["kvs"]], [uKVD])
        if stop_after == "ret_p1":
            return
        of = list(range(NU))
        ob = list(range(NCX - 1, -1, -1)) + list(range(NU - 1, NCX - 1, -1))
        S = rc["S"]
        op("dve", lambda e: e.memset(S[:], 0.0), [], [S])
        for t in range(NU):
            cf, cb = of[t], ob[t]
            op("act", lambda e: e.activation(out=rc["Sb"][:], in_=S[:], func=AF.Copy), [S], [rc["Sb"]])
            dma("sp", STD[cf, 0:64], rc["Sb"][0:64], [rc["Sb"]], [uSTD])
            dma("sp", STD[cb, 64:128], rc["Sb"][64:128], [rc["Sb"]], [uSTD])
            dma("sp", rc["kvs"][0:64], KVD[cf, 0:64], [uKVD], [rc["kvs"]])
            dma("sp", rc["kvs"][64:128], KVD[cb, 64:128], [uKVD], [rc["kvs"]])
            op("dve", lambda e: e.tensor_tensor(out=S[:], in0=S[:], in1=bc(rc["DEC"][:, :].unsqueeze(2), [128, 4, 128]),
                                                op=ALU.mult), [rc["DEC"]], [S])
            op("dve", lambda e: e.tensor_tensor(out=S[:], in0=S[:], in1=rc["kvs"][:], op=ALU.add), [rc["kvs"]], [S])
        if stop_after == "ret_scan":
            return
        for (is_ctx, tiles) in supertiles(b, True):
            for si, ti in enumerate(tiles):
                u = ti if is_ctx else NCX + ti
                xt = ret_qkv(b, is_ctx, ti, True)
                if stop_after in ("ret_q", "ret_qa", "ret_qb"):
                    return
                dma("sp", rc["Sl"][:], STD[u], [uSTD], [rc["Sl"]])
                for h in range(4):
                    op("pe", lambda e, h=h: e.matmul(PB[3][:, h * 128:(h + 1) * 128], lhsT=rc["kT"][:, h, :],
                                                     rhs=rc["qT"][:, h, :], start=True, stop=True),
                       [rc["kT"], rc["qT"]], [PB[3]])
                op("dve", lambda e: e.tensor_tensor(out=rc["scm"][:],
                                                    in0=PB[3][:, :].rearrange("p (h c) -> p h c", h=4),
                                                    in1=rc["DT"][:], op=ALU.mult), [PB[3], rc["DT"]], [rc["scm"]])
                for h in range(4):
                    op("pe", lambda e, h=h: e.matmul(PB[2][:, h * 128:(h + 1) * 128], lhsT=rc["scm"][:, h, :],
                                                     rhs=rc["vb"][:, h * 128:(h + 1) * 128], start=True, stop=False),
                       [rc["scm"], rc["vb"]], [PB[2]])
                    op("pe", lambda e, h=h: e.matmul(PB[2][:, h * 128:(h + 1) * 128], lhsT=rc["qdT"][:, h, :],
                                                     rhs=rc["Sl"][:, h, :], start=False, stop=True),
                       [rc["qdT"], rc["Sl"]], [PB[2]])
                if stop_after == "ret_o":
                    return
                om = rc["om"]
                op("act", lambda e: e.activation(out=om[:], in_=PB[2][:, :], func=AF.Copy), [PB[2]], [om])
                ov = om[:, :].rearrange("p (h c) -> p h c", h=4)
                tv = rt[0][:, 0:512].rearrange("p (h c) -> p h c", h=4)
                op("dve", lambda e: e.tensor_tensor(out=tv, in0=ov, in1=ov, op=ALU.mult), [om], [rt[0]])
                op("dve", lambda e: e.tensor_reduce(out=sq8[:, 0:4], in_=tv, axis=AX.X, op=ALU.add), [rt[0]], [sq8])
                op("dve", lambda e: e.tensor_scalar(out=sq8[:, 0:4], in0=sq8[:, 0:4], scalar1=1.0 / 128, scalar2=EPS,
                                                    op0=ALU.mult, op1=ALU.add), [], [sq8])
                op("act", lambda e: e.activation(out=sq8[:, 0:4], in_=sq8[:, 0:4], func=AF.Sqrt), [sq8], [sq8])
                op("dve", lambda e: e.reciprocal(out=sq8[:, 0:4], in_=sq8[:, 0:4]), [sq8], [sq8])
                op("dve", lambda e: e.tensor_tensor(out=ov, in0=ov, in1=bc(sq8[:, 0:4].unsqueeze(2), [128, 4, 128]),
                                                    op=ALU.mult), [sq8], [om])
                op("dve", lambda e: e.tensor_tensor(out=rc["omb"][:], in0=om[:], in1=rc["gsl"][:], op=ALU.mult),
                   [om, rc["gsl"]], [rc["omb"]])
                for h in range(4):
                    op("pe", lambda e, h=h: e.transpose(out=PT2[:, h * 128:(h + 1) * 128],
                                                        in_=rc["omb"][:, h * 128:(h + 1) * 128], identity=ident_b[:]),
                       [rc["omb"], ident_b], [PT2])
                op("act", lambda e, si=si: e.activation(out=mixT[:, :, si * 128:(si + 1) * 128],
                                                        in_=PT2[:, 0:512].rearrange("p (h t) -> p h t", h=4),
                                                        func=AF.Copy), [PT2], [mixT])
            if stop_after == "ret_m":
                return
            out_proj_store(l, b, is_ctx, tiles, first, 4)

    diff_c = {}

    def diff_setup():
        lam_sb = k.sb([128, 256], F32, "lam_sb")
        dma("sp", lam_sb[:], lam_in.partition_broadcast(128), [uIN], [lam_sb])
        l2 = k.sb([128, 2], F32, "l2")
        lv = lam_sb[:, :].rearrange("p (a b c) -> p a b c", a=2, b=2)
        tv = rt[0][:, 0:128].rearrange("p (a c) -> p a c", a=2)
        op("dve", lambda e: e.tensor_tensor(out=tv, in0=lv[:, :, 0, :], in1=lv[:, :, 1, :], op=ALU.mult),
           [lam_sb], [rt[0]])
        op("dve", lambda e: e.tensor_reduce(out=l2[:], in_=tv, axis=AX.X, op=ALU.add), [rt[0]], [l2])
        op("act", lambda e: e.activation(out=l2[:], in_=l2[:], func=AF.Exp), [l2], [l2])
        lam_init = 0.8 - 0.6 * math.exp(-0.3 * 1)
        nl = k.sb([128, 1], F32, "neglam")
        op("dve", lambda e: e.tensor_tensor(out=nl[:], in0=l2[:, 1:2], in1=l2[:, 0:1], op=ALU.subtract), [l2], [nl])
        op("dve", lambda e: e.tensor_scalar(out=nl[:], in0=nl[:], scalar1=-lam_init, scalar2=None, op0=ALU.add),
           [], [nl])
        sg = k.sb([128, 1], F32, "sublng")
        dma("sp", sg[:], subln_in.rearrange("(p o) -> p o", o=1), [uIN], [sg])
        op("dve", lambda e: e.tensor_scalar(out=sg[:], in0=sg[:], scalar1=1.0 - lam_init, scalar2=None, op0=ALU.mult),
           [], [sg])
        diff_c.update(nl=nl, sg=sg)

    def diff_pass(b, grp, first):
      with k.scope():
        l = 1
        alloc_attn([128, 4, NU * 128], [128, NU, 512], 1536, [128, 4, D], [128, 4, 512], [128, 4, 512])
        diff_setup()
        nl, sg = diff_c["nl"], diff_c["sg"]
        load_rows_attn(1, b, 0)
        load_rows_attn(1, NB, 1)
        load_w(Wp, 0, od_w_in, 1024 + grp * 512, 512)
        load_w(Wp, 512, od_w_in, 2048 + grp * 512, 512)
        load_w(Wp, 1024, od_w_in, grp * 512, 512)
        load_wo(od_w_out[grp * 512:(grp + 1) * 512, :], 128, 4)
        for u, (is_ctx, ti) in enumerate(tiles_of(b)):
            sap, sunit = src_tile(l, b, is_ctx, ti)
            xt, hT = tile_prep(sap, sunit, NB if is_ctx else b)
            proj(hT, Wp, 0, 512, PB[0])
            proj(hT, Wp, 512, 512, PB[1])
            op("act", lambda e: e.activation(out=qtm[:, 0:512], in_=PB[0][:, :], func=AF.Copy), [PB[0]], [qtm])
            op("act", lambda e, u=u: e.activation(out=Vall[:, u, :], in_=PB[1][:, :], func=AF.Copy), [PB[1]], [Vall])
            if not is_ctx:
                rope_apply((qtm, 512), (qtm, 0), 8, ti, rt)
                s0 = 512
            else:
                s0 = 0
            op("act", lambda e, s0=s0: e.activation(out=qtb[:, 0:512], in_=qtm[:, s0:s0 + 512], func=AF.Copy),
               [qtm], [qtb])
            for h in range(4):
                op("pe", lambda e, h=h: e.transpose(out=PT2[:, h * 128:(h + 1) * 128],
                                                    in_=qtb[:, h * 128:(h + 1) * 128], identity=ident_b[:]),
                   [qtb, ident_b], [PT2])
            op("act", lambda e, u=u: e.activation(out=KTall[:, :, u * 128:(u + 1) * 128],
                                                  in_=PT2[:, 0:512].rearrange("p (h t) -> p h t", h=4), func=AF.Copy),
               [PT2], [KTall])
        for (is_ctx, tiles) in supertiles(b, False):
            N = 128 * len(tiles)
            for si, ti in enumerate(tiles):
                sap, sunit = src_tile(l, b, is_ctx, ti)
                xt, hT = tile_prep(sap, sunit, b)
                proj(hT, Wp, 1024, 512, PB[0])
                op("act", lambda e: e.activation(out=qtm[:, 0:512], in_=PB[0][:, :], func=AF.Copy), [PB[0]], [qtm])
                rope_apply((qtm, 512), (qtm, 0), 8, ti, rt)
                op("act", lambda e: e.activation(out=qtb[:, 0:512], in_=qtm[:, 512:1024], func=AF.Copy), [qtm], [qtb])
                for h in range(4):
                    op("pe", lambda e, h=h: e.transpose(out=PT2[:, h * 128:(h + 1) * 128],
                                                        in_=qtb[:, h * 128:(h + 1) * 128], identity=ident_b[:]),
                       [qtb, ident_b], [PT2])
                op("act", lambda e, si=si: e.activation(out=QTst[:, 0:4, si * 128:(si + 1) * 128],
                                                        in_=PT2[:, 0:512].rearrange("p (h t) -> p h t", h=4),
                                                        func=AF.Copy), [PT2], [QTst])
            for h in range(4):
                for c in range(2):
                    accO, accD = (PB[2], PB[3]) if c == 0 else (PB[4], PB[5])
                    for u in range(NU):
                        sp_ = PB[u % 2]
                        op("pe", lambda e, u=u, sp_=sp_, c=c: e.matmul(
                            sp_[:, 0:N], lhsT=KTall[c * 64:(c + 1) * 64, h, u * 128:(u + 1) * 128],
                            rhs=QTst[c * 64:(c + 1) * 64, h, 0:N], start=True, stop=True), [KTall, QTst], [sp_])
                        pt = PTr[cnt["ptr"] % 3]
                        cnt["ptr"] += 1
                        op("act", lambda e, sp_=sp_, pt=pt: e.activation(out=pt[:, 0:N], in_=sp_[:, 0:N], func=AF.Exp,
                                                                         scale=0.125), [sp_], [pt])
                        op("pe", lambda e, u=u, pt=pt: e.matmul(
                            accO[:, 0:N], lhsT=Vall[:, u, h * 128:(h + 1) * 128], rhs=pt[:, 0:N],
                            start=(u == 0), stop=(u == NU - 1)), [Vall, pt], [accO])
                        op("pe", lambda e, u=u, pt=pt: e.matmul(
                            accD[:, 0:N], lhsT=ones_b[:], rhs=pt[:, 0:N],
                            start=(u == 0), stop=(u == NU - 1)), [ones_b, pt], [accD])
                op("dve", lambda e: e.reciprocal(out=rec[:, 0:N], in_=PB[3][:, 0:N]), [PB[3]], [rec])
                op("dve", lambda e: e.reciprocal(out=rec2[:, 0:N], in_=PB[5][:, 0:N]), [PB[5]], [rec2])
                op("dve", lambda e: e.tensor_tensor(out=ot1[:, 0:N], in0=PB[2][:, 0:N], in1=rec[:, 0:N], op=ALU.mult),
                   [PB[2], rec], [ot1])
                op("dve", lambda e: e.tensor_tensor(out=ot2[:, 0:N], in0=PB[4][:, 0:N], in1=rec2[:, 0:N], op=ALU.mult),
                   [PB[4], rec2], [ot2])
                op("dve", lambda e: e.scalar_tensor_tensor(out=ot1[:, 0:N], in0=ot2[:, 0:N], scalar=nl[:, 0:1],
                                                           in1=ot1[:, 0:N], op0=ALU.mult, op1=ALU.add),
                   [ot2, nl], [ot1])
                op("act", lambda e: e.activation(out=osq[:, 0:N], in_=ot1[:, 0:N], func=AF.Square), [ot1], [osq])
                op("pe", lambda e: e.matmul(PB[0][:, 0:N], lhsT=ones_b[:], rhs=osq[:, 0:N], start=True, stop=True),
                   [ones_b, osq], [PB[0]])
                op("dve", lambda e: e.tensor_scalar(out=rec[:, 0:N], in0=PB[0][:, 0:N], scalar1=1.0 / 128, scalar2=EPS,
                                                    op0=ALU.mult, op1=ALU.add), [PB[0]], [rec])
                op("act", lambda e: e.activation(out=rec[:, 0:N], in_=rec[:, 0:N], func=AF.Sqrt), [rec], [rec])
                op("dve", lambda e: e.reciprocal(out=rec[:, 0:N], in_=rec[:, 0:N]), [rec], [rec])
                op("dve", lambda e, h=h: e.scalar_tensor_tensor(out=mixT[:, h, 0:N], in0=ot1[:, 0:N], scalar=sg[:, 0:1],
                                                                in1=rec[:, 0:N], op0=ALU.mult, op1=ALU.mult),
                   [ot1, sg, rec], [mixT])
            out_proj_store(l, b, is_ctx, tiles, first, 4)

    pc = {}

    def peer_setup():
        nonlocal Wp
        Wp = k.sb([128, KT, 2048], BF16, "Wq")
        alloc_rows_peer()
        pc["keys"] = k.sb([128, 16, 128], BF16, "pkeys")
        scr = k.sb([128, 2048], F32, "pscr")
        pc["scr"] = scr
        pc["h2"] = k.sb([128, D], F32, "h2")
        pc["h2b"] = k.sb([128, D], BF16, "h2b")
        pc["h2T"] = k.sb([128, KT, 128], BF16, "h2T")
        pc["qT"] = k.sb([128, 16, 128], BF16, "pqT")
        pc["sc"] = k.sb([128, 16, 128], F32, "psc")
        pc["vals"] = k.sb([128, 16, 16], F32, "pvals")
        pc["idx"] = k.sb([128, 16, 16], U32, "pidx_")
        pc["idxf"] = k.sb([128, 16, 16], F32, "pidxf")
        pc["cand"] = k.sb([128, 8, 256], F32, "pcand")
        pc["best"] = k.sb([128, 8, 16], F32, "pbest")
        pc["pos"] = k.sb([128, 8, 16], U32, "ppos")
        pc["pa"] = k.sb([128, 8, 16], U32, "ppa")
        pc["pb"] = k.sb([128, 8, 16], U32, "ppb")
        pc["paf"] = k.sb([128, 8, 16], F32, "ppaf")
        pc["pbf"] = k.sb([128, 8, 16], F32, "ppbf")
        pc["isel"] = k.sb([128, 8, 16], F32, "pisel")
        pc["jsel"] = k.sb([128, 8, 16], F32, "pjsel")
        pc["eid"] = k.sb([128, 128], U32, "peid")
        pc["gate"] = k.sb([128, 8, 16], F32, "pgate")
        pc["z"] = k.sb([128, 8], F32, "pz")
        pc["dots"] = k.sb([128, 128], F32, "pdots")
        pc["wgt"] = k.sb([128, 128], F32, "pwgt")
        pc["acc"] = k.sb([128, D], F32, "pacc")
        pc["gb"] = [k.sb([128, 2 * D], BF16, "pgb%d" % i) for i in range(8)]
        pc["dring"] = [k.sb([128, 1], F32, "pdr%d" % i) for i in range(4)]
        pc["wring"] = [k.sb([128, 1], F32, "pwr%d" % i) for i in range(4)]
        pc["dring2"] = [k.sb([128, 1], F32, "pdr2%d" % i) for i in range(4)]
        pc["stg"] = [k.sb([128, D], F32, "pstg%d" % i) for i in range(2)]
        pc["junkb"] = k.sb([128, D], BF16, "pjunkb")
        pc["dg"] = [k.sb([128, 128], BF16, "pdg%d" % i) for i in range(3)]
        pc["fg"] = k.sb([128, D], F32, "pfg")
        dma("sp", pc["fg"][:], final_g.partition_broadcast(128), [uIN], [pc["fg"]])
        pc["gi"] = 0

    def peer_load(l):
        ci = 0
        for (src, c0) in ((peer_u[l], 0), (peer_v[l], D)):
            for r0 in range(0, 16384, 128):
                st_ = pc["stg"][ci % 2]
                gbf = pc["gb"][ci % 8]
                ci += 1
                dma("sp", st_[:], src[r0:r0 + 128, :], [uIN], [st_])
                op("act", lambda e: e.activation(out=gbf[:, 0:D], in_=st_[:], func=AF.Copy), [st_], [gbf])
                dma("act", UVB[l][r0:r0 + 128, c0:c0 + D], gbf[:, 0:D], [gbf], [uTAB])
        load_w(Wp, 0, peer_wq[l], 0, 2048)
        sv = pc["scr"][:, :].rearrange("p (j n) -> p j n", j=16)
        dma("sp", sv, keysT[l], [uIN], [pc["scr"]])
        op("pool", lambda e: e.tensor_copy(out=pc["keys"][:], in_=sv), [pc["scr"]], [pc["keys"]])

    def top16(src, src2, vals_ap, idx_ap):
        (sbuf_, sap), (s2buf, s2ap) = src, src2
        (vb, vf), (ib, if_) = vals_ap, idx_ap
        op("dve", lambda e: e.max(out=vf(0, 8), in_=sap), [sbuf_], [vb])
        op("dve", lambda e: e.max_index(out=if_(0, 8), in_max=vf(0, 8), in_values=sap), [vb, sbuf_], [ib])
        op("dve", lambda e: e.match_replace(out=s2ap, in_to_replace=vf(0, 8), in_values=sap, imm_value=NEG),
           [vb, sbuf_], [s2buf])
        op("dve", lambda e: e.max(out=vf(8, 16), in_=s2ap), [s2buf], [vb])
        op("dve", lambda e: e.max_index(out=if_(8, 16), in_max=vf(8, 16), in_values=s2ap), [vb, s2buf], [ib])

    def peer_tile(l, b, is_ctx, ti, final):
        p = pc
        slot = 1 if is_ctx else 0
        scr_j = p["scr"][:, :].rearrange("p (j n) -> p j n", j=16)
        scr_h = p["scr"][:, :].rearrange("p (h c) -> p h c", h=8)
        if l == 0:
            sap, sunit = dst_tile(0, b, is_ctx, ti)
        else:
            sap, sunit = dst_tile(1, b, is_ctx, ti)
        xt = xt_ring[cnt["xt"] % 2]
        cnt["xt"] += 1
        dma("sp", xt[:], sap, [sunit], [xt])
        rstd_of(xt, 1)
        h2, h2b, h2T = p["h2"], p["h2b"], p["h2T"]
        op("dve", lambda e: e.scalar_tensor_tensor(out=h2[:], in0=xt[:], scalar=st4[:, 1:2], in1=rows["G2"][slot][:],
                                                   op0=ALU.mult, op1=ALU.mult), [xt, st4, rows["G2"][slot]], [h2])
        op("dve", lambda e: e.tensor_tensor(out=h2[:], in0=h2[:], in1=rows["SH2"][slot][:], op=ALU.add),
           [rows["SH2"][slot]], [h2])
        op("act", lambda e: e.activation(out=h2b[:], in_=h2[:], func=AF.Copy), [h2], [h2b])
        for kk in range(KT):
            op("pe", lambda e, kk=kk: e.transpose(out=PT_[:, kk * 128:(kk + 1) * 128],
                                                  in_=h2b[:, kk * 128:(kk + 1) * 128], identity=ident_b[:]),
               [h2b, ident_b], [PT_])
        op("act", lambda e: e.activation(out=h2T[:], in_=PT_[:, :].rearrange("p (k t) -> p k t", k=KT), func=AF.Copy),
           [PT_], [h2T])
        for j in range(16):
            pbk = PB[j // 4]
            for kk in range(KT):
                op("pe", lambda e, j=j, kk=kk, pbk=pbk: e.matmul(
                    pbk[:, (j % 4) * 128:(j % 4 + 1) * 128], lhsT=Wp[:, kk, j * 128:(j + 1) * 128], rhs=h2T[:, kk, :],
                    start=(kk == 0), stop=(kk == KT - 1)), [Wp, h2T], [pbk])
            if j % 4 == 3:
                g = j // 4
                op("act", lambda e, g=g, pbk=pbk: e.activation(
                    out=p["qT"][:, g * 4:(g + 1) * 4, :], in_=pbk[:, :].rearrange("p (j t) -> p j t", j=4),
                    func=AF.Copy), [pbk], [p["qT"]])
        for j in range(16):
            pbk = PB[j // 4]
            op("pe", lambda e, j=j, pbk=pbk: e.matmul(pbk[:, (j % 4) * 128:(j % 4 + 1) * 128], lhsT=p["qT"][:, j, :],
                                                      rhs=p["keys"][:, j, :], start=True, stop=True),
               [p["qT"], p["keys"]], [pbk])
            if j % 4 == 3:
                g = j // 4
                op("act", lambda e, g=g, pbk=pbk: e.activation(
                    out=p["sc"][:, g * 4:(g + 1) * 4, :], in_=pbk[:, :].rearrange("p (j n) -> p j n", j=4),
                    func=AF.Copy), [pbk], [p["sc"]])
        for j in range(16):
            top16((p["sc"], p["sc"][:, j, :]), (p["scr"], scr_j[:, j, :]),
                  (p["vals"], lambda a, c, j=j: p["vals"][:, j, a:c]), (p["idx"], lambda a, c, j=j: p["idx"][:, j, a:c]))
        op("dve", lambda e: e.tensor_copy(out=p["idxf"][:], in_=p["idx"][:]), [p["idx"]], [p["idxf"]])
        v4 = p["vals"][:, :, :].rearrange("p (h two) k -> p h two k", two=2)
        i4 = p["idxf"][:, :, :].rearrange("p (h two) k -> p h two k", two=2)
        c4 = p["cand"][:, :, :].rearrange("p h (a c) -> p h a c", a=16)
        op("dve", lambda e: e.tensor_tensor(out=c4, in0=bc(v4[:, :, 0, :].unsqueeze(3), [128, 8, 16, 16]),
                                            in1=bc(v4[:, :, 1, :].unsqueeze(2), [128, 8, 16, 16]), op=ALU.add),
           [p["vals"]], [p["cand"]])
        for h in range(8):
            top16((p["cand"], p["cand"][:, h, :]), (p["scr"], scr_h[:, h, :]),
                  (p["best"], lambda a, c, h=h: p["best"][:, h, a:c]), (p["pos"], lambda a, c, h=h: p["pos"][:, h, a:c]))
        op("dve", lambda e: e.tensor_single_scalar(out=p["pa"][:], in_=p["pos"][:], scalar=4,
                                                   op=ALU.logical_shift_right), [p["pos"]], [p["pa"]])
        op("dve", lambda e: e.tensor_single_scalar(out=p["pb"][:], in_=p["pos"][:], scalar=15, op=ALU.bitwise_and),
           [p["pos"]], [p["pb"]])
        op("dve", lambda e: e.tensor_copy(out=p["paf"][:], in_=p["pa"][:]), [p["pa"]], [p["paf"]])
        op("dve", lambda e: e.tensor_copy(out=p["pbf"][:], in_=p["pb"][:]), [p["pb"]], [p["pbf"]])
        e4 = p["scr"][:, :].rearrange("p (h k a) -> p h k a", h=8, k=16)
        io4 = bc(iota16[:, :].unsqueeze(1).unsqueeze(1), [128, 8, 16, 16])
        for (pf, half, dstb) in ((p["paf"], 0, p["isel"]), (p["pbf"], 1, p["jsel"])):
            op("dve", lambda e, pf=pf: e.tensor_tensor(out=e4, in0=bc(pf[:, :, :].unsqueeze(3), [128, 8, 16, 16]),
                                                       in1=io4, op=ALU.is_equal), [pf, iota16], [p["scr"]])
            op("dve", lambda e, half=half: e.tensor_tensor(out=e4, in0=e4,
                                                           in1=bc(i4[:, :, half, :].unsqueeze(2), [128, 8, 16, 16]),
                                                           op=ALU.mult), [p["idxf"]], [p["scr"]])
            op("dve", lambda e, dstb=dstb: e.tensor_reduce(out=dstb[:], in_=e4, axis=AX.X, op=ALU.add),
               [p["scr"]], [dstb])
        op("dve", lambda e: e.scalar_tensor_tensor(out=p["isel"][:], in0=p["isel"][:], scalar=128.0, in1=p["jsel"][:],
                                                   op0=ALU.mult, op1=ALU.add), [p["jsel"]], [p["isel"]])
        op("dve", lambda e: e.tensor_copy(out=p["eid"][:, :].rearrange("p (h k) -> p h k", h=8), in_=p["isel"][:]),
           [p["isel"]], [p["eid"]])
        op("dve", lambda e: e.tensor_tensor(out=p["gate"][:], in0=p["best"][:],
                                            in1=bc(p["best"][:, :, 0:1], [128, 8, 16]), op=ALU.subtract),
           [p["best"]], [p["gate"]])
        op("act", lambda e: e.activation(out=p["gate"][:], in_=p["gate"][:], func=AF.Exp), [p["gate"]], [p["gate"]])
        op("dve", lambda e: e.tensor_reduce(out=p["z"][:], in_=p["gate"][:], axis=AX.X, op=ALU.add),
           [p["gate"]], [p["z"]])
        op("dve", lambda e: e.reciprocal(out=p["z"][:], in_=p["z"][:]), [p["z"]], [p["z"]])
        op("dve", lambda e: e.tensor_tensor(out=p["gate"][:], in0=p["gate"][:],
                                            in1=bc(p["z"][:, :].unsqueeze(2), [128, 8, 16]), op=ALU.mult),
           [p["z"]], [p["gate"]])
        acc = p["acc"]
        gflat = p["gate"][:, :, :].rearrange("p h k -> p (h k)")
        pend = None

        def finish_slot(ps):
            s_, gb_, wr_ = ps
            dg = p["dg"][s_ % 3]
            op("dve", lambda e: e.tensor_scalar(out=dg[:], in0=ident_f[:], scalar1=wr_[:, 0:1],
                                                scalar2=gflat[:, s_:s_ + 1], op0=ALU.mult, op1=ALU.mult),
               [ident_f, wr_, p["gate"]], [dg])
            for half in range(2):
                op("pe", lambda e, half=half: e.matmul(
                    PB[4 + half][:, :], lhsT=dg[:], rhs=gb_[:, D + half * 512:D + (half + 1) * 512],
                    start=(s_ == 0), stop=(s_ == 127)), [dg, gb_], [PB[4 + half]])

        for s in range(128):
            gbuf = p["gb"][p["gi"] % 8]
            dr = p["dring"][p["gi"] % 4]
            wr = p["wring"][p["gi"] % 4]
            p["gi"] += 1
            dma("pool", gbuf[:], UVB[l], [uTAB, p["eid"]], [gbuf], indirect=p["eid"][:, s:s + 1])
            op("dve", lambda e: e.scalar_tensor_tensor(
                out=p["junkb"][:], in0=gbuf[:, 0:D], scalar=1.0, in1=h2b[:], op0=ALU.mult, op1=ALU.mult,
                accum_out=dr[:, 0:1]), [gbuf, h2b], [p["junkb"], dr])
            dr2 = p["dring2"][(p["gi"] - 1) % 4]
            op("dve", lambda e: e.tensor_copy(out=dr2[:, 0:1], in_=dr[:, 0:1]), [dr], [dr2])
            op("act", lambda e: e.activation(out=wr[:, 0:1], in_=dr2[:, 0:1], func=AF.Gelu), [dr2], [wr])
            if pend is not None:
                finish_slot(pend)
            pend = (s, gbuf, wr)
        finish_slot(pend)
        for half in range(2):
            op("dve", lambda e, half=half: e.tensor_tensor(out=acc[:, half * 512:(half + 1) * 512],
                                                           in0=PB[4 + half][:, :],
                                                           in1=rows["g2"][slot][:, half * 512:(half + 1) * 512],
                                                           op=ALU.mult), [PB[4 + half], rows["g2"][slot]], [acc])
        op("dve", lambda e: e.tensor_tensor(out=ynew[:], in0=acc[:], in1=xt[:], op=ALU.add), [acc, xt], [ynew])
        if not final:
            dma("sp", sap, ynew[:], [ynew], [sunit])
        else:
            rstd_of(ynew, 2)
            op("dve", lambda e: e.scalar_tensor_tensor(out=ynew[:], in0=ynew[:], scalar=st4[:, 2:3], in1=p["fg"][:],
                                                       op0=ALU.mult, op1=ALU.mult), [st4, p["fg"]], [ynew])
            dma("sp", out_d[b, ti * 128:(ti + 1) * 128, :], ynew[:], [ynew], [uOUT])


    def dump(srcu, src, is_final=False):
        for b in range(NB):
            for ti in range(NT):
                xt = xt_ring[cnt["xt"] % 2]
                cnt["xt"] += 1
                dma("sp", xt[:], src[b, ti * 128:(ti + 1) * 128, :], [srcu], [xt])
                dma("sp", out_d[b, ti * 128:(ti + 1) * 128, :], xt[:], [xt], [uOUT])

    done = False
    if stop_after == "const":
        dump(uIN, x_in)
        done = True
    if not done:
        mod_phase(0)
        if stop_after == "mod":
            dump(uIN, x_in)
            done = True
    if not done:
        for b in range(NB):
            if stop_after != "ret0":
                gqa_pass(b, True)
            if stop_after != "gqa0":
                ret_pass(b, stop_after == "ret0")
        if stop_after in ("mix0", "ret0", "gqa0", "ret_setup", "ret_p1", "ret_scan", "ret_q", "ret_o", "ret_m", "ret_qa", "ret_qb"):
            dump(uXSA, XSA)
            done = True
    if not done:
        with k.scope():
            peer_setup()
            peer_load(0)
            for b in range(NB):
                load_rows_peer(0, b, 0)
                load_rows_peer(0, NB, 1)
                for (is_ctx, ti) in tiles_of(b):
                    peer_tile(0, b, is_ctx, ti, False)
        if stop_after == "peer0":
            dump(uXSA, XSA)
            done = True
    if not done:
        mod_phase(1)
        for b in range(NB):
            diff_pass(b, 0, True)
            diff_pass(b, 1, False)
        if stop_after == "mix1":
            dump(uXSB, XSB)
            done = True
    if not done:
        with k.scope():
            peer_setup()
            peer_load(1)
            for b in range(NB):
                load_rows_peer(1, b, 0)
                for ti in range(NT):
                    peer_tile(1, b, False, ti, True)
    k.finish([uOUT])
    es.close()
    return nc


def host_consts(S):
    NT = S // 128
    GRID_W = 64
    t = np.arange(S)
    row = (t // GRID_W).astype(np.float32)
    col = (t % GRID_W).astype(np.float32)
    inv = (10000.0 ** (-np.arange(0, 32, 2, dtype=np.float32) / 32.0)).astype(np.float32)
    ar = row[:, None] * inv[None, :]
    ac = col[:, None] * inv[None, :]
    tab = np.concatenate([np.cos(ar), np.sin(ar), np.cos(ac), np.sin(ac)], axis=1).astype(np.float32)
    rope = np.ascontiguousarray(tab.reshape(NT, 128, 64).transpose(1, 0, 2))
    ident = np.eye(128, dtype=np.float32)
    iota16 = np.broadcast_to(np.arange(16, dtype=np.float32)[None, :], (128, 16)).copy()
    j = np.arange(128, dtype=np.float32)
    relm = (j[None, :] - j[:, None]).astype(np.float32)
    pidx = np.stack([127.0 - j, j], axis=1).astype(np.float32)
    fidx = np.concatenate([np.broadcast_to((j + 1.0)[None, :], (64, 128)),
                           np.broadcast_to((128.0 - j)[None, :], (64, 128))], axis=0).astype(np.float32).copy()
    return dict(rope=rope, ident=ident, iota16=iota16, relm=relm, pidx=pidx, fidx=fidx)


def make_in_maps(inp, NB, n_cores):
    f = lambda a: np.ascontiguousarray(np.asarray(a, dtype=np.float32))
    x, c, ctx, c_ctx = f(inp["x"]), f(inp["c"]), f(inp["ctx"]), f(inp["c_ctx"])
    S = x.shape[1]
    shared = dict(
        ada_w=f(inp["ada_w"]), ada_b=f(inp["ada_b"]),
        ada_bT=np.ascontiguousarray(f(inp["ada_b"]).reshape(2, 48, 128).transpose(0, 2, 1)),
        n1gT=np.ascontiguousarray(f(inp["norm1_g"]).reshape(2, KT, 128).transpose(0, 2, 1)),
        n2g=f(inp["norm2_g"]), final_g=f(inp["final_g"]),
        ev_w_in=f(inp["ev_w_in"])[0], ev_w_out=f(inp["ev_w_out"])[0],
        od_w_in=f(inp["od_w_in"])[0], od_w_out=f(inp["od_w_out"])[0],
        qg=f(inp["gqa_q_norm_g"])[0], kg=f(inp["gqa_k_norm_g"])[0],
        rate=f(inp["ret_log_rate"]).reshape(8), lam=f(inp["diff_lambda"]).reshape(256),
        subln=f(inp["diff_subln_g"]).reshape(128),
        peer_wq=f(inp["peer_w_q"]),
        keysT=np.ascontiguousarray(f(inp["peer_keys"]).reshape(2, 16, 128, 128).transpose(0, 3, 1, 2)),
        peer_u0=f(inp["peer_u"][0]), peer_u1=f(inp["peer_u"][1]),
        peer_v0=f(inp["peer_v"][0]), peer_v1=f(inp["peer_v"][1]),
    )
    shared.update(host_consts(S))
    maps = []
    for i in range(n_cores):
        bs = slice(i * NB, (i + 1) * NB)
        cv = np.concatenate([c[bs], c_ctx[None, :]], axis=0)
        cT = np.ascontiguousarray(cv.reshape(NB + 1, KT, 128).transpose(2, 1, 0))
        m = dict(shared)
        m.update(x=np.ascontiguousarray(x[bs]), ctx=np.ascontiguousarray(ctx[bs]), cT=cT)
        maps.append(m)
    return maps


def kernel(**inputs):
    n_cores = 8
    B, S, _ = inputs["x"].shape
    LC = inputs["ctx"].shape[1]
    NB = B // n_cores
    nc = build(NB, S, LC)
    maps = make_in_maps(inputs, NB, n_cores)
    res = run_bass_kernel_spmd(nc, maps, core_ids=list(range(n_cores)))
    return np.concatenate([r["out"] for r in res.results], axis=0).astype(np.float32)
```

```python
import math
from contextlib import ExitStack

import numpy as np
import concourse.bass as bass
import concourse.mybir as mybir
from concourse.bass_utils import run_bass_kernel_spmd

F32 = mybir.dt.float32
BF16 = mybir.dt.bfloat16
U32 = mybir.dt.uint32
AF = mybir.ActivationFunctionType
ALU = mybir.AluOpType
AX = mybir.AxisListType

D = 1024
KT = 8
EPS = 1e-6
NEG = -1.0e30


class Buf:
    __slots__ = ("t", "w", "r")

    def __init__(self, t=None):
        self.t = t
        self.w = None
        self.r = {}

    def __getitem__(self, k):
        return self.t[k]


class KB:
    RING = 6

    def __init__(self, nc, es):
        self.nc = nc
        self.es = es
        self.h = {"pe": nc.tensor, "act": nc.scalar, "dve": nc.vector, "pool": nc.gpsimd, "sp": nc.sync}
        self.sem = {}
        self.cnt = {}
        self.seen = {}
        self.ring = {}
        for e in self.h:
            self.sem[e] = es.enter_context(nc.semaphore("s_" + e))
            self.cnt[e] = 0
            self.seen[e] = {}
        self.nsem = 0
        self.uid = 0
        self.cur = es

    def _ring(self, q):
        if q not in self.ring:
            sems = [self.es.enter_context(self.nc.semaphore("d_%s_%d" % (q, i))) for i in range(self.RING)]
            self.ring[q] = {"sems": sems, "vals": [0] * self.RING, "i": 0}
        return self.ring[q]

    def sb(self, shape, dt, name=None):
        self.uid += 1
        nm = "%s_%d" % (name or "sb", self.uid)
        return Buf(self.cur.enter_context(self.nc.sbuf_tensor(nm, list(shape), dt)))

    def barrier(self):
        for e in self.h:
            for o in self.h:
                if o != e and self.cnt[o] > 0:
                    self._wait(e, (self.sem[o], self.cnt[o], o, o))
            for q, ring in self.ring.items():
                for j in range(self.RING):
                    if ring["vals"][j] > 0:
                        self._wait(e, (ring["sems"][j], ring["vals"][j], None, "d_%s_%d" % (q, j)))

    def scope(self):
        kb = self

        class _S:
            def __enter__(s_):
                s_.prev = kb.cur
                s_.st = ExitStack()
                kb.cur = s_.st
                return s_

            def __exit__(s_, *a):
                kb.barrier()
                s_.st.close()
                kb.cur = s_.prev
                return False
        return _S()

    def ps(self, shape, dt, name=None):
        self.uid += 1
        return Buf(self.es.enter_context(self.nc.psum_tensor(name or ("ps%d" % self.uid), list(shape), dt)))

    def _wait(self, eng, tok):
        if tok is None:
            return
        sem, val, owner, key = tok
        if owner == "pe" and eng == "pe":
            return
        if self.seen[eng].get(key, 0) >= val:
            return
        self.h[eng].wait_ge(sem, val)
        self.seen[eng][key] = val

    def _deps(self, eng, reads, writes):
        for b in reads:
            self._wait(eng, b.w)
        for b in writes:
            self._wait(eng, b.w)
            for t in b.r.values():
                self._wait(eng, t)

    def op(self, eng, fn, reads=(), writes=()):
        self._deps(eng, reads, writes)
        ins = fn(self.h[eng])
        self.cnt[eng] += 1
        ins.then_inc(self.sem[eng], 1)
        tok = (self.sem[eng], self.cnt[eng], eng, eng)
        for b in reads:
            b.r[eng] = tok
        for b in writes:
            b.w = tok
            b.r = {}
        return tok

    def dma(self, q, out, in_, reads=(), writes=(), indirect=None, **kw):
        self._deps(q, reads, writes)
        ring = self._ring(q)
        j = ring["i"] % self.RING
        ring["i"] += 1
        sem = ring["sems"][j]
        prev = ring["vals"][j]
        key = "d_%s_%d" % (q, j)
        if prev > 0:
            self._wait(q, (sem, prev, None, key))
        if indirect is None:
            ins = self.h[q].dma_start(out=out, in_=in_, **kw)
        else:
            ins = self.h[q].indirect_dma_start(out=out, out_offset=None, in_=in_,
                                               in_offset=bass.IndirectOffsetOnAxis(ap=indirect, axis=0),
                                               bounds_check=self.bnd_reg(), oob_is_err=False)
        ins.then_inc(sem, 16)
        ring["vals"][j] = prev + 16
        tok = (sem, prev + 16, None, key)
        for b in reads:
            b.r[key] = tok
        for b in writes:
            b.w = tok
            b.r = {}
        return tok

    def bnd_reg(self):
        if getattr(self, "_bnd", None) is None:
            self._bnd = self.nc.gpsimd.alloc_register("bnd")
            self.nc.gpsimd.reg_mov(self._bnd, 16383)
        return self._bnd

    def finish(self, bufs):
        for b in bufs:
            self._wait("sp", b.w)


def bc(ap, shape):
    return ap.to_broadcast(list(shape))


def build(NB, S, LC, stop_after=None, dbg=False):
    NT = S // 128
    NCX = LC // 128
    NU = NCX + NT
    R = NB + 1
    nc = bass.Bass("TRN2", target_bir_lowering=False)

    def din(name, shape, dt=F32):
        return nc.dram_tensor(name, list(shape), dt, kind="ExternalInput").ap()

    x_in = din("x", [NB, S, D])
    ctx_in = din("ctx", [NB, LC, D])
    cT_in = din("cT", [128, KT, R])
    ada_w = din("ada_w", [2, D, 6 * D])
    ada_b = din("ada_b", [2, 6 * D])
    ada_bT = din("ada_bT", [2, 128, 48])
    n1gT = din("n1gT", [2, 128, KT])
    n2g = din("n2g", [2, D])
    final_g = din("final_g", [D])
    ev_w_in = din("ev_w_in", [D, 2304])
    ev_w_out = din("ev_w_out", [D, D])
    od_w_in = din("od_w_in", [D, 3072])
    od_w_out = din("od_w_out", [D, D])
    qg_in = din("qg", [64])
    kg_in = din("kg", [64])
    rate_in = din("rate", [8])
    lam_in = din("lam", [256])
    subln_in = din("subln", [128])
    peer_wq = din("peer_wq", [2, D, 2048])
    keysT = din("keysT", [2, 128, 16, 128])
    peer_u = [din("peer_u%d" % i, [16384, D]) for i in range(2)]
    peer_v = [din("peer_v%d" % i, [16384, D]) for i in range(2)]
    ident_in = din("ident", [128, 128])
    rope_in = din("rope", [128, NT, 64])
    iota_in = din("iota16", [128, 16])
    relm_in = din("relm", [128, 128])
    pidx_in = din("pidx", [128, 2])
    fidx_in = din("fidx", [128, 128])
    out_d = nc.dram_tensor("out", [NB, S, D], F32, kind="ExternalOutput").ap()

    def dscr(name, shape, dt=F32):
        return nc.dram_tensor(name, list(shape), dt, kind="Internal").ap()

    XSA = dscr("XSA", [NB, S, D])
    XCA = dscr("XCA", [NB, LC, D])
    XSB = dscr("XSB", [NB, S, D])
    MODR = dscr("MODR", [2, R, 6 * D])
    KVD = dscr("KVD", [NU, 128, 4, 128])
    STD = dscr("STD", [NU, 128, 4, 128], BF16)
    UVB = [dscr("UVB%d" % i, [16384, 2 * D], BF16) for i in range(2)]
    uTAB = Buf()

    es = ExitStack()
    k = KB(nc, es)
    op, dma = k.op, k.dma
    uXSA, uXCA, uXSB, uMODR, uKVD, uSTD, uOUT = (Buf() for _ in range(7))
    uIN = Buf()

    ident_f = k.sb([128, 128], F32, "ident_f")
    ident_b = k.sb([128, 128], BF16, "ident_b")
    ones_b = k.sb([128, 128], BF16, "ones_b")
    iota16 = k.sb([128, 16], F32, "iota16")
    rope = k.sb([128, NT, 64], F32, "rope")
    dma("sp", ident_f[:], ident_in, [uIN], [ident_f])
    dma("sp", iota16[:], iota_in, [uIN], [iota16])
    dma("sp", rope[:], rope_in, [uIN], [rope])
    op("dve", lambda e: e.tensor_copy(out=ident_b[:], in_=ident_f[:]), [ident_f], [ident_b])
    op("dve", lambda e: e.memset(ones_b[:], 1.0), [], [ones_b])

    PB = [k.ps([128, 512], F32, "pb%d" % i) for i in range(6)]
    PT_ = k.ps([128, 1024], BF16, "ptb")
    PT2 = k.ps([128, 1024], BF16, "ptb2")

    sc_silu = k.sb([128, KT, R], F32, "sc_silu")
    dma("sp", sc_silu[:], cT_in, [uIN], [sc_silu])
    op("act", lambda e: e.activation(out=sc_silu[:], in_=sc_silu[:], func=AF.Silu), [sc_silu], [sc_silu])
    A1 = k.sb([128, R, KT], F32, "A1")
    B1 = k.sb([128, R, KT], F32, "B1")
    modT = k.sb([128, 16, R], F32, "modT")
    n1g_sb = k.sb([128, KT], F32, "n1g_sb")
    abT_sb = k.sb([128, 48], F32, "abT_sb")
    rows = {}

    xt_ring = [k.sb([128, D], F32, "xt%d" % i) for i in range(2)]
    xn_b = k.sb([128, D], BF16, "xn_b")
    junk = k.sb([128, D], F32, "junk")
    st4 = k.sb([128, 8], F32, "st4")
    hT_ring = [k.sb([128, KT, 128], BF16, "hT%d" % i) for i in range(2)]
    CW = 256
    wstage = [k.sb([128, KT, CW], F32, "wst%d" % i) for i in range(2)]
    cnt = {"xt": 0, "hT": 0, "ws": 0}

    def load_w(dst, col0, src_ap, c0, n):
        off = 0
        while off < n:
            m = min(CW, n - off)
            ws = wstage[cnt["ws"] % 2]
            cnt["ws"] += 1
            dma("sp", ws[:, :, 0:m], src_ap[:, c0 + off:c0 + off + m].rearrange("(k p) n -> p k n", p=128),
                [uIN], [ws])
            op("pool", lambda e, ws=ws, m=m, o=off: e.tensor_copy(out=dst[:, :, col0 + o:col0 + o + m],
                                                               in_=ws[:, :, 0:m]), [ws], [dst])
            off += m

    def rstd_of(xt, col):
        op("act", lambda e: e.activation(out=junk[:], in_=xt[:], func=AF.Square, accum_out=st4[:, col:col + 1]),
           [xt], [junk, st4])
        op("dve", lambda e: e.tensor_scalar(out=st4[:, col:col + 1], in0=st4[:, col:col + 1], scalar1=1.0 / D,
                                            scalar2=EPS, op0=ALU.mult, op1=ALU.add), [st4], [st4])
        op("act", lambda e: e.activation(out=st4[:, col:col + 1], in_=st4[:, col:col + 1], func=AF.Sqrt),
           [st4], [st4])
        op("dve", lambda e: e.reciprocal(out=st4[:, col:col + 1], in_=st4[:, col:col + 1]), [st4], [st4])

    def tile_prep(src_ap, src_unit, ri):
        xt = xt_ring[cnt["xt"] % 2]
        cnt["xt"] += 1
        hT = hT_ring[cnt["hT"] % 2]
        cnt["hT"] += 1
        dma("sp", xt[:], src_ap, [src_unit], [xt])
        rstd_of(xt, 0)
        op("dve", lambda e: e.tensor_scalar(out=xn_b[:], in0=xt[:], scalar1=st4[:, 0:1], scalar2=None,
                                            op0=ALU.mult), [xt, st4], [xn_b])
        for kk in range(KT):
            op("pe", lambda e, kk=kk: e.transpose(out=PT_[:, kk * 128:(kk + 1) * 128],
                                                  in_=xn_b[:, kk * 128:(kk + 1) * 128], identity=ident_b[:]),
               [xn_b, ident_b], [PT_])
        ptv = PT_[:, :].rearrange("p (k t) -> p k t", k=KT)
        op("dve", lambda e: e.tensor_tensor(out=hT[:], in0=ptv, in1=bc(A1[:, ri, :].unsqueeze(2), [128, KT, 128]),
                                            op=ALU.mult), [PT_, A1], [hT])
        op("dve", lambda e: e.tensor_tensor(out=hT[:], in0=hT[:], in1=bc(B1[:, ri, :].unsqueeze(2), [128, KT, 128]),
                                            op=ALU.add), [B1], [hT])
        return xt, hT

    def proj(hT, W, c0, n, pbank, poff=0):
        for kk in range(KT):
            op("pe", lambda e, kk=kk: e.matmul(pbank[:, poff:poff + n], lhsT=hT[:, kk, :], rhs=W[:, kk, c0:c0 + n],
                                               start=(kk == 0), stop=(kk == KT - 1)), [hT, W], [pbank])

    def rope_apply(dst, src, nh, ti, tmp):
        (db, d0), (sbf, s0) = dst, src
        sv = sbf[:, s0:s0 + nh * 64].rearrange("p (h a b c) -> p h a b c", h=nh, a=2, b=2)
        dv = db[:, d0:d0 + nh * 64].rearrange("p (h a b c) -> p h a b c", h=nh, a=2, b=2)
        t1 = tmp[0][:, 0:nh * 32].rearrange("p (h a c) -> p h a c", h=nh, a=2)
        t2 = tmp[1][:, 0:nh * 32].rearrange("p (h a c) -> p h a c", h=nh, a=2)
        rv = rope[:, ti, :].rearrange("p (a b c) -> p a b c", a=2, b=2)
        cosb = bc(rv[:, :, 0, :].unsqueeze(1), [128, nh, 2, 16])
        sinb = bc(rv[:, :, 1, :].unsqueeze(1), [128, nh, 2, 16])
        u1 = sv[:, :, :, 0, :]
        u2 = sv[:, :, :, 1, :]
        op("dve", lambda e: e.tensor_tensor(out=t1, in0=u1, in1=cosb, op=ALU.mult), [sbf, rope], [tmp[0]])
        op("dve", lambda e: e.tensor_tensor(out=t2, in0=u2, in1=sinb, op=ALU.mult), [sbf, rope], [tmp[1]])
        op("dve", lambda e: e.tensor_tensor(out=dv[:, :, :, 0, :], in0=t1, in1=t2, op=ALU.subtract),
           [tmp[0], tmp[1]], [db])
        op("dve", lambda e: e.tensor_tensor(out=t1, in0=u1, in1=sinb, op=ALU.mult), [sbf, rope], [tmp[0]])
        op("dve", lambda e: e.tensor_tensor(out=t2, in0=u2, in1=cosb, op=ALU.mult), [sbf, rope], [tmp[1]])
        op("dve", lambda e: e.tensor_tensor(out=dv[:, :, :, 1, :], in0=t1, in1=t2, op=ALU.add),
           [tmp[0], tmp[1]], [db])

    def mod_phase(l):
      with k.scope():
        modrow_sb = k.sb([R, 6 * D], F32, "modrow_sb")
        adab_rows = k.sb([R, 6 * D], F32, "adab_rows")
        dma("sp", adab_rows[:], ada_b[l, :].partition_broadcast(R), [uIN], [adab_rows])
        dma("sp", abT_sb[:], ada_bT[l], [uIN], [abT_sb])
        dma("sp", n1g_sb[:], n1gT[l], [uIN], [n1g_sb])
        pm, pt = PB[0], PB[1]
        for cc in range(6 * D // CW):
            ws = wstage[cnt["ws"] % 2]
            cnt["ws"] += 1
            dma("sp", ws[:], ada_w[l, :, cc * CW:(cc + 1) * CW].rearrange("(k p) n -> p k n", p=128), [uIN], [ws])
            for kk in range(KT):
                op("pe", lambda e, kk=kk: e.matmul(pm[0:R, 0:CW], lhsT=sc_silu[:, kk, :], rhs=ws[:, kk, :],
                                                   start=(kk == 0), stop=(kk == KT - 1)), [sc_silu, ws], [pm])
            op("dve", lambda e, cc=cc: e.tensor_tensor(out=modrow_sb[:, cc * CW:(cc + 1) * CW], in0=pm[0:R, 0:CW],
                                                       in1=adab_rows[:, cc * CW:(cc + 1) * CW], op=ALU.add),
               [pm, adab_rows], [modrow_sb])
            if cc < 2048 // CW:
                for jj in range(CW // 128):
                    j = cc * (CW // 128) + jj
                    for kk in range(KT):
                        op("pe", lambda e, kk=kk, jj=jj, j=j: e.matmul(
                            pt[:, j * R:(j + 1) * R], lhsT=ws[:, kk, jj * 128:(jj + 1) * 128], rhs=sc_silu[:, kk, :],
                            start=(kk == 0), stop=(kk == KT - 1)), [sc_silu, ws], [pt])
        op("dve", lambda e: e.tensor_tensor(out=modT[:], in0=pt[:, 0:16 * R].rearrange("p (j r) -> p j r", r=R),
                                            in1=bc(abT_sb[:, 0:16].unsqueeze(2), [128, 16, R]), op=ALU.add),
           [pt, abT_sb], [modT])
        for r in range(R):
            op("dve", lambda e, r=r: e.scalar_tensor_tensor(out=A1[:, r, :], in0=modT[:, 8:16, r], scalar=1.0,
                                                            in1=n1g_sb[:], op0=ALU.add, op1=ALU.mult),
               [modT, n1g_sb], [A1])
            op("dve", lambda e, r=r: e.tensor_copy(out=B1[:, r, :], in_=modT[:, 0:8, r]), [modT], [B1])
        dma("sp", MODR[l], modrow_sb[:], [modrow_sb], [uMODR])

    def alloc_rows_attn():
        rows["G1"] = [k.sb([128, D], F32, "rowG1_%d" % i) for i in range(2)]

    def alloc_rows_peer():
        rows["G2"] = [k.sb([128, D], F32, "rowG2_%d" % i) for i in range(2)]
        rows["SH2"] = [k.sb([128, D], F32, "rowSH2_%d" % i) for i in range(2)]
        rows["g2"] = [k.sb([128, D], F32, "rowg2_%d" % i) for i in range(2)]

    def load_rows_attn(l, r, slot):
        dma("sp", rows["G1"][slot][:], MODR[l, r, 2 * D:3 * D].partition_broadcast(128), [uMODR], [rows["G1"][slot]])

    def load_rows_peer(l, r, slot):
        def row(c0):
            return MODR[l, r, c0:c0 + D].partition_broadcast(128)
        rowG2, rowSH2, rowg2 = rows["G2"], rows["SH2"], rows["g2"]
        n2g_row = junk
        dma("sp", n2g_row[:], n2g[l, :].partition_broadcast(128), [uIN], [n2g_row])
        dma("sp", rowSH2[slot][:], row(3 * D), [uMODR], [rowSH2[slot]])
        dma("sp", rowG2[slot][:], row(4 * D), [uMODR], [rowG2[slot]])
        dma("sp", rowg2[slot][:], row(5 * D), [uMODR], [rowg2[slot]])
        op("dve", lambda e: e.scalar_tensor_tensor(out=rowG2[slot][:], in0=rowG2[slot][:], scalar=1.0,
                                                   in1=n2g_row[:], op0=ALU.add, op1=ALU.mult),
           [n2g_row], [rowG2[slot]])

    ynew = k.sb([128, D], F32, "ynew")
    KTall = Vall = Wp = Wo = QTst = mixT = basex = PTr = qtm = qtb = rt = sq8 = None
    rec = rec2 = ot1 = ot2 = osq = qg_row = kg_row = None
    cnt["ptr"] = 0

    def alloc_attn(kt_shape, v_shape, wcols, wo_shape, q_shape, mix_shape):
        nonlocal KTall, Vall, Wp, Wo, QTst, mixT, basex, PTr, qtm, qtb, rt, sq8, rec, rec2, ot1, ot2, osq
        nonlocal qg_row, kg_row
        if kt_shape is not None:
            KTall = k.sb(kt_shape, BF16, "KTall")
            Vall = k.sb(v_shape, BF16, "Vall")
        Wp = k.sb([128, KT, wcols], BF16, "Wp")
        Wo = k.sb(wo_shape, BF16, "Wo")
        QTst = k.sb(q_shape, BF16, "QTst")
        mixT = k.sb(mix_shape, BF16, "mixT")
        basex = k.sb([128, D], F32, "basex")
        PTr = [k.sb([128, 512], BF16, "ptr%d" % i) for i in range(3)]
        qtm = k.sb([128, 1024], F32, "qtm")
        qtb = k.sb([128, 1024], BF16, "qtb")
        rt = [k.sb([128, 512], F32, "rt%d" % i) for i in range(2)]
        sq8 = k.sb([128, 16], F32, "sq8")
        rec = k.sb([128, 512], F32, "rec")
        rec2 = k.sb([128, 512], F32, "rec2")
        ot1 = k.sb([128, 512], F32, "ot1")
        ot2 = k.sb([128, 512], F32, "ot2")
        osq = k.sb([128, 512], BF16, "osq")
        qg_row = k.sb([128, 64], F32, "qg_row")
        kg_row = k.sb([128, 64], F32, "kg_row")
        dma("sp", qg_row[:], qg_in.partition_broadcast(128), [uIN], [qg_row])
        dma("sp", kg_row[:], kg_in.partition_broadcast(128), [uIN], [kg_row])
        alloc_rows_attn()

    def head_rms(src_buf, c0, nh, g_row, dst_buf, d0):
        sv = src_buf[:, c0:c0 + nh * 64].rearrange("p (h c) -> p h c", h=nh)
        tv = rt[0][:, 0:nh * 64].rearrange("p (h c) -> p h c", h=nh)
        op("dve", lambda e: e.tensor_tensor(out=tv, in0=sv, in1=sv, op=ALU.mult), [src_buf], [rt[0]])
        op("dve", lambda e: e.tensor_reduce(out=sq8[:, 0:nh], in_=tv, axis=AX.X, op=ALU.add), [rt[0]], [sq8])
        op("dve", lambda e: e.tensor_scalar(out=sq8[:, 0:nh], in0=sq8[:, 0:nh], scalar1=1.0 / 64, scalar2=EPS,
                                            op0=ALU.mult, op1=ALU.add), [], [sq8])
        op("act", lambda e: e.activation(out=sq8[:, 0:nh], in_=sq8[:, 0:nh], func=AF.Sqrt), [sq8], [sq8])
        op("dve", lambda e: e.reciprocal(out=sq8[:, 0:nh], in_=sq8[:, 0:nh]), [sq8], [sq8])
        dv = dst_buf[:, d0:d0 + nh * 64].rearrange("p (h c) -> p h c", h=nh)
        op("dve", lambda e: e.tensor_tensor(out=dv, in0=sv, in1=bc(sq8[:, 0:nh].unsqueeze(2), [128, nh, 64]),
                                            op=ALU.mult), [src_buf, sq8], [dst_buf])
        op("dve", lambda e: e.tensor_tensor(out=dv, in0=dv, in1=bc(g_row[:, :].unsqueeze(1), [128, nh, 64]),
                                            op=ALU.mult), [g_row], [dst_buf])

    def tiles_of(b):
        return [(True, i) for i in range(NCX)] + [(False, i) for i in range(NT)]

    def src_tile(l, b, is_ctx, i):
        if l == 0:
            return (ctx_in[b, i * 128:(i + 1) * 128, :], uIN) if is_ctx else (x_in[b, i * 128:(i + 1) * 128, :], uIN)
        return (XCA[b, i * 128:(i + 1) * 128, :], uXCA) if is_ctx else (XSA[b, i * 128:(i + 1) * 128, :], uXSA)

    def dst_tile(l, b, is_ctx, i):
        if l == 0:
            return (XCA[b, i * 128:(i + 1) * 128, :], uXCA) if is_ctx else (XSA[b, i * 128:(i + 1) * 128, :], uXSA)
        return (None, None) if is_ctx else (XSB[b, i * 128:(i + 1) * 128, :], uXSB)

    def out_proj_store(l, b, is_ctx, tiles, first, nchunk):
        for si, ti in enumerate(tiles):
            for half in range(2):
                for c in range(nchunk):
                    op("pe", lambda e, c=c, half=half, si=si: e.matmul(
                        PB[4 + half][:, :], lhsT=mixT[:, c, si * 128:(si + 1) * 128],
                        rhs=Wo[:, c, half * 512:(half + 1) * 512], start=(c == 0), stop=(c == nchunk - 1)),
                       [mixT, Wo], [PB[4 + half]])
            dap, dunit = dst_tile(l, b, is_ctx, ti)
            base = basex
            if first:
                sap, sunit = src_tile(l, b, is_ctx, ti)
                dma("sp", base[:], sap, [sunit], [base])
            else:
                dma("sp", base[:], dap, [dunit], [base])
            g1 = rows["G1"][1 if is_ctx else 0]
            for half in range(2):
                op("dve", lambda e, half=half: e.tensor_tensor(out=ynew[:, half * 512:(half + 1) * 512],
                                                               in0=PB[4 + half][:, :],
                                                               in1=g1[:, half * 512:(half + 1) * 512], op=ALU.mult),
                   [PB[4 + half], g1], [ynew])
            op("dve", lambda e: e.tensor_tensor(out=ynew[:], in0=ynew[:], in1=base[:], op=ALU.add), [base], [ynew])
            dma("sp", dap, ynew[:], [ynew], [dunit])

    def supertiles(b, with_ctx):
        sts = []
        if with_ctx:
            sts.append((True, list(range(NCX))))
        for s0 in range(0, NT, 4):
            sts.append((False, list(range(s0, min(s0 + 4, NT)))))
        return sts

    def load_wo(src_rows, pk, nchunk):
        for c0 in range(0, D, CW):
            stg = wstage[cnt["ws"] % 2]
            cnt["ws"] += 1
            dma("sp", stg[0:pk, 0:nchunk, :], src_rows[:, c0:c0 + CW].rearrange("(c p) n -> p c n", p=pk),
                [uIN], [stg])
            op("pool", lambda e: e.tensor_copy(out=Wo[0:pk, 0:nchunk, c0:c0 + CW], in_=stg[0:pk, 0:nchunk, :]),
               [stg], [Wo])

    def gqa_pass(b, first):
      with k.scope():
        l = 0
        alloc_attn([64, 2, NU * 128], [128, NU, 256], 768, [64, 8, D], [64, 8, 512], [64, 8, 512])
        load_rows_attn(0, b, 0)
        load_rows_attn(0, NB, 1)
        load_w(Wp, 0, ev_w_in, 512, 256)
        load_w(Wp, 256, ev_w_in, 0, 512)
        load_wo(ev_w_out[0:512, :], 64, 8)
        vv = Vall[:, :, 0:256].rearrange("p u (h c) -> p u h c", h=2)
        op("pool", lambda e: e.memset(vv[:, :, :, 64:128], 1.0), [], [Vall])
        for u, (is_ctx, ti) in enumerate(tiles_of(b)):
            sap, sunit = src_tile(l, b, is_ctx, ti)
            xt, hT = tile_prep(sap, sunit, NB if is_ctx else b)
            proj(hT, Wp, 0, 256, PB[0])
            op("act", lambda e: e.activation(out=qtm[:, 0:256], in_=PB[0][:, 0:256], func=AF.Copy), [PB[0]], [qtm])
            head_rms(qtm, 0, 2, kg_row, qtm, 0)
            if not is_ctx:
                rope_apply((qtm, 256), (qtm, 0), 2, ti, rt)
                ksrc = 256
            else:
                ksrc = 0
            op("act", lambda e, ksrc=ksrc: e.activation(out=qtb[:, 0:128], in_=qtm[:, ksrc:ksrc + 128], func=AF.Copy),
               [qtm], [qtb])
            for h in range(2):
                op("pe", lambda e, h=h: e.transpose(out=PT2[0:64, h * 128:(h + 1) * 128],
                                                    in_=qtb[:, h * 64:(h + 1) * 64], identity=ident_b[:]),
                   [qtb, ident_b], [PT2])
            op("act", lambda e, u=u: e.activation(
                out=KTall[0:64, 0:2, u * 128:(u + 1) * 128],
                in_=PT2[0:64, 0:256].rearrange("p (h t) -> p h t", h=2), func=AF.Copy), [PT2], [KTall])
            op("act", lambda e, u=u: e.activation(
                out=Vall[:, u, 0:256].rearrange("p (h c) -> p h c", h=2)[:, :, 0:64],
                in_=qtm[:, 128:256].rearrange("p (h c) -> p h c", h=2), func=AF.Copy), [qtm], [Vall])
        for (is_ctx, tiles) in supertiles(b, True):
            N = 128 * len(tiles)
            for si, ti in enumerate(tiles):
                sap, sunit = src_tile(l, b, is_ctx, ti)
                xt, hT = tile_prep(sap, sunit, NB if is_ctx else b)
                proj(hT, Wp, 256, 512, PB[0])
                op("act", lambda e: e.activation(out=qtm[:, 0:512], in_=PB[0][:, :], func=AF.Copy), [PB[0]], [qtm])
                head_rms(qtm, 0, 8, qg_row, qtm, 0)
                if not is_ctx:
                    rope_apply((qtm, 512), (qtm, 0), 8, ti, rt)
                    qsrc = 512
                else:
                    qsrc = 0
                op("act", lambda e, qsrc=qsrc: e.activation(out=qtb[:, 0:512], in_=qtm[:, qsrc:qsrc + 512],
                                                           func=AF.Copy), [qtm], [qtb])
                for h in range(8):
                    op("pe", lambda e, h=h: e.transpose(out=PT2[0:64, h * 128:(h + 1) * 128],
                                                        in_=qtb[:, h * 64:(h + 1) * 64], identity=ident_b[:]),
                       [qtb, ident_b], [PT2])
                op("act", lambda e, si=si: e.activation(
                    out=QTst[0:64, :, si * 128:(si + 1) * 128],
                    in_=PT2[0:64, :].rearrange("p (h t) -> p h t", h=8), func=AF.Copy), [PT2], [QTst])
            keys = list(range(NCX)) if is_ctx else list(range(NU))
            for head in range(8):
                kvh = head // 4
                acc = PB[2 + head % 2]
                for ui, u in enumerate(keys):
                    sp_ = PB[ui % 2]
                    op("pe", lambda e, u=u, sp_=sp_: e.matmul(
                        sp_[:, 0:N], lhsT=KTall[0:64, kvh, u * 128:(u + 1) * 128], rhs=QTst[0:64, head, 0:N],
                        start=True, stop=True), [KTall, QTst], [sp_])
                    pt = PTr[cnt["ptr"] % 3]
                    cnt["ptr"] += 1
                    op("act", lambda e, sp_=sp_, pt=pt: e.activation(out=pt[:, 0:N], in_=sp_[:, 0:N], func=AF.Exp,
                                                                     scale=0.125), [sp_], [pt])
                    op("pe", lambda e, u=u, pt=pt, ui=ui: e.matmul(
                        acc[:, 0:N], lhsT=Vall[:, u, kvh * 128:(kvh + 1) * 128], rhs=pt[:, 0:N],
                        start=(ui == 0), stop=(ui == len(keys) - 1)), [Vall, pt], [acc])
                op("dve", lambda e: e.reciprocal(out=rec[64:128, 0:N], in_=acc[64:128, 0:N]), [acc], [rec])
                op("dve", lambda e: e.tensor_tensor(out=mixT[0:64, head, 0:N], in0=acc[0:64, 0:N],
                                                    in1=rec[64:128, 0:N], op=ALU.mult), [acc, rec], [mixT])
            out_proj_store(l, b, is_ctx, tiles, first, 8)

    ret_c = {}

    def ret_setup():
        lg = k.sb([128, 8], F32, "lg")
        dma("sp", lg[:], rate_in.partition_broadcast(128), [uIN], [lg])
        op("act", lambda e: e.activation(out=lg[:], in_=lg[:], func=AF.Exp), [lg], [lg])
        op("dve", lambda e: e.tensor_scalar(out=lg[:], in0=lg[:], scalar1=-1.0, scalar2=None, op0=ALU.mult), [], [lg])
        relm = k.sb([128, 128], F32, "relm")
        pidx = k.sb([128, 2], F32, "pidx")
        fidx = k.sb([128, 128], F32, "fidx")
        dma("sp", relm[:], relm_in, [uIN], [relm])
        dma("sp", pidx[:], pidx_in, [uIN], [pidx])
        dma("sp", fidx[:], fidx_in, [uIN], [fidx])
        relp = k.sb([128, 128], F32, "relp")
        reln = k.sb([128, 128], F32, "reln")
        mp = k.sb([128, 128], F32, "mp")
        mn = k.sb([128, 128], F32, "mn")
        op("dve", lambda e: e.tensor_scalar(out=relp[:], in0=relm[:], scalar1=0.0, scalar2=None, op0=ALU.max),
           [relm], [relp])
        op("dve", lambda e: e.tensor_scalar(out=reln[:], in0=relm[:], scalar1=-1.0, scalar2=0.0, op0=ALU.mult,
                                            op1=ALU.max), [relm], [reln])
        op("dve", lambda e: e.tensor_scalar(out=mp[:], in0=relm[:], scalar1=0.0, scalar2=None, op0=ALU.is_ge),
           [relm], [mp])
        op("dve", lambda e: e.tensor_scalar(out=mn[:], in0=relm[:], scalar1=0.0, scalar2=None, op0=ALU.is_le),
           [relm], [mn])
        DT = k.sb([128, 4, 128], F32, "DT")
        QD = k.sb([128, 4, 128], F32, "QD")
        KD = k.sb([128, 4, 2], F32, "KD")
        DEC = k.sb([128, 4], F32, "DEC")
        tmpm = k.sb([128, 128], F32, "tmpm")
        for h in range(4):
            lf, lb = lg[:, h:h + 1], lg[:, 4 + h:5 + h]
            op("act", lambda e, lf=lf: e.activation(out=tmpm[:], in_=relp[:], func=AF.Exp, scale=lf), [relp, lg], [tmpm])
            op("dve", lambda e, h=h: e.tensor_tensor(out=DT[:, h, :], in0=tmpm[:], in1=mp[:], op=ALU.mult),
               [tmpm, mp], [DT])
            op("act", lambda e, lb=lb: e.activation(out=tmpm[:], in_=reln[:], func=AF.Exp, scale=lb), [reln, lg], [tmpm])
            op("dve", lambda e: e.tensor_tensor(out=tmpm[:], in0=tmpm[:], in1=mn[:], op=ALU.mult), [mn], [tmpm])
            op("dve", lambda e, h=h: e.tensor_tensor(out=DT[:, h, :], in0=DT[:, h, :], in1=tmpm[:], op=ALU.add),
               [tmpm], [DT])
            op("act", lambda e, h=h, lf=lf: e.activation(out=QD[0:64, h, :], in_=fidx[0:64, :], func=AF.Exp,
                                                         scale=lg[0:64, h:h + 1]), [fidx, lg], [QD])
            op("act", lambda e, h=h: e.activation(out=QD[64:128, h, :], in_=fidx[64:128, :], func=AF.Exp,
                                                  scale=lg[64:128, 4 + h:5 + h]), [fidx, lg], [QD])
            op("act", lambda e, h=h, lf=lf: e.activation(out=KD[:, h, 0:1], in_=pidx[:, 0:1], func=AF.Exp, scale=lf),
               [pidx, lg], [KD])
            op("act", lambda e, h=h, lb=lb: e.activation(out=KD[:, h, 1:2], in_=pidx[:, 1:2], func=AF.Exp, scale=lb),
               [pidx, lg], [KD])
            op("act", lambda e, h=h: e.activation(out=DEC[0:64, h:h + 1], in_=lg[0:64, h:h + 1], func=AF.Exp,
                                                  scale=128.0), [lg], [DEC])
            op("act", lambda e, h=h: e.activation(out=DEC[64:128, h:h + 1], in_=lg[64:128, 4 + h:5 + h], func=AF.Exp,
                                                  scale=128.0), [lg], [DEC])
        ret_c.update(DT=DT, QD=QD, KD=KD, DEC=DEC)
        ret_c["kbd"] = k.sb([128, 4, 128], BF16, "kbd")
        ret_c["vb"] = k.sb([128, 512], BF16, "vbb")
        ret_c["kvs"] = k.sb([128, 4, 128], F32, "kvs")
        ret_c["S"] = k.sb([128, 4, 128], F32, "Sst")
        ret_c["Sb"] = k.sb([128, 4, 128], BF16, "Sbb")
        ret_c["Sl"] = k.sb([128, 4, 128], BF16, "Sl")
        ret_c["qdup"] = k.sb([128, 4, 128], BF16, "qdup")
        ret_c["qT"] = k.sb([64, 4, 128], BF16, "qTr")
        ret_c["qdT"] = k.sb([128, 4, 128], BF16, "qdT")
        ret_c["kT"] = k.sb([64, 4, 128], BF16, "kTr")
        ret_c["scm"] = k.sb([128, 4, 128], BF16, "scm")
        ret_c["gsl"] = k.sb([128, 512], F32, "gsl")
        ret_c["om"] = k.sb([128, 512], F32, "om")
        ret_c["omb"] = k.sb([128, 512], BF16, "omb")

    def ret_qkv(b, is_ctx, ti, need_q):
        l = 0
        rc = ret_c
        sap, sunit = src_tile(l, b, is_ctx, ti)
        xt, hT = tile_prep(sap, sunit, NB if is_ctx else b)
        proj(hT, Wp, 0, 512, PB[0])
        proj(hT, Wp, 512, 512, PB[1])
        op("act", lambda e: e.activation(out=qtm[:, 0:256], in_=PB[0][:, 0:256], func=AF.Copy), [PB[0]], [qtm])
        op("act", lambda e: e.activation(out=qtm[:, 256:512], in_=PB[0][:, 256:512], func=AF.Copy, scale=0.125),
           [PB[0]], [qtm])
        op("act", lambda e: e.activation(out=rc["vb"][:], in_=PB[1][:, :], func=AF.Copy), [PB[1]], [rc["vb"]])
        if not is_ctx:
            rope_apply((qtm, 512), (qtm, 0), 8, ti, rt)
            s0 = 512
        else:
            s0 = 0
        kv4 = qtm[:, s0 + 256:s0 + 512].rearrange("p (h c) -> p h c", h=4)
        op("dve", lambda e: e.tensor_copy(out=qtb[:, 0:256], in_=qtm[:, s0 + 256:s0 + 512]), [qtm], [qtb])
        for d_ in range(2):
            op("dve", lambda e, d_=d_: e.tensor_tensor(
                out=rc["kbd"][:, :, d_ * 64:(d_ + 1) * 64], in0=kv4,
                in1=bc(rc["KD"][:, :, d_:d_ + 1], [128, 4, 64]), op=ALU.mult), [qtm, rc["KD"]], [rc["kbd"]])
        for h in range(4):
            op("pe", lambda e, h=h: e.transpose(out=PT2[0:64, h * 128:(h + 1) * 128], in_=qtb[:, h * 64:(h + 1) * 64],
                                                identity=ident_b[:]), [qtb, ident_b], [PT2])
        op("act", lambda e: e.activation(out=rc["kT"][:], in_=PT2[0:64, 0:512].rearrange("p (h t) -> p h t", h=4),
                                         func=AF.Copy), [PT2], [rc["kT"]])
        if need_q:
            qv4 = qtm[:, s0:s0 + 256].rearrange("p (h c) -> p h c", h=4)
            for d_ in range(2):
                op("dve", lambda e, d_=d_: e.tensor_copy(out=rc["qdup"][:, :, d_ * 64:(d_ + 1) * 64], in_=qv4),
                   [qtm], [rc["qdup"]])
            for h in range(4):
                op("pe", lambda e, h=h: e.transpose(out=PT_[:, h * 128:(h + 1) * 128], in_=rc["qdup"][:, h, :],
                                                    identity=ident_b[:]), [rc["qdup"], ident_b], [PT_])
            ptv = PT_[:, 0:512].rearrange("p (h t) -> p h t", h=4)
            op("dve", lambda e: e.tensor_copy(out=rc["qT"][:], in_=PT_[0:64, 0:512].rearrange("p (h t) -> p h t", h=4)),
               [PT_], [rc["qT"]])
            op("dve", lambda e: e.tensor_tensor(out=rc["qdT"][:], in0=ptv, in1=rc["QD"][:], op=ALU.mult),
               [PT_, rc["QD"]], [rc["qdT"]])
            if stop_after == "ret_qa":
                return xt
            proj(hT, Wp, 1024, 512, PB[2])
            if stop_after == "ret_qb":
                return xt
            op("act", lambda e: e.activation(out=rc["gsl"][:], in_=PB[2][:, :], func=AF.Silu), [PB[2]], [rc["gsl"]])
        return xt

    def ret_pass(b, first):
      with k.scope():
        l = 0
        alloc_attn(None, None, 1536, [128, 4, D], [128, 1, 128], [128, 4, 512])
        ret_setup()
        rc = ret_c
        load_rows_attn(0, b, 0)
        load_rows_attn(0, NB, 1)
        load_w(Wp, 0, ev_w_in, 768, 256)
        load_w(Wp, 256, ev_w_in, 1024, 256)
        load_w(Wp, 512, ev_w_in, 1280, 512)
        load_w(Wp, 1024, ev_w_in, 1792, 512)
        load_wo(ev_w_out[512:1024, :], 128, 4)
        tl = tiles_of(b)
        if stop_after == "ret_setup":
            return
        for u, (is_ctx, ti) in enumerate(tl):
            ret_qkv(b, is_ctx, ti, False)
            for h in range(4):
                op("pe", lambda e, h=h: e.matmul(PB[3][:, h * 128:(h + 1) * 128], lhsT=rc["kbd"][:, h, :],
                                                 rhs=rc["vb"][:, h * 128:(h + 1) * 128], start=True, stop=True),
                   [rc["kbd"], rc["vb"]], [PB[3]])
            op("act", lambda e: e.activation(out=rc["kvs"][:], in_=PB[3][:, :].rearrange("p (h c) -> p h c", h=4),
                                             func=AF.Copy), [PB[3]], [rc["kvs"]])
            dma("sp", KVD[u], rc["kvs"][:], [rc["kvs"]], [uKVD])
        if stop_after == "ret_p1":
            return
        of = list(range(NU))
        ob = list(range(NCX - 1, -1, -1)) + list(range(NU - 1, NCX - 1, -1))
        S = rc["S"]
        op("dve", lambda e: e.memset(S[:], 0.0), [], [S])
        for t in range(NU):
            cf, cb = of[t], ob[t]
            op("act", lambda e: e.activation(out=rc["Sb"][:], in_=S[:], func=AF.Copy), [S], [rc["Sb"]])
            dma("sp", STD[cf, 0:64], rc["Sb"][0:64], [rc["Sb"]], [uSTD])
            dma("sp", STD[cb, 64:128], rc["Sb"][64:128], [rc["Sb"]], [uSTD])
            dma("sp", rc["kvs"][0:64], KVD[cf, 0:64], [uKVD], [rc["kvs"]])
            dma("sp", rc["kvs"][64:128], KVD[cb, 64:128], [uKVD], [rc["kvs"]])
            op("dve", lambda e: e.tensor_tensor(out=S[:], in0=S[:], in1=bc(rc["DEC"][:, :].unsqueeze(2), [128, 4, 128]),
                                                op=ALU.mult), [rc["DEC"]], [S])
            op("dve", lambda e: e.tensor_tensor(out=S[:], in0=S[:], in1=rc["kvs"][:], op=ALU.add), [rc["kvs"]], [S])
        if stop_after == "ret_scan":
            return
        for (is_ctx, tiles) in supertiles(b, True):
            for si, ti in enumerate(tiles):
                u = ti if is_ctx else NCX + ti
                xt = ret_qkv(b, is_ctx, ti, True)
                if stop_after in ("ret_q", "ret_qa", "ret_qb"):
                    return
                dma("sp", rc["Sl"][:], STD[u], [uSTD], [rc["Sl"]])
                for h in range(4):
                    op("pe", lambda e, h=h: e.matmul(PB[3][:, h * 128:(h + 1) * 128], lhsT=rc["kT"][:, h, :],
                                                     rhs=rc["qT"][:, h, :], start=True, stop=True),
                       [rc["kT"], rc["qT"]], [PB[3]])
                op("dve", lambda e: e.tensor_tensor(out=rc["scm"][:],
                                                    in0=PB[3][:, :].rearrange("p (h c) -> p h c", h=4),
                                                    in1=rc["DT"][:], op=ALU.mult), [PB[3], rc["DT"]], [rc["scm"]])
                for h in range(4):
                    op("pe", lambda e, h=h: e.matmul(PB[2][:, h * 128:(h + 1) * 128], lhsT=rc["scm"][:, h, :],
                                                     rhs=rc["vb"][:, h * 128:(h + 1) * 128], start=True, stop=False),
                       [rc["scm"], rc["vb"]], [PB[2]])
                    op("pe", lambda e, h=h: e.matmul(PB[2][:, h * 128:(h + 1) * 128], lhsT=rc["qdT"][:, h, :],
                                                     rhs=rc["Sl"][:, h, :], start=False, stop=True),
                       [rc["qdT"], rc["Sl"]], [PB[2]])
                if stop_after == "ret_o":
                    return
                om = rc["om"]
                op("act", lambda e: e.activation(out=om[:], in_=PB[2][:, :], func=AF.Copy), [PB[2]], [om])
                ov = om[:, :].rearrange("p (h c) -> p h c", h=4)
                tv = rt[0][:, 0:512].rearrange("p (h c) -> p h c", h=4)
                op("dve", lambda e: e.tensor_tensor(out=tv, in0=ov, in1=ov, op=ALU.mult), [om], [rt[0]])
                op("dve", lambda e: e.tensor_reduce(out=sq8[:, 0:4], in_=tv, axis=AX.X, op=ALU.add), [rt[0]], [sq8])
                op("dve", lambda e: e.tensor_scalar(out=sq8[:, 0:4], in0=sq8[:, 0:4], scalar1=1.0 / 128, scalar2=EPS,
                                                    op0=ALU.mult, op1=ALU.add), [], [sq8])
                op("act", lambda e: e.activation(out=sq8[:, 0:4], in_=sq8[:, 0:4], func=AF.Sqrt), [sq8], [sq8])
                op("dve", lambda e: e.reciprocal(out=sq8[:, 0:4], in_=sq8[:, 0:4]), [sq8], [sq8])
                op("dve", lambda e: e.tensor_tensor(out=ov, in0=ov, in1=bc(sq8[:, 0:4].unsqueeze(2), [128, 4, 128]),
                                                    op=ALU.mult), [sq8], [om])
                op("dve", lambda e: e.tensor_tensor(out=rc["omb"][:], in0=om[:], in1=rc["gsl"][:], op=ALU.mult),
                   [om, rc["gsl"]], [rc["omb"]])
                for h in range(4):
                    op("pe", lambda e, h=h: e.transpose(out=PT2[:, h * 128:(h + 1) * 128],
                                                        in_=rc["omb"][:, h * 128:(h + 1) * 128], identity=ident_b[:]),
                       [rc["omb"], ident_b], [PT2])
                op("act", lambda e, si=si: e.activation(out=mixT[:, :, si * 128:(si + 1) * 128],
                                                        in_=PT2[:, 0:512].rearrange("p (h t) -> p h t", h=4),
                                                        func=AF.Copy), [PT2], [mixT])
            if stop_after == "ret_m":
                return
            out_proj_store(l, b, is_ctx, tiles, first, 4)

    diff_c = {}

    def diff_setup():
        lam_sb = k.sb([128, 256], F32, "lam_sb")
        dma("sp", lam_sb[:], lam_in.partition_broadcast(128), [uIN], [lam_sb])
        l2 = k.sb([128, 2], F32, "l2")
        lv = lam_sb[:, :].rearrange("p (a b c) -> p a b c", a=2, b=2)
        tv = rt[0][:, 0:128].rearrange("p (a c) -> p a c", a=2)
        op("dve", lambda e: e.tensor_tensor(out=tv, in0=lv[:, :, 0, :], in1=lv[:, :, 1, :], op=ALU.mult),
           [lam_sb], [rt[0]])
        op("dve", lambda e: e.tensor_reduce(out=l2[:], in_=tv, axis=AX.X, op=ALU.add), [rt[0]], [l2])
        op("act", lambda e: e.activation(out=l2[:], in_=l2[:], func=AF.Exp), [l2], [l2])
        lam_init = 0.8 - 0.6 * math.exp(-0.3 * 1)
        nl = k.sb([128, 1], F32, "neglam")
        op("dve", lambda e: e.tensor_tensor(out=nl[:], in0=l2[:, 1:2], in1=l2[:, 0:1], op=ALU.subtract), [l2], [nl])
        op("dve", lambda e: e.tensor_scalar(out=nl[:], in0=nl[:], scalar1=-lam_init, scalar2=None, op0=ALU.add),
           [], [nl])
        sg = k.sb([128, 1], F32, "sublng")
        dma("sp", sg[:], subln_in.rearrange("(p o) -> p o", o=1), [uIN], [sg])
        op("dve", lambda e: e.tensor_scalar(out=sg[:], in0=sg[:], scalar1=1.0 - lam_init, scalar2=None, op0=ALU.mult),
           [], [sg])
        diff_c.update(nl=nl, sg=sg)

    def diff_pass(b, grp, first):
      with k.scope():
        l = 1
        alloc_attn([128, 4, NU * 128], [128, NU, 512], 1536, [128, 4, D], [128, 4, 512], [128, 4, 512])
        diff_setup()
        nl, sg = diff_c["nl"], diff_c["sg"]
        load_rows_attn(1, b, 0)
        load_rows_attn(1, NB, 1)
        load_w(Wp, 0, od_w_in, 1024 + grp * 512, 512)
        load_w(Wp, 512, od_w_in, 2048 + grp * 512, 512)
        load_w(Wp, 1024, od_w_in, grp * 512, 512)
        load_wo(od_w_out[grp * 512:(grp + 1) * 512, :], 128, 4)
        for u, (is_ctx, ti) in enumerate(tiles_of(b)):
            sap, sunit = src_tile(l, b, is_ctx, ti)
            xt, hT = tile_prep(sap, sunit, NB if is_ctx else b)
            proj(hT, Wp, 0, 512, PB[0])
            proj(hT, Wp, 512, 512, PB[1])
            op("act", lambda e: e.activation(out=qtm[:, 0:512], in_=PB[0][:, :], func=AF.Copy), [PB[0]], [qtm])
            op("act", lambda e, u=u: e.activation(out=Vall[:, u, :], in_=PB[1][:, :], func=AF.Copy), [PB[1]], [Vall])
            if not is_ctx:
                rope_apply((qtm, 512), (qtm, 0), 8, ti, rt)
                s0 = 512
            else:
                s0 = 0
            op("act", lambda e, s0=s0: e.activation(out=qtb[:, 0:512], in_=qtm[:, s0:s0 + 512], func=AF.Copy),
               [qtm], [qtb])
            for h in range(4):
                op("pe", lambda e, h=h: e.transpose(out=PT2[:, h * 128:(h + 1) * 128],
                                                    in_=qtb[:, h * 128:(h + 1) * 128], identity=ident_b[:]),
                   [qtb, ident_b], [PT2])
            op("act", lambda e, u=u: e.activation(out=KTall[:, :, u * 128:(u + 1) * 128],
                                                  in_=PT2[:, 0:512].rearrange("p (h t) -> p h t", h=4), func=AF.Copy),
               [PT2], [KTall])
        for (is_ctx, tiles) in supertiles(b, False):
            N = 128 * len(tiles)
            for si, ti in enumerate(tiles):
                sap, sunit = src_tile(l, b, is_ctx, ti)
                xt, hT = tile_prep(sap, sunit, b)
                proj(hT, Wp, 1024, 512, PB[0])
                op("act", lambda e: e.activation(out=qtm[:, 0:512], in_=PB[0][:, :], func=AF.Copy), [PB[0]], [qtm])
                rope_apply((qtm, 512), (qtm, 0), 8, ti, rt)
                op("act", lambda e: e.activation(out=qtb[:, 0:512], in_=qtm[:, 512:1024], func=AF.Copy), [qtm], [qtb])
                for h in range(4):
                    op("pe", lambda e, h=h: e.transpose(out=PT2[:, h * 128:(h + 1) * 128],
                                                        in_=qtb[:, h * 128:(h + 1) * 128], identity=ident_b[:]),
                       [qtb, ident_b], [PT2])
                op("act", lambda e, si=si: e.activation(out=QTst[:, 0:4, si * 128:(si + 1) * 128],
                                                        in_=PT2[:, 0:512].rearrange("p (h t) -> p h t", h=4),
                                                        func=AF.Copy), [PT2], [QTst])
            for h in range(4):
                for c in range(2):
                    accO, accD = (PB[2], PB[3]) if c == 0 else (PB[4], PB[5])
                    for u in range(NU):
                        sp_ = PB[u % 2]
                        op("pe", lambda e, u=u, sp_=sp_, c=c: e.matmul(
                            sp_[:, 0:N], lhsT=KTall[c * 64:(c + 1) * 64, h, u * 128:(u + 1) * 128],
                            rhs=QTst[c * 64:(c + 1) * 64, h, 0:N], start=True, stop=True), [KTall, QTst], [sp_])
                        pt = PTr[cnt["ptr"] % 3]
                        cnt["ptr"] += 1
                        op("act", lambda e, sp_=sp_, pt=pt: e.activation(out=pt[:, 0:N], in_=sp_[:, 0:N], func=AF.Exp,
                                                                         scale=0.125), [sp_], [pt])
                        op("pe", lambda e, u=u, pt=pt: e.matmul(
                            accO[:, 0:N], lhsT=Vall[:, u, h * 128:(h + 1) * 128], rhs=pt[:, 0:N],
                            start=(u == 0), stop=(u == NU - 1)), [Vall, pt], [accO])
                        op("pe", lambda e, u=u, pt=pt: e.matmul(
                            accD[:, 0:N], lhsT=ones_b[:], rhs=pt[:, 0:N],
                            start=(u == 0), stop=(u == NU - 1)), [ones_b, pt], [accD])
                op("dve", lambda e: e.reciprocal(out=rec[:, 0:N], in_=PB[3][:, 0:N]), [PB[3]], [rec])
                op("dve", lambda e: e.reciprocal(out=rec2[:, 0:N], in_=PB[5][:, 0:N]), [PB[5]], [rec2])
                op("dve", lambda e: e.tensor_tensor(out=ot1[:, 0:N], in0=PB[2][:, 0:N], in1=rec[:, 0:N], op=ALU.mult),
                   [PB[2], rec], [ot1])
                op("dve", lambda e: e.tensor_tensor(out=ot2[:, 0:N], in0=PB[4][:, 0:N], in1=rec2[:, 0:N], op=ALU.mult),
                   [PB[4], rec2], [ot2])
                op("dve", lambda e: e.scalar_tensor_tensor(out=ot1[:, 0:N], in0=ot2[:, 0:N], scalar=nl[:, 0:1],
                                                           in1=ot1[:, 0:N], op0=ALU.mult, op1=ALU.add),
                   [ot2, nl], [ot1])
                op("act", lambda e: e.activation(out=osq[:, 0:N], in_=ot1[:, 0:N], func=AF.Square), [ot1], [osq])
                op("pe", lambda e: e.matmul(PB[0][:, 0:N], lhsT=ones_b[:], rhs=osq[:, 0:N], start=True, stop=True),
                   [ones_b, osq], [PB[0]])
                op("dve", lambda e: e.tensor_scalar(out=rec[:, 0:N], in0=PB[0][:, 0:N], scalar1=1.0 / 128, scalar2=EPS,
                                                    op0=ALU.mult, op1=ALU.add), [PB[0]], [rec])
                op("act", lambda e: e.activation(out=rec[:, 0:N], in_=rec[:, 0:N], func=AF.Sqrt), [rec], [rec])
                op("dve", lambda e: e.reciprocal(out=rec[:, 0:N], in_=rec[:, 0:N]), [rec], [rec])
                op("dve", lambda e, h=h: e.scalar_tensor_tensor(out=mixT[:, h, 0:N], in0=ot1[:, 0:N], scalar=sg[:, 0:1],
                                                                in1=rec[:, 0:N], op0=ALU.mult, op1=ALU.mult),
                   [ot1, sg, rec], [mixT])
            out_proj_store(l, b, is_ctx, tiles, first, 4)

    pc = {}

    def peer_setup():
        nonlocal Wp
        Wp = k.sb([128, KT, 2048], BF16, "Wq")
        alloc_rows_peer()
        pc["keys"] = k.sb([128, 16, 128], BF16, "pkeys")
        scr = k.sb([128, 2048], F32, "pscr")
        pc["scr"] = scr
        pc["h2"] = k.sb([128, D], F32, "h2")
        pc["h2b"] = k.sb([128, D], BF16, "h2b")
        pc["h2T"] = k.sb([128, KT, 128], BF16, "h2T")
        pc["qT"] = k.sb([128, 16, 128], BF16, "pqT")
        pc["sc"] = k.sb([128, 16, 128], F32, "psc")
        pc["vals"] = k.sb([128, 16, 16], F32, "pvals")
        pc["idx"] = k.sb([128, 16, 16], U32, "pidx_")
        pc["idxf"] = k.sb([128, 16, 16], F32, "pidxf")
        pc["cand"] = k.sb([128, 8, 256], F32, "pcand")
        pc["best"] = k.sb([128, 8, 16], F32, "pbest")
        pc["pos"] = k.sb([128, 8, 16], U32, "ppos")
        pc["pa"] = k.sb([128, 8, 16], U32, "ppa")
        pc["pb"] = k.sb([128, 8, 16], U32, "ppb")
        pc["paf"] = k.sb([128, 8, 16], F32, "ppaf")
        pc["pbf"] = k.sb([128, 8, 16], F32, "ppbf")
        pc["isel"] = k.sb([128, 8, 16], F32, "pisel")
        pc["jsel"] = k.sb([128, 8, 16], F32, "pjsel")
        pc["eid"] = k.sb([128, 128], U32, "peid")
        pc["gate"] = k.sb([128, 8, 16], F32, "pgate")
        pc["z"] = k.sb([128, 8], F32, "pz")
        pc["dots"] = k.sb([128, 128], F32, "pdots")
        pc["wgt"] = k.sb([128, 128], F32, "pwgt")
        pc["acc"] = k.sb([128, D], F32, "pacc")
        pc["gb"] = [k.sb([128, 2 * D], BF16, "pgb%d" % i) for i in range(8)]
        pc["dring"] = [k.sb([128, 1], F32, "pdr%d" % i) for i in range(4)]
        pc["wring"] = [k.sb([128, 1], F32, "pwr%d" % i) for i in range(4)]
        pc["dring2"] = [k.sb([128, 1], F32, "pdr2%d" % i) for i in range(4)]
        pc["stg"] = [k.sb([128, D], F32, "pstg%d" % i) for i in range(2)]
        pc["junkb"] = k.sb([128, D], BF16, "pjunkb")
        pc["dg"] = [k.sb([128, 128], BF16, "pdg%d" % i) for i in range(3)]
        pc["fg"] = k.sb([128, D], F32, "pfg")
        dma("sp", pc["fg"][:], final_g.partition_broadcast(128), [uIN], [pc["fg"]])
        pc["gi"] = 0

    def peer_load(l):
        ci = 0
        for (src, c0) in ((peer_u[l], 0), (peer_v[l], D)):
            for r0 in range(0, 16384, 128):
                st_ = pc["stg"][ci % 2]
                gbf = pc["gb"][ci % 8]
                ci += 1
                dma("sp", st_[:], src[r0:r0 + 128, :], [uIN], [st_])
                op("act", lambda e: e.activation(out=gbf[:, 0:D], in_=st_[:], func=AF.Copy), [st_], [gbf])
                dma("act", UVB[l][r0:r0 + 128, c0:c0 + D], gbf[:, 0:D], [gbf], [uTAB])
        load_w(Wp, 0, peer_wq[l], 0, 2048)
        sv = pc["scr"][:, :].rearrange("p (j n) -> p j n", j=16)
        dma("sp", sv, keysT[l], [uIN], [pc["scr"]])
        op("pool", lambda e: e.tensor_copy(out=pc["keys"][:], in_=sv), [pc["scr"]], [pc["keys"]])

    def top16(src, src2, vals_ap, idx_ap):
        (sbuf_, sap), (s2buf, s2ap) = src, src2
        (vb, vf), (ib, if_) = vals_ap, idx_ap
        op("dve", lambda e: e.max(out=vf(0, 8), in_=sap), [sbuf_], [vb])
        op("dve", lambda e: e.max_index(out=if_(0, 8), in_max=vf(0, 8), in_values=sap), [vb, sbuf_], [ib])
        op("dve", lambda e: e.match_replace(out=s2ap, in_to_replace=vf(0, 8), in_values=sap, imm_value=NEG),
           [vb, sbuf_], [s2buf])
        op("dve", lambda e: e.max(out=vf(8, 16), in_=s2ap), [s2buf], [vb])
        op("dve", lambda e: e.max_index(out=if_(8, 16), in_max=vf(8, 16), in_values=s2ap), [vb, s2buf], [ib])

    def peer_tile(l, b, is_ctx, ti, final):
        p = pc
        slot = 1 if is_ctx else 0
        scr_j = p["scr"][:, :].rearrange("p (j n) -> p j n", j=16)
        scr_h = p["scr"][:, :].rearrange("p (h c) -> p h c", h=8)
        if l == 0:
            sap, sunit = dst_tile(0, b, is_ctx, ti)
        else:
            sap, sunit = dst_tile(1, b, is_ctx, ti)
        xt = xt_ring[cnt["xt"] % 2]
        cnt["xt"] += 1
        dma("sp", xt[:], sap, [sunit], [xt])
        rstd_of(xt, 1)
        h2, h2b, h2T = p["h2"], p["h2b"], p["h2T"]
        op("dve", lambda e: e.scalar_tensor_tensor(out=h2[:], in0=xt[:], scalar=st4[:, 1:2], in1=rows["G2"][slot][:],
                                                   op0=ALU.mult, op1=ALU.mult), [xt, st4, rows["G2"][slot]], [h2])
        op("dve", lambda e: e.tensor_tensor(out=h2[:], in0=h2[:], in1=rows["SH2"][slot][:], op=ALU.add),
           [rows["SH2"][slot]], [h2])
        op("act", lambda e: e.activation(out=h2b[:], in_=h2[:], func=AF.Copy), [h2], [h2b])
        for kk in range(KT):
            op("pe", lambda e, kk=kk: e.transpose(out=PT_[:, kk * 128:(kk + 1) * 128],
                                                  in_=h2b[:, kk * 128:(kk + 1) * 128], identity=ident_b[:]),
               [h2b, ident_b], [PT_])
        op("act", lambda e: e.activation(out=h2T[:], in_=PT_[:, :].rearrange("p (k t) -> p k t", k=KT), func=AF.Copy),
           [PT_], [h2T])
        for j in range(16):
            pbk = PB[j // 4]
            for kk in range(KT):
                op("pe", lambda e, j=j, kk=kk, pbk=pbk: e.matmul(
                    pbk[:, (j % 4) * 128:(j % 4 + 1) * 128], lhsT=Wp[:, kk, j * 128:(j + 1) * 128], rhs=h2T[:, kk, :],
                    start=(kk == 0), stop=(kk == KT - 1)), [Wp, h2T], [pbk])
            if j % 4 == 3:
                g = j // 4
                op("act", lambda e, g=g, pbk=pbk: e.activation(
                    out=p["qT"][:, g * 4:(g + 1) * 4, :], in_=pbk[:, :].rearrange("p (j t) -> p j t", j=4),
                    func=AF.Copy), [pbk], [p["qT"]])
        for j in range(16):
            pbk = PB[j // 4]
            op("pe", lambda e, j=j, pbk=pbk: e.matmul(pbk[:, (j % 4) * 128:(j % 4 + 1) * 128], lhsT=p["qT"][:, j, :],
                                                      rhs=p["keys"][:, j, :], start=True, stop=True),
               [p["qT"], p["keys"]], [pbk])
            if j % 4 == 3:
                g = j // 4
                op("act", lambda e, g=g, pbk=pbk: e.activation(
                    out=p["sc"][:, g * 4:(g + 1) * 4, :], in_=pbk[:, :].rearrange("p (j n) -> p j n", j=4),
                    func=AF.Copy), [pbk], [p["sc"]])
        for j in range(16):
            top16((p["sc"], p["sc"][:, j, :]), (p["scr"], scr_j[:, j, :]),
                  (p["vals"], lambda a, c, j=j: p["vals"][:, j, a:c]), (p["idx"], lambda a, c, j=j: p["idx"][:, j, a:c]))
        op("dve", lambda e: e.tensor_copy(out=p["idxf"][:], in_=p["idx"][:]), [p["idx"]], [p["idxf"]])
        v4 = p["vals"][:, :, :].rearrange("p (h two) k -> p h two k", two=2)
        i4 = p["idxf"][:, :, :].rearrange("p (h two) k -> p h two k", two=2)
        c4 = p["cand"][:, :, :].rearrange("p h (a c) -> p h a c", a=16)
        op("dve", lambda e: e.tensor_tensor(out=c4, in0=bc(v4[:, :, 0, :].unsqueeze(3), [128, 8, 16, 16]),
                                            in1=bc(v4[:, :, 1, :].unsqueeze(2), [128, 8, 16, 16]), op=ALU.add),
           [p["vals"]], [p["cand"]])
        for h in range(8):
            top16((p["cand"], p["cand"][:, h, :]), (p["scr"], scr_h[:, h, :]),
                  (p["best"], lambda a, c, h=h: p["best"][:, h, a:c]), (p["pos"], lambda a, c, h=h: p["pos"][:, h, a:c]))
        op("dve", lambda e: e.tensor_single_scalar(out=p["pa"][:], in_=p["pos"][:], scalar=4,
                                                   op=ALU.logical_shift_right), [p["pos"]], [p["pa"]])
        op("dve", lambda e: e.tensor_single_scalar(out=p["pb"][:], in_=p["pos"][:], scalar=15, op=ALU.bitwise_and),
           [p["pos"]], [p["pb"]])
        op("dve", lambda e: e.tensor_copy(out=p["paf"][:], in_=p["pa"][:]), [p["pa"]], [p["paf"]])
        op("dve", lambda e: e.tensor_copy(out=p["pbf"][:], in_=p["pb"][:]), [p["pb"]], [p["pbf"]])
        e4 = p["scr"][:, :].rearrange("p (h k a) -> p h k a", h=8, k=16)
        io4 = bc(iota16[:, :].unsqueeze(1).unsqueeze(1), [128, 8, 16, 16])
        for (pf, half, dstb) in ((p["paf"], 0, p["isel"]), (p["pbf"], 1, p["jsel"])):
            op("dve", lambda e, pf=pf: e.tensor_tensor(out=e4, in0=bc(pf[:, :, :].unsqueeze(3), [128, 8, 16, 16]),
                                                       in1=io4, op=ALU.is_equal), [pf, iota16], [p["scr"]])
            op("dve", lambda e, half=half: e.tensor_tensor(out=e4, in0=e4,
                                                           in1=bc(i4[:, :, half, :].unsqueeze(2), [128, 8, 16, 16]),
                                                           op=ALU.mult), [p["idxf"]], [p["scr"]])
            op("dve", lambda e, dstb=dstb: e.tensor_reduce(out=dstb[:], in_=e4, axis=AX.X, op=ALU.add),
               [p["scr"]], [dstb])
        op("dve", lambda e: e.scalar_tensor_tensor(out=p["isel"][:], in0=p["isel"][:], scalar=128.0, in1=p["jsel"][:],
                                                   op0=ALU.mult, op1=ALU.add), [p["jsel"]], [p["isel"]])
        op("dve", lambda e: e.tensor_copy(out=p["eid"][:, :].rearrange("p (h k) -> p h k", h=8), in_=p["isel"][:]),
           [p["isel"]], [p["eid"]])
        op("dve", lambda e: e.tensor_tensor(out=p["gate"][:], in0=p["best"][:],
                                            in1=bc(p["best"][:, :, 0:1], [128, 8, 16]), op=ALU.subtract),
           [p["best"]], [p["gate"]])
        op("act", lambda e: e.activation(out=p["gate"][:], in_=p["gate"][:], func=AF.Exp), [p["gate"]], [p["gate"]])
        op("dve", lambda e: e.tensor_reduce(out=p["z"][:], in_=p["gate"][:], axis=AX.X, op=ALU.add),
           [p["gate"]], [p["z"]])
        op("dve", lambda e: e.reciprocal(out=p["z"][:], in_=p["z"][:]), [p["z"]], [p["z"]])
        op("dve", lambda e: e.tensor_tensor(out=p["gate"][:], in0=p["gate"][:],
                                            in1=bc(p["z"][:, :].unsqueeze(2), [128, 8, 16]), op=ALU.mult),
           [p["z"]], [p["gate"]])
        acc = p["acc"]
        gflat = p["gate"][:, :, :].rearrange("p h k -> p (h k)")
        pend = None

        def finish_slot(ps):
            s_, gb_, wr_ = ps
            dg = p["dg"][s_ % 3]
            op("dve", lambda e: e.tensor_scalar(out=dg[:], in0=ident_f[:], scalar1=wr_[:, 0:1],
                                                scalar2=gflat[:, s_:s_ + 1], op0=ALU.mult, op1=ALU.mult),
               [ident_f, wr_, p["gate"]], [dg])
            for half in range(2):
                op("pe", lambda e, half=half: e.matmul(
                    PB[4 + half][:, :], lhsT=dg[:], rhs=gb_[:, D + half * 512:D + (half + 1) * 512],
                    start=(s_ == 0), stop=(s_ == 127)), [dg, gb_], [PB[4 + half]])

        for s in range(128):
            gbuf = p["gb"][p["gi"] % 8]
            dr = p["dring"][p["gi"] % 4]
            wr = p["wring"][p["gi"] % 4]
            p["gi"] += 1
            dma("pool", gbuf[:], UVB[l], [uTAB, p["eid"]], [gbuf], indirect=p["eid"][:, s:s + 1])
            op("dve", lambda e: e.scalar_tensor_tensor(
                out=p["junkb"][:], in0=gbuf[:, 0:D], scalar=1.0, in1=h2b[:], op0=ALU.mult, op1=ALU.mult,
                accum_out=dr[:, 0:1]), [gbuf, h2b], [p["junkb"], dr])
            dr2 = p["dring2"][(p["gi"] - 1) % 4]
            op("dve", lambda e: e.tensor_copy(out=dr2[:, 0:1], in_=dr[:, 0:1]), [dr], [dr2])
            op("act", lambda e: e.activation(out=wr[:, 0:1], in_=dr2[:, 0:1], func=AF.Gelu), [dr2], [wr])
            if pend is not None:
                finish_slot(pend)
            pend = (s, gbuf, wr)
        finish_slot(pend)
        for half in range(2):
            op("dve", lambda e, half=half: e.tensor_tensor(out=acc[:, half * 512:(half + 1) * 512],
                                                           in0=PB[4 + half][:, :],
                                                           in1=rows["g2"][slot][:, half * 512:(half + 1) * 512],
                                                           op=ALU.mult), [PB[4 + half], rows["g2"][slot]], [acc])
        op("dve", lambda e: e.tensor_tensor(out=ynew[:], in0=acc[:], in1=xt[:], op=ALU.add), [acc, xt], [ynew])
        if not final:
            dma("sp", sap, ynew[:], [ynew], [sunit])
        else:
            rstd_of(ynew, 2)
            op("dve", lambda e: e.scalar_tensor_tensor(out=ynew[:], in0=ynew[:], scalar=st4[:, 2:3], in1=p["fg"][:],
                                                       op0=ALU.mult, op1=ALU.mult), [st4, p["fg"]], [ynew])
            dma("sp", out_d[b, ti * 128:(ti + 1) * 128, :], ynew[:], [ynew], [uOUT])


    def dump(srcu, src, is_final=False):
        for b in range(NB):
            for ti in range(NT):
                xt = xt_ring[cnt["xt"] % 2]
                cnt["xt"] += 1
                dma("sp", xt[:], src[b, ti * 128:(ti + 1) * 128, :], [srcu], [xt])
                dma("sp", out_d[b, ti * 128:(ti + 1) * 128, :], xt[:], [xt], [uOUT])

    done = False
    if stop_after == "const":
        dump(uIN, x_in)
        done = True
    if not done:
        mod_phase(0)
        if stop_after == "mod":
            dump(uIN, x_in)
            done = True
    if not done:
        for b in range(NB):
            if stop_after != "ret0":
                gqa_pass(b, True)
            if stop_after != "gqa0":
                ret_pass(b, stop_after == "ret0")
        if stop_after in ("mix0", "ret0", "gqa0", "ret_setup", "ret_p1", "ret_scan", "ret_q", "ret_o", "ret_m", "ret_qa", "ret_qb"):
            dump(uXSA, XSA)
            done = True
    if not done:
        with k.scope():
            peer_setup()
            peer_load(0)
            for b in range(NB):
                load_rows_peer(0, b, 0)
                load_rows_peer(0, NB, 1)
                for (is_ctx, ti) in tiles_of(b):
                    peer_tile(0, b, is_ctx, ti, False)
        if stop_after == "peer0":
            dump(uXSA, XSA)
            done = True
    if not done:
        mod_phase(1)
        for b in range(NB):
            diff_pass(b, 0, True)
            diff_pass(b, 1, False)
        if stop_after == "mix1":
            dump(uXSB, XSB)
            done = True
    if not done:
        with k.scope():
            peer_setup()
            peer_load(1)
            for b in range(NB):
                load_rows_peer(1, b, 0)
                for ti in range(NT):
                    peer_tile(1, b, False, ti, True)
    k.finish([uOUT])
    es.close()
    return nc


def host_consts(S):
    NT = S // 128
    GRID_W = 64
    t = np.arange(S)
    row = (t // GRID_W).astype(np.float32)
    col = (t % GRID_W).astype(np.float32)
    inv = (10000.0 ** (-np.arange(0, 32, 2, dtype=np.float32) / 32.0)).astype(np.float32)
    ar = row[:, None] * inv[None, :]
    ac = col[:, None] * inv[None, :]
    tab = np.concatenate([np.cos(ar), np.sin(ar), np.cos(ac), np.sin(ac)], axis=1).astype(np.float32)
    rope = np.ascontiguousarray(tab.reshape(NT, 128, 64).transpose(1, 0, 2))
    ident = np.eye(128, dtype=np.float32)
    iota16 = np.broadcast_to(np.arange(16, dtype=np.float32)[None, :], (128, 16)).copy()
    j = np.arange(128, dtype=np.float32)
    relm = (j[None, :] - j[:, None]).astype(np.float32)
    pidx = np.stack([127.0 - j, j], axis=1).astype(np.float32)
    fidx = np.concatenate([np.broadcast_to((j + 1.0)[None, :], (64, 128)),
                           np.broadcast_to((128.0 - j)[None, :], (64, 128))], axis=0).astype(np.float32).copy()
    return dict(rope=rope, ident=ident, iota16=iota16, relm=relm, pidx=pidx, fidx=fidx)


def make_in_maps(inp, NB, n_cores):
    f = lambda a: np.ascontiguousarray(np.asarray(a, dtype=np.float32))
    x, c, ctx, c_ctx = f(inp["x"]), f(inp["c"]), f(inp["ctx"]), f(inp["c_ctx"])
    S = x.shape[1]
    shared = dict(
        ada_w=f(inp["ada_w"]), ada_b=f(inp["ada_b"]),
        ada_bT=np.ascontiguousarray(f(inp["ada_b"]).reshape(2, 48, 128).transpose(0, 2, 1)),
        n1gT=np.ascontiguousarray(f(inp["norm1_g"]).reshape(2, KT, 128).transpose(0, 2, 1)),
        n2g=f(inp["norm2_g"]), final_g=f(inp["final_g"]),
        ev_w_in=f(inp["ev_w_in"])[0], ev_w_out=f(inp["ev_w_out"])[0],
        od_w_in=f(inp["od_w_in"])[0], od_w_out=f(inp["od_w_out"])[0],
        qg=f(inp["gqa_q_norm_g"])[0], kg=f(inp["gqa_k_norm_g"])[0],
        rate=f(inp["ret_log_rate"]).reshape(8), lam=f(inp["diff_lambda"]).reshape(256),
        subln=f(inp["diff_subln_g"]).reshape(128),
        peer_wq=f(inp["peer_w_q"]),
        keysT=np.ascontiguousarray(f(inp["peer_keys"]).reshape(2, 16, 128, 128).transpose(0, 3, 1, 2)),
        peer_u0=f(inp["peer_u"][0]), peer_u1=f(inp["peer_u"][1]),
        peer_v0=f(inp["peer_v"][0]), peer_v1=f(inp["peer_v"][1]),
    )
    shared.update(host_consts(S))
    maps = []
    for i in range(n_cores):
        bs = slice(i * NB, (i + 1) * NB)
        cv = np.concatenate([c[bs], c_ctx[None, :]], axis=0)
        cT = np.ascontiguousarray(cv.reshape(NB + 1, KT, 128).transpose(2, 1, 0))
        m = dict(shared)
        m.update(x=np.ascontiguousarray(x[bs]), ctx=np.ascontiguousarray(ctx[bs]), cT=cT)
        maps.append(m)
    return maps


def kernel(**inputs):
    n_cores = 8
    B, S, _ = inputs["x"].shape
    LC = inputs["ctx"].shape[1]
    NB = B // n_cores
    nc = build(NB, S, LC)
    maps = make_in_maps(inputs, NB, n_cores)
    res = run_bass_kernel_spmd(nc, maps, core_ids=list(range(n_cores)))
    return np.concatenate([r["out"] for r in res.results], axis=0).astype(np.float32)
```

```python
import math
from contextlib import ExitStack

import numpy as np
import concourse.bass as bass
import concourse.mybir as mybir
from concourse.bass_utils import run_bass_kernel_spmd

F32 = mybir.dt.float32
BF16 = mybir.dt.bfloat16
U32 = mybir.dt.uint32
AF = mybir.ActivationFunctionType
ALU = mybir.AluOpType
AX = mybir.AxisListType

D = 1024
KT = 8
EPS = 1e-6
NEG = -1.0e30


class Buf:
    __slots__ = ("t", "w", "r")

    def __init__(self, t=None):
        self.t = t
        self.w = None
        self.r = {}

    def __getitem__(self, k):
        return self.t[k]


class KB:
    RING = 6

    def __init__(self, nc, es):
        self.nc = nc
        self.es = es
        self.h = {"pe": nc.tensor, "act": nc.scalar, "dve": nc.vector, "pool": nc.gpsimd, "sp": nc.sync}
        self.sem = {}
        self.cnt = {}
        self.seen = {}
        self.ring = {}
        for e in self.h:
            self.sem[e] = es.enter_context(nc.semaphore("s_" + e))
            self.cnt[e] = 0
            self.seen[e] = {}
        self.nsem = 0
        self.uid = 0
        self.cur = es

    def _ring(self, q):
        if q not in self.ring:
            sems = [self.es.enter_context(self.nc.semaphore("d_%s_%d" % (q, i))) for i in range(self.RING)]
            self.ring[q] = {"sems": sems, "vals": [0] * self.RING, "i": 0}
        return self.ring[q]

    def sb(self, shape, dt, name=None):
        self.uid += 1
        nm = "%s_%d" % (name or "sb", self.uid)
        return Buf(self.cur.enter_context(self.nc.sbuf_tensor(nm, list(shape), dt)))

    def barrier(self):
        for e in self.h:
            for o in self.h:
                if o != e and self.cnt[o] > 0:
                    self._wait(e, (self.sem[o], self.cnt[o], o, o))
            for q, ring in self.ring.items():
                for j in range(self.RING):
                    if ring["vals"][j] > 0:
                        self._wait(e, (ring["sems"][j], ring["vals"][j], None, "d_%s_%d" % (q, j)))

    def scope(self):
        kb = self

        class _S:
            def __enter__(s_):
                s_.prev = kb.cur
                s_.st = ExitStack()
                kb.cur = s_.st
                return s_

            def __exit__(s_, *a):
                kb.barrier()
                s_.st.close()
                kb.cur = s_.prev
                return False
        return _S()

    def ps(self, shape, dt, name=None):
        self.uid += 1
        return Buf(self.es.enter_context(self.nc.psum_tensor(name or ("ps%d" % self.uid), list(shape), dt)))

    def _wait(self, eng, tok):
        if tok is None:
            return
        sem, val, owner, key = tok
        if owner == "pe" and eng == "pe":
            return
        if self.seen[eng].get(key, 0) >= val:
            return
        self.h[eng].wait_ge(sem, val)
        self.seen[eng][key] = val

    def _deps(self, eng, reads, writes):
        for b in reads:
            self._wait(eng, b.w)
        for b in writes:
            self._wait(eng, b.w)
            for t in b.r.values():
                self._wait(eng, t)

    def op(self, eng, fn, reads=(), writes=()):
        self._deps(eng, reads, writes)
        ins = fn(self.h[eng])
        self.cnt[eng] += 1
        ins.then_inc(self.sem[eng], 1)
        tok = (self.sem[eng], self.cnt[eng], eng, eng)
        for b in reads:
            b.r[eng] = tok
        for b in writes:
            b.w = tok
            b.r = {}
        return tok

    def dma(self, q, out, in_, reads=(), writes=(), indirect=None, **kw):
        self._deps(q, reads, writes)
        ring = self._ring(q)
        j = ring["i"] % self.RING
        ring["i"] += 1
        sem = ring["sems"][j]
        prev = ring["vals"][j]
        key = "d_%s_%d" % (q, j)
        if prev > 0:
            self._wait(q, (sem, prev, None, key))
        if indirect is None:
            ins = self.h[q].dma_start(out=out, in_=in_, **kw)
        else:
            ins = self.h[q].indirect_dma_start(out=out, out_offset=None, in_=in_,
                                               in_offset=bass.IndirectOffsetOnAxis(ap=indirect, axis=0),
                                               bounds_check=self.bnd_reg(), oob_is_err=False)
        ins.then_inc(sem, 16)
        ring["vals"][j] = prev + 16
        tok = (sem, prev + 16, None, key)
        for b in reads:
            b.r[key] = tok
        for b in writes:
            b.w = tok
            b.r = {}
        return tok

    def bnd_reg(self):
        if getattr(self, "_bnd", None) is None:
            self._bnd = self.nc.gpsimd.alloc_register("bnd")
            self.nc.gpsimd.reg_mov(self._bnd, 16383)
        return self._bnd

    def finish(self, bufs):
        for b in bufs:
            self._wait("sp", b.w)


def bc(ap, shape):
    return ap.to_broadcast(list(shape))


def build(NB, S, LC, stop_after=None, dbg=False):
    NT = S // 128
    NCX = LC // 128
    NU = NCX + NT
    R = NB + 1
    nc = bass.Bass("TRN2", target_bir_lowering=False)

    def din(name, shape, dt=F32):
        return nc.dram_tensor(name, list(shape), dt, kind="ExternalInput").ap()

    x_in = din("x", [NB, S, D])
    ctx_in = din("ctx", [NB, LC, D])
    cT_in = din("cT", [128, KT, R])
    ada_w = din("ada_w", [2, D, 6 * D])
    ada_b = din("ada_b", [2, 6 * D])
    ada_bT = din("ada_bT", [2, 128, 48])
    n1gT = din("n1gT", [2, 128, KT])
    n2g = din("n2g", [2, D])
    final_g = din("final_g", [D])
    ev_w_in = din("ev_w_in", [D, 2304])
    ev_w_out = din("ev_w_out", [D, D])
    od_w_in = din("od_w_in", [D, 3072])
    od_w_out = din("od_w_out", [D, D])
    qg_in = din("qg", [64])
    kg_in = din("kg", [64])
    rate_in = din("rate", [8])
    lam_in = din("lam", [256])
    subln_in = din("subln", [128])
    peer_wq = din("peer_wq", [2, D, 2048])
    keysT = din("keysT", [2, 128, 16, 128])
    peer_u = [din("peer_u%d" % i, [16384, D]) for i in range(2)]
    peer_v = [din("peer_v%d" % i, [16384, D]) for i in range(2)]
    ident_in = din("ident", [128, 128])
    rope_in = din("rope", [128, NT, 64])
    iota_in = din("iota16", [128, 16])
    relm_in = din("relm", [128, 128])
    pidx_in = din("pidx", [128, 2])
    fidx_in = din("fidx", [128, 128])
    out_d = nc.dram_tensor("out", [NB, S, D], F32, kind="ExternalOutput").ap()

    def dscr(name, shape, dt=F32):
        return nc.dram_tensor(name, list(shape), dt, kind="Internal").ap()

    XSA = dscr("XSA", [NB, S, D])
    XCA = dscr("XCA", [NB, LC, D])
    XSB = dscr("XSB", [NB, S, D])
    MODR = dscr("MODR", [2, R, 6 * D])
    KVD = dscr("KVD", [NU, 128, 4, 128])
    STD = dscr("STD", [NU, 128, 4, 128], BF16)
    UVB = [dscr("UVB%d" % i, [16384, 2 * D], BF16) for i in range(2)]
    uTAB = Buf()

    es = ExitStack()
    k = KB(nc, es)
    op, dma = k.op, k.dma
    uXSA, uXCA, uXSB, uMODR, uKVD, uSTD, uOUT = (Buf() for _ in range(7))
    uIN = Buf()

    ident_f = k.sb([128, 128], F32, "ident_f")
    ident_b = k.sb([128, 128], BF16, "ident_b")
    ones_b = k.sb([128, 128], BF16, "ones_b")
    iota16 = k.sb([128, 16], F32, "iota16")
    rope = k.sb([128, NT, 64], F32, "rope")
    dma("sp", ident_f[:], ident_in, [uIN], [ident_f])
    dma("sp", iota16[:], iota_in, [uIN], [iota16])
    dma("sp", rope[:], rope_in, [uIN], [rope])
    op("dve", lambda e: e.tensor_copy(out=ident_b[:], in_=ident_f[:]), [ident_f], [ident_b])
    op("dve", lambda e: e.memset(ones_b[:], 1.0), [], [ones_b])

    PB = [k.ps([128, 512], F32, "pb%d" % i) for i in range(6)]
    PT_ = k.ps([128, 1024], BF16, "ptb")
    PT2 = k.ps([128, 1024], BF16, "ptb2")

    sc_silu = k.sb([128, KT, R], F32, "sc_silu")
    dma("sp", sc_silu[:], cT_in, [uIN], [sc_silu])
    op("act", lambda e: e.activation(out=sc_silu[:], in_=sc_silu[:], func=AF.Silu), [sc_silu], [sc_silu])
    A1 = k.sb([128, R, KT], F32, "A1")
    B1 = k.sb([128, R, KT], F32, "B1")
    modT = k.sb([128, 16, R], F32, "modT")
    n1g_sb = k.sb([128, KT], F32, "n1g_sb")
    abT_sb = k.sb([128, 48], F32, "abT_sb")
    rows = {}

    xt_ring = [k.sb([128, D], F32, "xt%d" % i) for i in range(2)]
    xn_b = k.sb([128, D], BF16, "xn_b")
    junk = k.sb([128, D], F32, "junk")
    st4 = k.sb([128, 8], F32, "st4")
    hT_ring = [k.sb([128, KT, 128], BF16, "hT%d" % i) for i in range(2)]
    CW = 256
    wstage = [k.sb([128, KT, CW], F32, "wst%d" % i) for i in range(2)]
    cnt = {"xt": 0, "hT": 0, "ws": 0}

    def load_w(dst, col0, src_ap, c0, n):
        off = 0
        while off < n:
            m = min(CW, n - off)
            ws = wstage[cnt["ws"] % 2]
            cnt["ws"] += 1
            dma("sp", ws[:, :, 0:m], src_ap[:, c0 + off:c0 + off + m].rearrange("(k p) n -> p k n", p=128),
                [uIN], [ws])
            op("pool", lambda e, ws=ws, m=m, o=off: e.tensor_copy(out=dst[:, :, col0 + o:col0 + o + m],
                                                               in_=ws[:, :, 0:m]), [ws], [dst])
            off += m

    def rstd_of(xt, col):
        op("act", lambda e: e.activation(out=junk[:], in_=xt[:], func=AF.Square, accum_out=st4[:, col:col + 1]),
           [xt], [junk, st4])
        op("dve", lambda e: e.tensor_scalar(out=st4[:, col:col + 1], in0=st4[:, col:col + 1], scalar1=1.0 / D,
                                            scalar2=EPS, op0=ALU.mult, op1=ALU.add), [st4], [st4])
        op("act", lambda e: e.activation(out=st4[:, col:col + 1], in_=st4[:, col:col + 1], func=AF.Sqrt),
           [st4], [st4])
        op("dve", lambda e: e.reciprocal(out=st4[:, col:col + 1], in_=st4[:, col:col + 1]), [st4], [st4])

    def tile_prep(src_ap, src_unit, ri):
        xt = xt_ring[cnt["xt"] % 2]
        cnt["xt"] += 1
        hT = hT_ring[cnt["hT"] % 2]
        cnt["hT"] += 1
        dma("sp", xt[:], src_ap, [src_unit], [xt])
        rstd_of(xt, 0)
        op("dve", lambda e: e.tensor_scalar(out=xn_b[:], in0=xt[:], scalar1=st4[:, 0:1], scalar2=None,
                                            op0=ALU.mult), [xt, st4], [xn_b])
        for kk in range(KT):
            op("pe", lambda e, kk=kk: e.transpose(out=PT_[:, kk * 128:(kk + 1) * 128],
                                                  in_=xn_b[:, kk * 128:(kk + 1) * 128], identity=ident_b[:]),
               [xn_b, ident_b], [PT_])
        ptv = PT_[:, :].rearrange("p (k t) -> p k t", k=KT)
        op("dve", lambda e: e.tensor_tensor(out=hT[:], in0=ptv, in1=bc(A1[:, ri, :].unsqueeze(2), [128, KT, 128]),
                                            op=ALU.mult), [PT_, A1], [hT])
        op("dve", lambda e: e.tensor_tensor(out=hT[:], in0=hT[:], in1=bc(B1[:, ri, :].unsqueeze(2), [128, KT, 128]),
                                            op=ALU.add), [B1], [hT])
        return xt, hT

    def proj(hT, W, c0, n, pbank, poff=0):
        for kk in range(KT):
            op("pe", lambda e, kk=kk: e.matmul(pbank[:, poff:poff + n], lhsT=hT[:, kk, :], rhs=W[:, kk, c0:c0 + n],
                                               start=(kk == 0), stop=(kk == KT - 1)), [hT, W], [pbank])

    def rope_apply(dst, src, nh, ti, tmp):
        (db, d0), (sbf, s0) = dst, src
        sv = sbf[:, s0:s0 + nh * 64].rearrange("p (h a b c) -> p h a b c", h=nh, a=2, b=2)
        dv = db[:, d0:d0 + nh * 64].rearrange("p (h a b c) -> p h a b c", h=nh, a=2, b=2)
        t1 = tmp[0][:, 0:nh * 32].rearrange("p (h a c) -> p h a c", h=nh, a=2)
        t2 = tmp[1][:, 0:nh * 32].rearrange("p (h a c) -> p h a c", h=nh, a=2)
        rv = rope[:, ti, :].rearrange("p (a b c) -> p a b c", a=2, b=2)
        cosb = bc(rv[:, :, 0, :].unsqueeze(1), [128, nh, 2, 16])
        sinb = bc(rv[:, :, 1, :].unsqueeze(1), [128, nh, 2, 16])
        u1 = sv[:, :, :, 0, :]
        u2 = sv[:, :, :, 1, :]
        op("dve", lambda e: e.tensor_tensor(out=t1, in0=u1, in1=cosb, op=ALU.mult), [sbf, rope], [tmp[0]])
        op("dve", lambda e: e.tensor_tensor(out=t2, in0=u2, in1=sinb, op=ALU.mult), [sbf, rope], [tmp[1]])
        op("dve", lambda e: e.tensor_tensor(out=dv[:, :, :, 0, :], in0=t1, in1=t2, op=ALU.subtract),
           [tmp[0], tmp[1]], [db])
        op("dve", lambda e: e.tensor_tensor(out=t1, in0=u1, in1=sinb, op=ALU.mult), [sbf, rope], [tmp[0]])
        op("dve", lambda e: e.tensor_tensor(out=t2, in0=u2, in1=cosb, op=ALU.mult), [sbf, rope], [tmp[1]])
        op("dve", lambda e: e.tensor_tensor(out=dv[:, :, :, 1, :], in0=t1, in1=t2, op=ALU.add),
           [tmp[0], tmp[1]], [db])

    def mod_phase(l):
      with k.scope():
        modrow_sb = k.sb([R, 6 * D], F32, "modrow_sb")
        adab_rows = k.sb([R, 6 * D], F32, "adab_rows")
        dma("sp", adab_rows[:], ada_b[l, :].partition_broadcast(R), [uIN], [adab_rows])
        dma("sp", abT_sb[:], ada_bT[l], [uIN], [abT_sb])
        dma("sp", n1g_sb[:], n1gT[l], [uIN], [n1g_sb])
        pm, pt = PB[0], PB[1]
        for cc in range(6 * D // CW):
            ws = wstage[cnt["ws"] % 2]
            cnt["ws"] += 1
            dma("sp", ws[:], ada_w[l, :, cc * CW:(cc + 1) * CW].rearrange("(k p) n -> p k n", p=128), [uIN], [ws])
            for kk in range(KT):
                op("pe", lambda e, kk=kk: e.matmul(pm[0:R, 0:CW], lhsT=sc_silu[:, kk, :], rhs=ws[:, kk, :],
                                                   start=(kk == 0), stop=(kk == KT - 1)), [sc_silu, ws], [pm])
            op("dve", lambda e, cc=cc: e.tensor_tensor(out=modrow_sb[:, cc * CW:(cc + 1) * CW], in0=pm[0:R, 0:CW],
                                                       in1=adab_rows[:, cc * CW:(cc + 1) * CW], op=ALU.add),
               [pm, adab_rows], [modrow_sb])
            if cc < 2048 // CW:
                for jj in range(CW // 128):
                    j = cc * (CW // 128) + jj
                    for kk in range(KT):
                        op("pe", lambda e, kk=kk, jj=jj, j=j: e.matmul(
                            pt[:, j * R:(j + 1) * R], lhsT=ws[:, kk, jj * 128:(jj + 1) * 128], rhs=sc_silu[:, kk, :],
                            start=(kk == 0), stop=(kk == KT - 1)), [sc_silu, ws], [pt])
        op("dve", lambda e: e.tensor_tensor(out=modT[:], in0=pt[:, 0:16 * R].rearrange("p (j r) -> p j r", r=R),
                                            in1=bc(abT_sb[:, 0:16].unsqueeze(2), [128, 16, R]), op=ALU.add),
           [pt, abT_sb], [modT])
        for r in range(R):
            op("dve", lambda e, r=r: e.scalar_tensor_tensor(out=A1[:, r, :], in0=modT[:, 8:16, r], scalar=1.0,
                                                            in1=n1g_sb[:], op0=ALU.add, op1=ALU.mult),
               [modT, n1g_sb], [A1])
            op("dve", lambda e, r=r: e.tensor_copy(out=B1[:, r, :], in_=modT[:, 0:8, r]), [modT], [B1])
        dma("sp", MODR[l], modrow_sb[:], [modrow_sb], [uMODR])

    def alloc_rows_attn():
        rows["G1"] = [k.sb([128, D], F32, "rowG1_%d" % i) for i in range(2)]

    def alloc_rows_peer():
        rows["G2"] = [k.sb([128, D], F32, "rowG2_%d" % i) for i in range(2)]
        rows["SH2"] = [k.sb([128, D], F32, "rowSH2_%d" % i) for i in range(2)]
        rows["g2"] = [k.sb([128, D], F32, "rowg2_%d" % i) for i in range(2)]

    def load_rows_attn(l, r, slot):
        dma("sp", rows["G1"][slot][:], MODR[l, r, 2 * D:3 * D].partition_broadcast(128), [uMODR], [rows["G1"][slot]])

    def load_rows_peer(l, r, slot):
        def row(c0):
            return MODR[l, r, c0:c0 + D].partition_broadcast(128)
        rowG2, rowSH2, rowg2 = rows["G2"], rows["SH2"], rows["g2"]
        n2g_row = junk
        dma("sp", n2g_row[:], n2g[l, :].partition_broadcast(128), [uIN], [n2g_row])
        dma("sp", rowSH2[slot][:], row(3 * D), [uMODR], [rowSH2[slot]])
        dma("sp", rowG2[slot][:], row(4 * D), [uMODR], [rowG2[slot]])
        dma("sp", rowg2[slot][:], row(5 * D), [uMODR], [rowg2[slot]])
        op("dve", lambda e: e.scalar_tensor_tensor(out=rowG2[slot][:], in0=rowG2[slot][:], scalar=1.0,
                                                   in1=n2g_row[:], op0=ALU.add, op1=ALU.mult),
           [n2g_row], [rowG2[slot]])

    ynew = k.sb([128, D], F32, "ynew")
    KTall = Vall = Wp = Wo = QTst = mixT = basex = PTr = qtm = qtb = rt = sq8 = None
    rec = rec2 = ot1 = ot2 = osq = qg_row = kg_row = None
    cnt["ptr"] = 0

    def alloc_attn(kt_shape, v_shape, wcols, wo_shape, q_shape, mix_shape):
        nonlocal KTall, Vall, Wp, Wo, QTst, mixT, basex, PTr, qtm, qtb, rt, sq8, rec, rec2, ot1, ot2, osq
        nonlocal qg_row, kg_row
        if kt_shape is not None:
            KTall = k.sb(kt_shape, BF16, "KTall")
            Vall = k.sb(v_shape, BF16, "Vall")
        Wp = k.sb([128, KT, wcols], BF16, "Wp")
        Wo = k.sb(wo_shape, BF16, "Wo")
        QTst = k.sb(q_shape, BF16, "QTst")
        mixT = k.sb(mix_shape, BF16, "mixT")
        basex = k.sb([128, D], F32, "basex")
        PTr = [k.sb([128, 512], BF16, "ptr%d" % i) for i in range(3)]
        qtm = k.sb([128, 1024], F32, "qtm")
        qtb = k.sb([128, 1024], BF16, "qtb")
        rt = [k.sb([128, 512], F32, "rt%d" % i) for i in range(2)]
        sq8 = k.sb([128, 16], F32, "sq8")
        rec = k.sb([128, 512], F32, "rec")
        rec2 = k.sb([128, 512], F32, "rec2")
        ot1 = k.sb([128, 512], F32, "ot1")
        ot2 = k.sb([128, 512], F32, "ot2")
        osq = k.sb([128, 512], BF16, "osq")
        qg_row = k.sb([128, 64], F32, "qg_row")
        kg_row = k.sb([128, 64], F32, "kg_row")
        dma("sp", qg_row[:], qg_in.partition_broadcast(128), [uIN], [qg_row])
        dma("sp", kg_row[:], kg_in.partition_broadcast(128), [uIN], [kg_row])
        alloc_rows_attn()

    def head_rms(src_buf, c0, nh, g_row, dst_buf, d0):
        sv = src_buf[:, c0:c0 + nh * 64].rearrange("p (h c) -> p h c", h=nh)
        tv = rt[0][:, 0:nh * 64].rearrange("p (h c) -> p h c", h=nh)
        op("dve", lambda e: e.tensor_tensor(out=tv, in0=sv, in1=sv, op=ALU.mult), [src_buf], [rt[0]])
        op("dve", lambda e: e.tensor_reduce(out=sq8[:, 0:nh], in_=tv, axis=AX.X, op=ALU.add), [rt[0]], [sq8])
        op("dve", lambda e: e.tensor_scalar(out=sq8[:, 0:nh], in0=sq8[:, 0:nh], scalar1=1.0 / 64, scalar2=EPS,
                                            op0=ALU.mult, op1=ALU.add), [], [sq8])
        op("act", lambda e: e.activation(out=sq8[:, 0:nh], in_=sq8[:, 0:nh], func=AF.Sqrt), [sq8], [sq8])
        op("dve", lambda e: e.reciprocal(out=sq8[:, 0:nh], in_=sq8[:, 0:nh]), [sq8], [sq8])
        dv = dst_buf[:, d0:d0 + nh * 64].rearrange("p (h c) -> p h c", h=nh)
        op("dve", lambda e: e.tensor_tensor(out=dv, in0=sv, in1=bc(sq8[:, 0:nh].unsqueeze(2), [128, nh, 64]),
                                            op=ALU.mult), [src_buf, sq8], [dst_buf])
        op("dve", lambda e: e.tensor_tensor(out=dv, in0=dv, in1=bc(g_row[:, :].unsqueeze(1), [128, nh, 64]),
                                            op=ALU.mult), [g_row], [dst_buf])

    def tiles_of(b):
        return [(True, i) for i in range(NCX)] + [(False, i) for i in range(NT)]

    def src_tile(l, b, is_ctx, i):
        if l == 0:
            return (ctx_in[b, i * 128:(i + 1) * 128, :], uIN) if is_ctx else (x_in[b, i * 128:(i + 1) * 128, :], uIN)
        return (XCA[b, i * 128:(i + 1) * 128, :], uXCA) if is_ctx else (XSA[b, i * 128:(i + 1) * 128, :], uXSA)

    def dst_tile(l, b, is_ctx, i):
        if l == 0:
            return (XCA[b, i * 128:(i + 1) * 128, :], uXCA) if is_ctx else (XSA[b, i * 128:(i + 1) * 128, :], uXSA)
        return (None, None) if is_ctx else (XSB[b, i * 128:(i + 1) * 128, :], uXSB)

    def out_proj_store(l, b, is_ctx, tiles, first, nchunk):
        for si, ti in enumerate(tiles):
            for half in range(2):
                for c in range(nchunk):
                    op("pe", lambda e, c=c, half=half, si=si: e.matmul(
                        PB[4 + half][:, :], lhsT=mixT[:, c, si * 128:(si + 1) * 128],
                        rhs=Wo[:, c, half * 512:(half + 1) * 512], start=(c == 0), stop=(c == nchunk - 1)),
                       [mixT, Wo], [PB[4 + half]])
            dap, dunit = dst_tile(l, b, is_ctx, ti)
            base = basex
            if first:
                sap, sunit = src_tile(l, b, is_ctx, ti)
                dma("sp", base[:], sap, [sunit], [base])
            else:
                dma("sp", base[:], dap, [dunit], [base])
            g1 = rows["G1"][1 if is_ctx else 0]
            for half in range(2):
                op("dve", lambda e, half=half: e.tensor_tensor(out=ynew[:, half * 512:(half + 1) * 512],
                                                               in0=PB[4 + half][:, :],
                                                               in1=g1[:, half * 512:(half + 1) * 512], op=ALU.mult),
                   [PB[4 + half], g1], [ynew])
            op("dve", lambda e: e.tensor_tensor(out=ynew[:], in0=ynew[:], in1=base[:], op=ALU.add), [base], [ynew])
            dma("pool", dap, ynew[:], [ynew], [dunit])

    def supertiles(b, with_ctx):
        sts = []
        if with_ctx:
            sts.append((True, list(range(NCX))))
        for s0 in range(0, NT, 4):
            sts.append((False, list(range(s0, min(s0 + 4, NT)))))
        return sts

    def load_wo(src_rows, pk, nchunk):
        for c0 in range(0, D, CW):
            stg = wstage[cnt["ws"] % 2]
            cnt["ws"] += 1
            dma("sp", stg[0:pk, 0:nchunk, :], src_rows[:, c0:c0 + CW].rearrange("(c p) n -> p c n", p=pk),
                [uIN], [stg])
            op("pool", lambda e: e.tensor_copy(out=Wo[0:pk, 0:nchunk, c0:c0 + CW], in_=stg[0:pk, 0:nchunk, :]),
               [stg], [Wo])

    def gqa_pass(b, first):
      with k.scope():
        l = 0
        alloc_attn([64, 2, NU * 128], [128, NU, 256], 768, [64, 8, D], [64, 8, 512], [64, 8, 512])
        load_rows_attn(0, b, 0)
        load_rows_attn(0, NB, 1)
        load_w(Wp, 0, ev_w_in, 512, 256)
        load_w(Wp, 256, ev_w_in, 0, 512)
        load_wo(ev_w_out[0:512, :], 64, 8)
        vv = Vall[:, :, 0:256].rearrange("p u (h c) -> p u h c", h=2)
        op("pool", lambda e: e.memset(vv[:, :, :, 64:128], 1.0), [], [Vall])
        for u, (is_ctx, ti) in enumerate(tiles_of(b)):
            sap, sunit = src_tile(l, b, is_ctx, ti)
            xt, hT = tile_prep(sap, sunit, NB if is_ctx else b)
            proj(hT, Wp, 0, 256, PB[0])
            op("act", lambda e: e.activation(out=qtm[:, 0:256], in_=PB[0][:, 0:256], func=AF.Copy), [PB[0]], [qtm])
            head_rms(qtm, 0, 2, kg_row, qtm, 0)
            if not is_ctx:
                rope_apply((qtm, 256), (qtm, 0), 2, ti, rt)
                ksrc = 256
            else:
                ksrc = 0
            op("act", lambda e, ksrc=ksrc: e.activation(out=qtb[:, 0:128], in_=qtm[:, ksrc:ksrc + 128], func=AF.Copy),
               [qtm], [qtb])
            for h in range(2):
                op("pe", lambda e, h=h: e.transpose(out=PT2[0:64, h * 128:(h + 1) * 128],
                                                    in_=qtb[:, h * 64:(h + 1) * 64], identity=ident_b[:]),
                   [qtb, ident_b], [PT2])
            op("act", lambda e, u=u: e.activation(
                out=KTall[0:64, 0:2, u * 128:(u + 1) * 128],
                in_=PT2[0:64, 0:256].rearrange("p (h t) -> p h t", h=2), func=AF.Copy), [PT2], [KTall])
            op("act", lambda e, u=u: e.activation(
                out=Vall[:, u, 0:256].rearrange("p (h c) -> p h c", h=2)[:, :, 0:64],
                in_=qtm[:, 128:256].rearrange("p (h c) -> p h c", h=2), func=AF.Copy), [qtm], [Vall])
        for (is_ctx, tiles) in supertiles(b, True):
            N = 128 * len(tiles)
            for si, ti in enumerate(tiles):
                sap, sunit = src_tile(l, b, is_ctx, ti)
                xt, hT = tile_prep(sap, sunit, NB if is_ctx else b)
                proj(hT, Wp, 256, 512, PB[0])
                op("act", lambda e: e.activation(out=qtm[:, 0:512], in_=PB[0][:, :], func=AF.Copy), [PB[0]], [qtm])
                head_rms(qtm, 0, 8, qg_row, qtm, 0)
                if not is_ctx:
                    rope_apply((qtm, 512), (qtm, 0), 8, ti, rt)
                    qsrc = 512
                else:
                    qsrc = 0
                op("act", lambda e, qsrc=qsrc: e.activation(out=qtb[:, 0:512], in_=qtm[:, qsrc:qsrc + 512],
                                                           func=AF.Copy), [qtm], [qtb])
                for h in range(8):
                    op("pe", lambda e, h=h: e.transpose(out=PT2[0:64, h * 128:(h + 1) * 128],
                                                        in_=qtb[:, h * 64:(h + 1) * 64], identity=ident_b[:]),
                       [qtb, ident_b], [PT2])
                op("act", lambda e, si=si: e.activation(
                    out=QTst[0:64, :, si * 128:(si + 1) * 128],
                    in_=PT2[0:64, :].rearrange("p (h t) -> p h t", h=8), func=AF.Copy), [PT2], [QTst])
            keys = list(range(NCX)) if is_ctx else list(range(NU))
            for head in range(8):
                kvh = head // 4
                acc = PB[2 + head % 2]
                for ui, u in enumerate(keys):
                    sp_ = PB[ui % 2]
                    op("pe", lambda e, u=u, sp_=sp_: e.matmul(
                        sp_[:, 0:N], lhsT=KTall[0:64, kvh, u * 128:(u + 1) * 128], rhs=QTst[0:64, head, 0:N],
                        start=True, stop=True), [KTall, QTst], [sp_])
                    pt = PTr[cnt["ptr"] % 3]
                    cnt["ptr"] += 1
                    op("act", lambda e, sp_=sp_, pt=pt: e.activation(out=pt[:, 0:N], in_=sp_[:, 0:N], func=AF.Exp,
                                                                     scale=0.125), [sp_], [pt])
                    op("pe", lambda e, u=u, pt=pt, ui=ui: e.matmul(
                        acc[:, 0:N], lhsT=Vall[:, u, kvh * 128:(kvh + 1) * 128], rhs=pt[:, 0:N],
                        start=(ui == 0), stop=(ui == len(keys) - 1)), [Vall, pt], [acc])
                op("dve", lambda e: e.reciprocal(out=rec[64:128, 0:N], in_=acc[64:128, 0:N]), [acc], [rec])
                op("dve", lambda e: e.tensor_tensor(out=mixT[0:64, head, 0:N], in0=acc[0:64, 0:N],
                                                    in1=rec[64:128, 0:N], op=ALU.mult), [acc, rec], [mixT])
            out_proj_store(l, b, is_ctx, tiles, first, 8)

    ret_c = {}

    def ret_setup():
        lg = k.sb([128, 8], F32, "lg")
        dma("sp", lg[:], rate_in.partition_broadcast(128), [uIN], [lg])
        op("act", lambda e: e.activation(out=lg[:], in_=lg[:], func=AF.Exp), [lg], [lg])
        op("dve", lambda e: e.tensor_scalar(out=lg[:], in0=lg[:], scalar1=-1.0, scalar2=None, op0=ALU.mult), [], [lg])
        relm = k.sb([128, 128], F32, "relm")
        pidx = k.sb([128, 2], F32, "pidx")
        fidx = k.sb([128, 128], F32, "fidx")
        dma("sp", relm[:], relm_in, [uIN], [relm])
        dma("sp", pidx[:], pidx_in, [uIN], [pidx])
        dma("sp", fidx[:], fidx_in, [uIN], [fidx])
        relp = k.sb([128, 128], F32, "relp")
        reln = k.sb([128, 128], F32, "reln")
        mp = k.sb([128, 128], F32, "mp")
        mn = k.sb([128, 128], F32, "mn")
        op("dve", lambda e: e.tensor_scalar(out=relp[:], in0=relm[:], scalar1=0.0, scalar2=None, op0=ALU.max),
           [relm], [relp])
        op("dve", lambda e: e.tensor_scalar(out=reln[:], in0=relm[:], scalar1=-1.0, scalar2=0.0, op0=ALU.mult,
                                            op1=ALU.max), [relm], [reln])
        op("dve", lambda e: e.tensor_scalar(out=mp[:], in0=relm[:], scalar1=0.0, scalar2=None, op0=ALU.is_ge),
           [relm], [mp])
        op("dve", lambda e: e.tensor_scalar(out=mn[:], in0=relm[:], scalar1=0.0, scalar2=None, op0=ALU.is_le),
           [relm], [mn])
        DT = k.sb([128, 4, 128], F32, "DT")
        QD = k.sb([128, 4, 128], F32, "QD")
        KD = k.sb([128, 4, 2], F32, "KD")
        DEC = k.sb([128, 4], F32, "DEC")
        tmpm = k.sb([128, 128], F32, "tmpm")
        for h in range(4):
            lf, lb = lg[:, h:h + 1], lg[:, 4 + h:5 + h]
            op("act", lambda e, lf=lf: e.activation(out=tmpm[:], in_=relp[:], func=AF.Exp, scale=lf), [relp, lg], [tmpm])
            op("dve", lambda e, h=h: e.tensor_tensor(out=DT[:, h, :], in0=tmpm[:], in1=mp[:], op=ALU.mult),
               [tmpm, mp], [DT])
            op("act", lambda e, lb=lb: e.activation(out=tmpm[:], in_=reln[:], func=AF.Exp, scale=lb), [reln, lg], [tmpm])
            op("dve", lambda e: e.tensor_tensor(out=tmpm[:], in0=tmpm[:], in1=mn[:], op=ALU.mult), [mn], [tmpm])
            op("dve", lambda e, h=h: e.tensor_tensor(out=DT[:, h, :], in0=DT[:, h, :], in1=tmpm[:], op=ALU.add),
               [tmpm], [DT])
            op("act", lambda e, h=h, lf=lf: e.activation(out=QD[0:64, h, :], in_=fidx[0:64, :], func=AF.Exp,
                                                         scale=lg[0:64, h:h + 1]), [fidx, lg], [QD])
            op("act", lambda e, h=h: e.activation(out=QD[64:128, h, :], in_=fidx[64:128, :], func=AF.Exp,
                                                  scale=lg[64:128, 4 + h:5 + h]), [fidx, lg], [QD])
            op("act", lambda e, h=h, lf=lf: e.activation(out=KD[:, h, 0:1], in_=pidx[:, 0:1], func=AF.Exp, scale=lf),
               [pidx, lg], [KD])
            op("act", lambda e, h=h, lb=lb: e.activation(out=KD[:, h, 1:2], in_=pidx[:, 1:2], func=AF.Exp, scale=lb),
               [pidx, lg], [KD])
            op("act", lambda e, h=h: e.activation(out=DEC[0:64, h:h + 1], in_=lg[0:64, h:h + 1], func=AF.Exp,
                                                  scale=128.0), [lg], [DEC])
            op("act", lambda e, h=h: e.activation(out=DEC[64:128, h:h + 1], in_=lg[64:128, 4 + h:5 + h], func=AF.Exp,
                                                  scale=128.0), [lg], [DEC])
        ret_c.update(DT=DT, QD=QD, KD=KD, DEC=DEC)
        ret_c["kbd"] = k.sb([128, 4, 128], BF16, "kbd")
        ret_c["vb"] = k.sb([128, 512], BF16, "vbb")
        ret_c["kvs"] = k.sb([128, 4, 128], F32, "kvs")
        ret_c["S"] = k.sb([128, 4, 128], F32, "Sst")
        ret_c["Sb"] = k.sb([128, 4, 128], BF16, "Sbb")
        ret_c["Sl"] = k.sb([128, 4, 128], BF16, "Sl")
        ret_c["qdup"] = k.sb([128, 4, 128], BF16, "qdup")
        ret_c["qT"] = k.sb([64, 4, 128], BF16, "qTr")
        ret_c["qdT"] = k.sb([128, 4, 128], BF16, "qdT")
        ret_c["kT"] = k.sb([64, 4, 128], BF16, "kTr")
        ret_c["scm"] = k.sb([128, 4, 128], BF16, "scm")
        ret_c["gsl"] = k.sb([128, 512], F32, "gsl")
        ret_c["om"] = k.sb([128, 512], F32, "om")
        ret_c["omb"] = k.sb([128, 512], BF16, "omb")

    def ret_qkv(b, is_ctx, ti, need_q):
        l = 0
        rc = ret_c
        sap, sunit = src_tile(l, b, is_ctx, ti)
        xt, hT = tile_prep(sap, sunit, NB if is_ctx else b)
        proj(hT, Wp, 0, 512, PB[0])
        proj(hT, Wp, 512, 512, PB[1])
        op("act", lambda e: e.activation(out=qtm[:, 0:256], in_=PB[0][:, 0:256], func=AF.Copy), [PB[0]], [qtm])
        op("act", lambda e: e.activation(out=qtm[:, 256:512], in_=PB[0][:, 256:512], func=AF.Copy, scale=0.125),
           [PB[0]], [qtm])
        op("act", lambda e: e.activation(out=rc["vb"][:], in_=PB[1][:, :], func=AF.Copy), [PB[1]], [rc["vb"]])
        if not is_ctx:
            rope_apply((qtm, 512), (qtm, 0), 8, ti, rt)
            s0 = 512
        else:
            s0 = 0
        kv4 = qtm[:, s0 + 256:s0 + 512].rearrange("p (h c) -> p h c", h=4)
        op("dve", lambda e: e.tensor_copy(out=qtb[:, 0:256], in_=qtm[:, s0 + 256:s0 + 512]), [qtm], [qtb])
        for d_ in range(2):
            op("dve", lambda e, d_=d_: e.tensor_tensor(
                out=rc["kbd"][:, :, d_ * 64:(d_ + 1) * 64], in0=kv4,
                in1=bc(rc["KD"][:, :, d_:d_ + 1], [128, 4, 64]), op=ALU.mult), [qtm, rc["KD"]], [rc["kbd"]])
        for h in range(4):
            op("pe", lambda e, h=h: e.transpose(out=PT2[0:64, h * 128:(h + 1) * 128], in_=qtb[:, h * 64:(h + 1) * 64],
                                                identity=ident_b[:]), [qtb, ident_b], [PT2])
        op("act", lambda e: e.activation(out=rc["kT"][:], in_=PT2[0:64, 0:512].rearrange("p (h t) -> p h t", h=4),
                                         func=AF.Copy), [PT2], [rc["kT"]])
        if need_q:
            qv4 = qtm[:, s0:s0 + 256].rearrange("p (h c) -> p h c", h=4)
            for d_ in range(2):
                op("dve", lambda e, d_=d_: e.tensor_copy(out=rc["qdup"][:, :, d_ * 64:(d_ + 1) * 64], in_=qv4),
                   [qtm], [rc["qdup"]])
            for h in range(4):
                op("pe", lambda e, h=h: e.transpose(out=PT_[:, h * 128:(h + 1) * 128], in_=rc["qdup"][:, h, :],
                                                    identity=ident_b[:]), [rc["qdup"], ident_b], [PT_])
            ptv = PT_[:, 0:512].rearrange("p (h t) -> p h t", h=4)
            op("dve", lambda e: e.tensor_copy(out=rc["qT"][:], in_=PT_[0:64, 0:512].rearrange("p (h t) -> p h t", h=4)),
               [PT_], [rc["qT"]])
            op("dve", lambda e: e.tensor_tensor(out=rc["qdT"][:], in0=ptv, in1=rc["QD"][:], op=ALU.mult),
               [PT_, rc["QD"]], [rc["qdT"]])
            if stop_after == "ret_qa":
                return xt
            proj(hT, Wp, 1024, 512, PB[2])
            if stop_after == "ret_qb":
                return xt
            op("act", lambda e: e.activation(out=rc["gsl"][:], in_=PB[2][:, :], func=AF.Silu), [PB[2]], [rc["gsl"]])
        return xt

    def ret_pass(b, first):
      with k.scope():
        l = 0
        alloc_attn(None, None, 1536, [128, 4, D], [128, 1, 128], [128, 4, 512])
        ret_setup()
        rc = ret_c
        load_rows_attn(0, b, 0)
        load_rows_attn(0, NB, 1)
        load_w(Wp, 0, ev_w_in, 768, 256)
        load_w(Wp, 256, ev_w_in, 1024, 256)
        load_w(Wp, 512, ev_w_in, 1280, 512)
        load_w(Wp, 1024, ev_w_in, 1792, 512)
        load_wo(ev_w_out[512:1024, :], 128, 4)
        tl = tiles_of(b)
        if stop_after == "ret_setup":
            return
        for u, (is_ctx, ti) in enumerate(tl):
            ret_qkv(b, is_ctx, ti, False)
            for h in range(4):
                op("pe", lambda e, h=h: e.matmul(PB[3][:, h * 128:(h + 1) * 128], lhsT=rc["kbd"][:, h, :],
                                                 rhs=rc["vb"][:, h * 128:(h + 1) * 128], start=True, stop=True),
                   [rc["kbd"], rc["vb"]], [PB[3]])
            op("act", lambda e: e.activation(out=rc["kvs"][:], in_=PB[3][:, :].rearrange("p (h c) -> p h c", h=4),
                                             func=AF.Copy), [PB[3]], [rc["kvs"]])
            dma("sp", KVD[u], rc["kvs"][:], [rc["kvs"]], [uKVD])
        if stop_after == "ret_p1":
            return
        of = list(range(NU))
        ob = list(range(NCX - 1, -1, -1)) + list(range(NU - 1, NCX - 1, -1))
        S = rc["S"]
        op("dve", lambda e: e.memset(S[:], 0.0), [], [S])
        for t in range(NU):
            cf, cb = of[t], ob[t]
            op("act", lambda e: e.activation(out=rc["Sb"][:], in_=S[:], func=AF.Copy), [S], [rc["Sb"]])
            dma("sp", STD[cf, 0:64], rc["Sb"][0:64], [rc["Sb"]], [uSTD])
            dma("sp", STD[cb, 64:128], rc["Sb"][64:128], [rc["Sb"]], [uSTD])
            dma("sp", rc["kvs"][0:64], KVD[cf, 0:64], [uKVD], [rc["kvs"]])
            dma("sp", rc["kvs"][64:128], KVD[cb, 64:128], [uKVD], [rc["kvs"]])
            op("dve", lambda e: e.tensor_tensor(out=S[:], in0=S[:], in1=bc(rc["DEC"][:, :].unsqueeze(2), [128, 4, 128]),
                                                op=ALU.mult), [rc["DEC"]], [S])
            op("dve", lambda e: e.tensor_tensor(out=S[:], in0=S[:], in1=rc["kvs"][:], op=ALU.add), [rc["kvs"]], [S])
        if stop_after == "ret_scan":
            return
        for (is_ctx, tiles) in supertiles(b, True):
            for si, ti in enumerate(tiles):
                u = ti if is_ctx else NCX + ti
                xt = ret_qkv(b, is_ctx, ti, True)
                if stop_after in ("ret_q", "ret_qa", "ret_qb"):
                    return
                dma("sp", rc["Sl"][:], STD[u], [uSTD], [rc["Sl"]])
                for h in range(4):
                    op("pe", lambda e, h=h: e.matmul(PB[3][:, h * 128:(h + 1) * 128], lhsT=rc["kT"][:, h, :],
                                                     rhs=rc["qT"][:, h, :], start=True, stop=True),
                       [rc["kT"], rc["qT"]], [PB[3]])
                op("dve", lambda e: e.tensor_tensor(out=rc["scm"][:],
                                                    in0=PB[3][:, :].rearrange("p (h c) -> p h c", h=4),
                                                    in1=rc["DT"][:], op=ALU.mult), [PB[3], rc["DT"]], [rc["scm"]])
                for h in range(4):
                    op("pe", lambda e, h=h: e.matmul(PB[2][:, h * 128:(h + 1) * 128], lhsT=rc["scm"][:, h, :],
                                                     rhs=rc["vb"][:, h * 128:(h + 1) * 128], start=True, stop=False),
                       [rc["scm"], rc["vb"]], [PB[2]])
                    op("pe", lambda e, h=h: e.matmul(PB[2][:, h * 128:(h + 1) * 128], lhsT=rc["qdT"][:, h, :],
                                                     rhs=rc["Sl"][:, h, :], start=False, stop=True),
                       [rc["qdT"], rc["Sl"]], [PB[2]])
                if stop_after == "ret_o":
                    return
                om = rc["om"]
                op("act", lambda e: e.activation(out=om[:], in_=PB[2][:, :], func=AF.Copy), [PB[2]], [om])
                ov = om[:, :].rearrange("p (h c) -> p h c", h=4)
                tv = rt[0][:, 0:512].rearrange("p (h c) -> p h c", h=4)
                op("dve", lambda e: e.tensor_tensor(out=tv, in0=ov, in1=ov, op=ALU.mult), [om], [rt[0]])
                op("dve", lambda e: e.tensor_reduce(out=sq8[:, 0:4], in_=tv, axis=AX.X, op=ALU.add), [rt[0]], [sq8])
                op("dve", lambda e: e.tensor_scalar(out=sq8[:, 0:4], in0=sq8[:, 0:4], scalar1=1.0 / 128, scalar2=EPS,
                                                    op0=ALU.mult, op1=ALU.add), [], [sq8])
                op("act", lambda e: e.activation(out=sq8[:, 0:4], in_=sq8[:, 0:4], func=AF.Sqrt), [sq8], [sq8])
                op("dve", lambda e: e.reciprocal(out=sq8[:, 0:4], in_=sq8[:, 0:4]), [sq8], [sq8])
                op("dve", lambda e: e.tensor_tensor(out=ov, in0=ov, in1=bc(sq8[:, 0:4].unsqueeze(2), [128, 4, 128]),
                                                    op=ALU.mult), [sq8], [om])
                op("dve", lambda e: e.tensor_tensor(out=rc["omb"][:], in0=om[:], in1=rc["gsl"][:], op=ALU.mult),
                   [om, rc["gsl"]], [rc["omb"]])
                for h in range(4):
                    op("pe", lambda e, h=h: e.transpose(out=PT2[:, h * 128:(h + 1) * 128],
                                                        in_=rc["omb"][:, h * 128:(h + 1) * 128], identity=ident_b[:]),
                       [rc["omb"], ident_b], [PT2])
                op("act", lambda e, si=si: e.activation(out=mixT[:, :, si * 128:(si + 1) * 128],
                                                        in_=PT2[:, 0:512].rearrange("p (h t) -> p h t", h=4),
                                                        func=AF.Copy), [PT2], [mixT])
            if stop_after == "ret_m":
                return
            out_proj_store(l, b, is_ctx, tiles, first, 4)

    diff_c = {}

    def diff_setup():
        lam_sb = k.sb([128, 256], F32, "lam_sb")
        dma("sp", lam_sb[:], lam_in.partition_broadcast(128), [uIN], [lam_sb])
        l2 = k.sb([128, 2], F32, "l2")
        lv = lam_sb[:, :].rearrange("p (a b c) -> p a b c", a=2, b=2)
        tv = rt[0][:, 0:128].rearrange("p (a c) -> p a c", a=2)
        op("dve", lambda e: e.tensor_tensor(out=tv, in0=lv[:, :, 0, :], in1=lv[:, :, 1, :], op=ALU.mult),
           [lam_sb], [rt[0]])
        op("dve", lambda e: e.tensor_reduce(out=l2[:], in_=tv, axis=AX.X, op=ALU.add), [rt[0]], [l2])
        op("act", lambda e: e.activation(out=l2[:], in_=l2[:], func=AF.Exp), [l2], [l2])
        lam_init = 0.8 - 0.6 * math.exp(-0.3 * 1)
        nl = k.sb([128, 1], F32, "neglam")
        op("dve", lambda e: e.tensor_tensor(out=nl[:], in0=l2[:, 1:2], in1=l2[:, 0:1], op=ALU.subtract), [l2], [nl])
        op("dve", lambda e: e.tensor_scalar(out=nl[:], in0=nl[:], scalar1=-lam_init, scalar2=None, op0=ALU.add),
           [], [nl])
        sg = k.sb([128, 1], F32, "sublng")
        dma("sp", sg[:], subln_in.rearrange("(p o) -> p o", o=1), [uIN], [sg])
        op("dve", lambda e: e.tensor_scalar(out=sg[:], in0=sg[:], scalar1=1.0 - lam_init, scalar2=None, op0=ALU.mult),
           [], [sg])
        diff_c.update(nl=nl, sg=sg)

    def diff_pass(b, grp, first):
      with k.scope():
        l = 1
        alloc_attn([128, 4, NU * 128], [128, NU, 512], 1536, [128, 4, D], [128, 4, 512], [128, 4, 512])
        diff_setup()
        nl, sg = diff_c["nl"], diff_c["sg"]
        load_rows_attn(1, b, 0)
        load_rows_attn(1, NB, 1)
        load_w(Wp, 0, od_w_in, 1024 + grp * 512, 512)
        load_w(Wp, 512, od_w_in, 2048 + grp * 512, 512)
        load_w(Wp, 1024, od_w_in, grp * 512, 512)
        load_wo(od_w_out[grp * 512:(grp + 1) * 512, :], 128, 4)
        for u, (is_ctx, ti) in enumerate(tiles_of(b)):
            sap, sunit = src_tile(l, b, is_ctx, ti)
            xt, hT = tile_prep(sap, sunit, NB if is_ctx else b)
            proj(hT, Wp, 0, 512, PB[0])
            proj(hT, Wp, 512, 512, PB[1])
            op("act", lambda e: e.activation(out=qtm[:, 0:512], in_=PB[0][:, :], func=AF.Copy), [PB[0]], [qtm])
            op("act", lambda e, u=u: e.activation(out=Vall[:, u, :], in_=PB[1][:, :], func=AF.Copy), [PB[1]], [Vall])
            if not is_ctx:
                rope_apply((qtm, 512), (qtm, 0), 8, ti, rt)
                s0 = 512
            else:
                s0 = 0
            op("act", lambda e, s0=s0: e.activation(out=qtb[:, 0:512], in_=qtm[:, s0:s0 + 512], func=AF.Copy),
               [qtm], [qtb])
            for h in range(4):
                op("pe", lambda e, h=h: e.transpose(out=PT2[:, h * 128:(h + 1) * 128],
                                                    in_=qtb[:, h * 128:(h + 1) * 128], identity=ident_b[:]),
                   [qtb, ident_b], [PT2])
            op("act", lambda e, u=u: e.activation(out=KTall[:, :, u * 128:(u + 1) * 128],
                                                  in_=PT2[:, 0:512].rearrange("p (h t) -> p h t", h=4), func=AF.Copy),
               [PT2], [KTall])
        for (is_ctx, tiles) in supertiles(b, False):
            N = 128 * len(tiles)
            for si, ti in enumerate(tiles):
                sap, sunit = src_tile(l, b, is_ctx, ti)
                xt, hT = tile_prep(sap, sunit, b)
                proj(hT, Wp, 1024, 512, PB[0])
                op("act", lambda e: e.activation(out=qtm[:, 0:512], in_=PB[0][:, :], func=AF.Copy), [PB[0]], [qtm])
                rope_apply((qtm, 512), (qtm, 0), 8, ti, rt)
                op("act", lambda e: e.activation(out=qtb[:, 0:512], in_=qtm[:, 512:1024], func=AF.Copy), [qtm], [qtb])
                for h in range(4):
                    op("pe", lambda e, h=h: e.transpose(out=PT2[:, h * 128:(h + 1) * 128],
                                                        in_=qtb[:, h * 128:(h + 1) * 128], identity=ident_b[:]),
                       [qtb, ident_b], [PT2])
                op("act", lambda e, si=si: e.activation(out=QTst[:, 0:4, si * 128:(si + 1) * 128],
                                                        in_=PT2[:, 0:512].rearrange("p (h t) -> p h t", h=4),
                                                        func=AF.Copy), [PT2], [QTst])
            for h in range(4):
                for c in range(2):
                    accO, accD = (PB[2], PB[3]) if c == 0 else (PB[4], PB[5])
                    for u in range(NU):
                        sp_ = PB[u % 2]
                        op("pe", lambda e, u=u, sp_=sp_, c=c: e.matmul(
                            sp_[:, 0:N], lhsT=KTall[c * 64:(c + 1) * 64, h, u * 128:(u + 1) * 128],
                            rhs=QTst[c * 64:(c + 1) * 64, h, 0:N], start=True, stop=True), [KTall, QTst], [sp_])
                        pt = PTr[cnt["ptr"] % 3]
                        cnt["ptr"] += 1
                        op("act", lambda e, sp_=sp_, pt=pt: e.activation(out=pt[:, 0:N], in_=sp_[:, 0:N], func=AF.Exp,
                                                                         scale=0.125), [sp_], [pt])
                        op("pe", lambda e, u=u, pt=pt: e.matmul(
                            accO[:, 0:N], lhsT=Vall[:, u, h * 128:(h + 1) * 128], rhs=pt[:, 0:N],
                            start=(u == 0), stop=(u == NU - 1)), [Vall, pt], [accO])
                        op("pe", lambda e, u=u, pt=pt: e.matmul(
                            accD[:, 0:N], lhsT=ones_b[:], rhs=pt[:, 0:N],
                            start=(u == 0), stop=(u == NU - 1)), [ones_b, pt], [accD])
                op("dve", lambda e: e.reciprocal(out=rec[:, 0:N], in_=PB[3][:, 0:N]), [PB[3]], [rec])
                op("dve", lambda e: e.reciprocal(out=rec2[:, 0:N], in_=PB[5][:, 0:N]), [PB[5]], [rec2])
                op("dve", lambda e: e.tensor_tensor(out=ot1[:, 0:N], in0=PB[2][:, 0:N], in1=rec[:, 0:N], op=ALU.mult),
                   [PB[2], rec], [ot1])
                op("dve", lambda e: e.tensor_tensor(out=ot2[:, 0:N], in0=PB[4][:, 0:N], in1=rec2[:, 0:N], op=ALU.mult),
                   [PB[4], rec2], [ot2])
                op("dve", lambda e: e.scalar_tensor_tensor(out=ot1[:, 0:N], in0=ot2[:, 0:N], scalar=nl[:, 0:1],
                                                           in1=ot1[:, 0:N], op0=ALU.mult, op1=ALU.add),
                   [ot2, nl], [ot1])
                op("act", lambda e: e.activation(out=osq[:, 0:N], in_=ot1[:, 0:N], func=AF.Square), [ot1], [osq])
                op("pe", lambda e: e.matmul(PB[0][:, 0:N], lhsT=ones_b[:], rhs=osq[:, 0:N], start=True, stop=True),
                   [ones_b, osq], [PB[0]])
                op("dve", lambda e: e.tensor_scalar(out=rec[:, 0:N], in0=PB[0][:, 0:N], scalar1=1.0 / 128, scalar2=EPS,
                                                    op0=ALU.mult, op1=ALU.add), [PB[0]], [rec])
                op("act", lambda e: e.activation(out=rec[:, 0:N], in_=rec[:, 0:N], func=AF.Sqrt), [rec], [rec])
                op("dve", lambda e: e.reciprocal(out=rec[:, 0:N], in_=rec[:, 0:N]), [rec], [rec])
                op("dve", lambda e, h=h: e.scalar_tensor_tensor(out=mixT[:, h, 0:N], in0=ot1[:, 0:N], scalar=sg[:, 0:1],
                                                                in1=rec[:, 0:N], op0=ALU.mult, op1=ALU.mult),
                   [ot1, sg, rec], [mixT])
            out_proj_store(l, b, is_ctx, tiles, first, 4)

    pc = {}

    def peer_setup():
        nonlocal Wp
        Wp = k.sb([128, KT, 2048], BF16, "Wq")
        alloc_rows_peer()
        pc["keys"] = k.sb([128, 16, 128], BF16, "pkeys")
        scr = k.sb([128, 2048], F32, "pscr")
        pc["scr"] = scr
        pc["h2"] = k.sb([128, D], F32, "h2")
        pc["h2b"] = k.sb([128, D], BF16, "h2b")
        pc["h2T"] = k.sb([128, KT, 128], BF16, "h2T")
        pc["qT"] = k.sb([128, 16, 128], BF16, "pqT")
        pc["sc"] = k.sb([128, 16, 128], F32, "psc")
        pc["vals"] = k.sb([128, 16, 16], F32, "pvals")
        pc["idx"] = k.sb([128, 16, 16], U32, "pidx_")
        pc["idxf"] = k.sb([128, 16, 16], F32, "pidxf")
        pc["cand"] = k.sb([128, 8, 256], F32, "pcand")
        pc["best"] = k.sb([128, 8, 16], F32, "pbest")
        pc["pos"] = k.sb([128, 8, 16], U32, "ppos")
        pc["pa"] = k.sb([128, 8, 16], U32, "ppa")
        pc["pb"] = k.sb([128, 8, 16], U32, "ppb")
        pc["paf"] = k.sb([128, 8, 16], F32, "ppaf")
        pc["pbf"] = k.sb([128, 8, 16], F32, "ppbf")
        pc["isel"] = k.sb([128, 8, 16], F32, "pisel")
        pc["jsel"] = k.sb([128, 8, 16], F32, "pjsel")
        pc["eid"] = k.sb([128, 128], U32, "peid")
        pc["gate"] = k.sb([128, 8, 16], F32, "pgate")
        pc["z"] = k.sb([128, 8], F32, "pz")
        pc["dots"] = k.sb([128, 128], F32, "pdots")
        pc["wgt"] = k.sb([128, 128], F32, "pwgt")
        pc["acc"] = k.sb([128, D], F32, "pacc")
        pc["gb"] = [k.sb([128, 2 * D], BF16, "pgb%d" % i) for i in range(8)]
        pc["dring"] = [k.sb([128, 1], F32, "pdr%d" % i) for i in range(4)]
        pc["wring"] = [k.sb([128, 1], F32, "pwr%d" % i) for i in range(4)]
        pc["dring2"] = [k.sb([128, 1], F32, "pdr2%d" % i) for i in range(4)]
        pc["stg"] = [k.sb([128, D], F32, "pstg%d" % i) for i in range(2)]
        pc["junkb"] = k.sb([128, D], BF16, "pjunkb")
        pc["dg"] = [k.sb([128, 128], BF16, "pdg%d" % i) for i in range(3)]
        pc["fg"] = k.sb([128, D], F32, "pfg")
        dma("sp", pc["fg"][:], final_g.partition_broadcast(128), [uIN], [pc["fg"]])
        pc["gi"] = 0

    def peer_load(l):
        ci = 0
        for (src, c0) in ((peer_u[l], 0), (peer_v[l], D)):
            for r0 in range(0, 16384, 128):
                st_ = pc["stg"][ci % 2]
                gbf = pc["gb"][ci % 8]
                ci += 1
                dma("sp", st_[:], src[r0:r0 + 128, :], [uIN], [st_])
                op("act", lambda e: e.activation(out=gbf[:, 0:D], in_=st_[:], func=AF.Copy), [st_], [gbf])
                dma("act", UVB[l][r0:r0 + 128, c0:c0 + D], gbf[:, 0:D], [gbf], [uTAB])
        load_w(Wp, 0, peer_wq[l], 0, 2048)
        sv = pc["scr"][:, :].rearrange("p (j n) -> p j n", j=16)
        dma("sp", sv, keysT[l], [uIN], [pc["scr"]])
        op("pool", lambda e: e.tensor_copy(out=pc["keys"][:], in_=sv), [pc["scr"]], [pc["keys"]])

    def top16(src, src2, vals_ap, idx_ap):
        (sbuf_, sap), (s2buf, s2ap) = src, src2
        (vb, vf), (ib, if_) = vals_ap, idx_ap
        op("dve", lambda e: e.max(out=vf(0, 8), in_=sap), [sbuf_], [vb])
        op("dve", lambda e: e.max_index(out=if_(0, 8), in_max=vf(0, 8), in_values=sap), [vb, sbuf_], [ib])
        op("dve", lambda e: e.match_replace(out=s2ap, in_to_replace=vf(0, 8), in_values=sap, imm_value=NEG),
           [vb, sbuf_], [s2buf])
        op("dve", lambda e: e.max(out=vf(8, 16), in_=s2ap), [s2buf], [vb])
        op("dve", lambda e: e.max_index(out=if_(8, 16), in_max=vf(8, 16), in_values=s2ap), [vb, s2buf], [ib])

    def peer_tile(l, b, is_ctx, ti, final):
        p = pc
        slot = 1 if is_ctx else 0
        scr_j = p["scr"][:, :].rearrange("p (j n) -> p j n", j=16)
        scr_h = p["scr"][:, :].rearrange("p (h c) -> p h c", h=8)
        if l == 0:
            sap, sunit = dst_tile(0, b, is_ctx, ti)
        else:
            sap, sunit = dst_tile(1, b, is_ctx, ti)
        xt = xt_ring[cnt["xt"] % 2]
        cnt["xt"] += 1
        dma("sp", xt[:], sap, [sunit], [xt])
        rstd_of(xt, 1)
        h2, h2b, h2T = p["h2"], p["h2b"], p["h2T"]
        op("dve", lambda e: e.scalar_tensor_tensor(out=h2[:], in0=xt[:], scalar=st4[:, 1:2], in1=rows["G2"][slot][:],
                                                   op0=ALU.mult, op1=ALU.mult), [xt, st4, rows["G2"][slot]], [h2])
        op("dve", lambda e: e.tensor_tensor(out=h2[:], in0=h2[:], in1=rows["SH2"][slot][:], op=ALU.add),
           [rows["SH2"][slot]], [h2])
        op("act", lambda e: e.activation(out=h2b[:], in_=h2[:], func=AF.Copy), [h2], [h2b])
        for kk in range(KT):
            op("pe", lambda e, kk=kk: e.transpose(out=PT_[:, kk * 128:(kk + 1) * 128],
                                                  in_=h2b[:, kk * 128:(kk + 1) * 128], identity=ident_b[:]),
               [h2b, ident_b], [PT_])
        op("act", lambda e: e.activation(out=h2T[:], in_=PT_[:, :].rearrange("p (k t) -> p k t", k=KT), func=AF.Copy),
           [PT_], [h2T])
        for j in range(16):
            pbk = PB[j // 4]
            for kk in range(KT):
                op("pe", lambda e, j=j, kk=kk, pbk=pbk: e.matmul(
                    pbk[:, (j % 4) * 128:(j % 4 + 1) * 128], lhsT=Wp[:, kk, j * 128:(j + 1) * 128], rhs=h2T[:, kk, :],
                    start=(kk == 0), stop=(kk == KT - 1)), [Wp, h2T], [pbk])
            if j % 4 == 3:
                g = j // 4
                op("act", lambda e, g=g, pbk=pbk: e.activation(
                    out=p["qT"][:, g * 4:(g + 1) * 4, :], in_=pbk[:, :].rearrange("p (j t) -> p j t", j=4),
                    func=AF.Copy), [pbk], [p["qT"]])
        for j in range(16):
            pbk = PB[j // 4]
            op("pe", lambda e, j=j, pbk=pbk: e.matmul(pbk[:, (j % 4) * 128:(j % 4 + 1) * 128], lhsT=p["qT"][:, j, :],
                                                      rhs=p["keys"][:, j, :], start=True, stop=True),
               [p["qT"], p["keys"]], [pbk])
            if j % 4 == 3:
                g = j // 4
                op("act", lambda e, g=g, pbk=pbk: e.activation(
                    out=p["sc"][:, g * 4:(g + 1) * 4, :], in_=pbk[:, :].rearrange("p (j n) -> p j n", j=4),
                    func=AF.Copy), [pbk], [p["sc"]])
        for j in range(16):
            top16((p["sc"], p["sc"][:, j, :]), (p["scr"], scr_j[:, j, :]),
                  (p["vals"], lambda a, c, j=j: p["vals"][:, j, a:c]), (p["idx"], lambda a, c, j=j: p["idx"][:, j, a:c]))
        op("dve", lambda e: e.tensor_copy(out=p["idxf"][:], in_=p["idx"][:]), [p["idx"]], [p["idxf"]])
        v4 = p["vals"][:, :, :].rearrange("p (h two) k -> p h two k", two=2)
        i4 = p["idxf"][:, :, :].rearrange("p (h two) k -> p h two k", two=2)
        c4 = p["cand"][:, :, :].rearrange("p h (a c) -> p h a c", a=16)
        op("dve", lambda e: e.tensor_tensor(out=c4, in0=bc(v4[:, :, 0, :].unsqueeze(3), [128, 8, 16, 16]),
                                            in1=bc(v4[:, :, 1, :].unsqueeze(2), [128, 8, 16, 16]), op=ALU.add),
           [p["vals"]], [p["cand"]])
        for h in range(8):
            top16((p["cand"], p["cand"][:, h, :]), (p["scr"], scr_h[:, h, :]),
                  (p["best"], lambda a, c, h=h: p["best"][:, h, a:c]), (p["pos"], lambda a, c, h=h: p["pos"][:, h, a:c]))
        op("dve", lambda e: e.tensor_single_scalar(out=p["pa"][:], in_=p["pos"][:], scalar=4,
                                                   op=ALU.logical_shift_right), [p["pos"]], [p["pa"]])
        op("dve", lambda e: e.tensor_single_scalar(out=p["pb"][:], in_=p["pos"][:], scalar=15, op=ALU.bitwise_and),
           [p["pos"]], [p["pb"]])
        op("dve", lambda e: e.tensor_copy(out=p["paf"][:], in_=p["pa"][:]), [p["pa"]], [p["paf"]])
        op("dve", lambda e: e.tensor_copy(out=p["pbf"][:], in_=p["pb"][:]), [p["pb"]], [p["pbf"]])
        e4 = p["scr"][:, :].rearrange("p (h k a) -> p h k a", h=8, k=16)
        io4 = bc(iota16[:, :].unsqueeze(1).unsqueeze(1), [128, 8, 16, 16])
        for (pf, half, dstb) in ((p["paf"], 0, p["isel"]), (p["pbf"], 1, p["jsel"])):
            op("dve", lambda e, pf=pf: e.tensor_tensor(out=e4, in0=bc(pf[:, :, :].unsqueeze(3), [128, 8, 16, 16]),
                                                       in1=io4, op=ALU.is_equal), [pf, iota16], [p["scr"]])
            op("dve", lambda e, half=half: e.tensor_tensor(out=e4, in0=e4,
                                                           in1=bc(i4[:, :, half, :].unsqueeze(2), [128, 8, 16, 16]),
                                                           op=ALU.mult), [p["idxf"]], [p["scr"]])
            op("dve", lambda e, dstb=dstb: e.tensor_reduce(out=dstb[:], in_=e4, axis=AX.X, op=ALU.add),
               [p["scr"]], [dstb])
        op("dve", lambda e: e.scalar_tensor_tensor(out=p["isel"][:], in0=p["isel"][:], scalar=128.0, in1=p["jsel"][:],
                                                   op0=ALU.mult, op1=ALU.add), [p["jsel"]], [p["isel"]])
        op("dve", lambda e: e.tensor_copy(out=p["eid"][:, :].rearrange("p (h k) -> p h k", h=8), in_=p["isel"][:]),
           [p["isel"]], [p["eid"]])
        op("dve", lambda e: e.tensor_tensor(out=p["gate"][:], in0=p["best"][:],
                                            in1=bc(p["best"][:, :, 0:1], [128, 8, 16]), op=ALU.subtract),
           [p["best"]], [p["gate"]])
        op("act", lambda e: e.activation(out=p["gate"][:], in_=p["gate"][:], func=AF.Exp), [p["gate"]], [p["gate"]])
        op("dve", lambda e: e.tensor_reduce(out=p["z"][:], in_=p["gate"][:], axis=AX.X, op=ALU.add),
           [p["gate"]], [p["z"]])
        op("dve", lambda e: e.reciprocal(out=p["z"][:], in_=p["z"][:]), [p["z"]], [p["z"]])
        op("dve", lambda e: e.tensor_tensor(out=p["gate"][:], in0=p["gate"][:],
                                            in1=bc(p["z"][:, :].unsqueeze(2), [128, 8, 16]), op=ALU.mult),
           [p["z"]], [p["gate"]])
        acc = p["acc"]
        gflat = p["gate"][:, :, :].rearrange("p h k -> p (h k)")
        pend = None

        def finish_slot(ps):
            s_, gb_, wr_ = ps
            dg = p["dg"][s_ % 3]
            op("dve", lambda e: e.tensor_scalar(out=dg[:], in0=ident_f[:], scalar1=wr_[:, 0:1],
                                                scalar2=gflat[:, s_:s_ + 1], op0=ALU.mult, op1=ALU.mult),
               [ident_f, wr_, p["gate"]], [dg])
            for half in range(2):
                op("pe", lambda e, half=half: e.matmul(
                    PB[4 + half][:, :], lhsT=dg[:], rhs=gb_[:, D + half * 512:D + (half + 1) * 512],
                    start=(s_ == 0), stop=(s_ == 127)), [dg, gb_], [PB[4 + half]])

        for s in range(128):
            gbuf = p["gb"][p["gi"] % 8]
            dr = p["dring"][p["gi"] % 4]
            wr = p["wring"][p["gi"] % 4]
            p["gi"] += 1
            dma("pool", gbuf[:], UVB[l], [uTAB, p["eid"]], [gbuf], indirect=p["eid"][:, s:s + 1])
            op("dve", lambda e: e.scalar_tensor_tensor(
                out=p["junkb"][:], in0=gbuf[:, 0:D], scalar=1.0, in1=h2b[:], op0=ALU.mult, op1=ALU.mult,
                accum_out=dr[:, 0:1]), [gbuf, h2b], [p["junkb"], dr])
            dr2 = p["dring2"][(p["gi"] - 1) % 4]
            op("dve", lambda e: e.tensor_copy(out=dr2[:, 0:1], in_=dr[:, 0:1]), [dr], [dr2])
            op("act", lambda e: e.activation(out=wr[:, 0:1], in_=dr2[:, 0:1], func=AF.Gelu), [dr2], [wr])
            if pend is not None:
                finish_slot(pend)
            pend = (s, gbuf, wr)
        finish_slot(pend)
        for half in range(2):
            op("dve", lambda e, half=half: e.tensor_tensor(out=acc[:, half * 512:(half + 1) * 512],
                                                           in0=PB[4 + half][:, :],
                                                           in1=rows["g2"][slot][:, half * 512:(half + 1) * 512],
                                                           op=ALU.mult), [PB[4 + half], rows["g2"][slot]], [acc])
        op("dve", lambda e: e.tensor_tensor(out=ynew[:], in0=acc[:], in1=xt[:], op=ALU.add), [acc, xt], [ynew])
        if not final:
            dma("sp", sap, ynew[:], [ynew], [sunit])
        else:
            rstd_of(ynew, 2)
            op("dve", lambda e: e.scalar_tensor_tensor(out=ynew[:], in0=ynew[:], scalar=st4[:, 2:3], in1=p["fg"][:],
                                                       op0=ALU.mult, op1=ALU.mult), [st4, p["fg"]], [ynew])
            dma("sp", out_d[b, ti * 128:(ti + 1) * 128, :], ynew[:], [ynew], [uOUT])


    def dump(srcu, src, is_final=False):
        for b in range(NB):
            for ti in range(NT):
                xt = xt_ring[cnt["xt"] % 2]
                cnt["xt"] += 1
                dma("sp", xt[:], src[b, ti * 128:(ti + 1) * 128, :], [srcu], [xt])
                dma("sp", out_d[b, ti * 128:(ti + 1) * 128, :], xt[:], [xt], [uOUT])

    done = False
    if stop_after == "const":
        dump(uIN, x_in)
        done = True
    if not done:
        mod_phase(0)
        if stop_after == "mod":
            dump(uIN, x_in)
            done = True
    if not done:
        for b in range(NB):
            if stop_after != "ret0":
                gqa_pass(b, True)
            if stop_after != "gqa0":
                ret_pass(b, stop_after == "ret0")
        if stop_after in ("mix0", "ret0", "gqa0", "ret_setup", "ret_p1", "ret_scan", "ret_q", "ret_o", "ret_m", "ret_qa", "ret_qb"):
            dump(uXSA, XSA)
            done = True
    if not done:
        with k.scope():
            peer_setup()
            peer_load(0)
            for b in range(NB):
                load_rows_peer(0, b, 0)
                load_rows_peer(0, NB, 1)
                for (is_ctx, ti) in tiles_of(b):
                    peer_tile(0, b, is_ctx, ti, False)
        if stop_after == "peer0":
            dump(uXSA, XSA)
            done = True
    if not done:
        mod_phase(1)
        for b in range(NB):
            diff_pass(b, 0, True)
            diff_pass(b, 1, False)
        if stop_after == "mix1":
            dump(uXSB, XSB)
            done = True
    if not done:
        with k.scope():
            peer_setup()
            peer_load(1)
            for b in range(NB):
                load_rows_peer(1, b, 0)
                for ti in range(NT):
                    peer_tile(1, b, False, ti, True)
    k.finish([uOUT])
    es.close()
    return nc


def host_consts(S):
    NT = S // 128
    GRID_W = 64
    t = np.arange(S)
    row = (t // GRID_W).astype(np.float32)
    col = (t % GRID_W).astype(np.float32)
    inv = (10000.0 ** (-np.arange(0, 32, 2, dtype=np.float32) / 32.0)).astype(np.float32)
    ar = row[:, None] * inv[None, :]
    ac = col[:, None] * inv[None, :]
    tab = np.concatenate([np.cos(ar), np.sin(ar), np.cos(ac), np.sin(ac)], axis=1).astype(np.float32)
    rope = np.ascontiguousarray(tab.reshape(NT, 128, 64).transpose(1, 0, 2))
    ident = np.eye(128, dtype=np.float32)
    iota16 = np.broadcast_to(np.arange(16, dtype=np.float32)[None, :], (128, 16)).copy()
    j = np.arange(128, dtype=np.float32)
    relm = (j[None, :] - j[:, None]).astype(np.float32)
    pidx = np.stack([127.0 - j, j], axis=1).astype(np.float32)
    fidx = np.concatenate([np.broadcast_to((j + 1.0)[None, :], (64, 128)),
                           np.broadcast_to((128.0 - j)[None, :], (64, 128))], axis=0).astype(np.float32).copy()
    return dict(rope=rope, ident=ident, iota16=iota16, relm=relm, pidx=pidx, fidx=fidx)


def make_in_maps(inp, NB, n_cores):
    f = lambda a: np.ascontiguousarray(np.asarray(a, dtype=np.float32))
    x, c, ctx, c_ctx = f(inp["x"]), f(inp["c"]), f(inp["ctx"]), f(inp["c_ctx"])
    S = x.shape[1]
    shared = dict(
        ada_w=f(inp["ada_w"]), ada_b=f(inp["ada_b"]),
        ada_bT=np.ascontiguousarray(f(inp["ada_b"]).reshape(2, 48, 128).transpose(0, 2, 1)),
        n1gT=np.ascontiguousarray(f(inp["norm1_g"]).reshape(2, KT, 128).transpose(0, 2, 1)),
        n2g=f(inp["norm2_g"]), final_g=f(inp["final_g"]),
        ev_w_in=f(inp["ev_w_in"])[0], ev_w_out=f(inp["ev_w_out"])[0],
        od_w_in=f(inp["od_w_in"])[0], od_w_out=f(inp["od_w_out"])[0],
        qg=f(inp["gqa_q_norm_g"])[0], kg=f(inp["gqa_k_norm_g"])[0],
        rate=f(inp["ret_log_rate"]).reshape(8), lam=f(inp["diff_lambda"]).reshape(256),
        subln=f(inp["diff_subln_g"]).reshape(128),
        peer_wq=f(inp["peer_w_q"]),
        keysT=np.ascontiguousarray(f(inp["peer_keys"]).reshape(2, 16, 128, 128).transpose(0, 3, 1, 2)),
        peer_u0=f(inp["peer_u"][0]), peer_u1=f(inp["peer_u"][1]),
        peer_v0=f(inp["peer_v"][0]), peer_v1=f(inp["peer_v"][1]),
    )
    shared.update(host_consts(S))
    maps = []
    for i in range(n_cores):
        bs = slice(i * NB, (i + 1) * NB)
        cv = np.concatenate([c[bs], c_ctx[None, :]], axis=0)
        cT = np.ascontiguousarray(cv.reshape(NB + 1, KT, 128).transpose(2, 1, 0))
        m = dict(shared)
        m.update(x=np.ascontiguousarray(x[bs]), ctx=np.ascontiguousarray(ctx[bs]), cT=cT)
        maps.append(m)
    return maps


def kernel(**inputs):
    n_cores = 8
    B, S, _ = inputs["x"].shape
    LC = inputs["ctx"].shape[1]
    NB = B // n_cores
    nc = build(NB, S, LC)
    maps = make_in_maps(inputs, NB, n_cores)
    res = run_bass_kernel_spmd(nc, maps, core_ids=list(range(n_cores)))
    return np.concatenate([r["out"] for r in res.results], axis=0).astype(np.float32)
```
